# Optimizing a Trainium2 kernel written in Bass

```python
import math
import jax
import jax.numpy as jnp
from jax import lax
import numpy as np

D_MODEL = 1024
BATCH = 16
SEQ = 4096
DEPTH = 2

CHUNK = 64
Q_BLOCK = 128
GMLP_BLOCK = 128

A_GROUPS = 4
A_GDIM = 64
A_WIDTH = A_GROUPS * A_GDIM
B_HEADS = 4
B_QK_DIM = 64
B_V_DIM = 2 * B_QK_DIM
B_WIDTH = B_HEADS * B_V_DIM
C_HEADS = 4
C_HEAD_DIM = 64
C_WIDTH = C_HEADS * C_HEAD_DIM
IDX_HEADS = 8
IDX_DIM = 32
TOPK_MAX = 256
MIX_WIDTH = A_WIDTH + B_WIDTH + C_WIDTH

FFN_HIDDEN = -(-8 * D_MODEL // (3 * 256)) * 256

OFF_A_U = 0
OFF_A_V = OFF_A_U + A_WIDTH
OFF_B_Q = OFF_A_V + A_WIDTH
OFF_B_K = OFF_B_Q + B_HEADS * 2 * B_QK_DIM
OFF_B_V = OFF_B_K + B_HEADS * 2 * B_QK_DIM
OFF_C_Q = OFF_B_V + B_WIDTH
OFF_C_K = OFF_C_Q + C_WIDTH
OFF_C_V = OFF_C_K + C_WIDTH
OFF_I_Q = OFF_C_V + C_WIDTH
OFF_I_K = OFF_I_Q + IDX_HEADS * IDX_DIM
OFF_I_W = OFF_I_K + IDX_DIM
IN_WIDTH = OFF_I_W + IDX_HEADS

DEEPNORM_ALPHA = (2 * DEPTH) ** 0.25
DEEPNORM_BETA = (8 * DEPTH) ** -0.25
LN_EPS = 1e-5

kernel_name = 'hybrid_gmlp_diffattn_dsa_deepnorm'


def layer_norm(x, g, b):
    xf = x.astype(jnp.float32)
    mu = jnp.mean(xf, axis=-1, keepdims=True)
    xc = xf - mu
    var = jnp.mean(xc * xc, axis=-1, keepdims=True)
    return (xc * lax.rsqrt(var + LN_EPS) * g.astype(jnp.float32) + b.astype(jnp.float32)).astype(x.dtype)


def rms_norm(x, g):
    xf = x.astype(jnp.float32)
    ms = jnp.mean(xf * xf, axis=-1, keepdims=True)
    return (xf * lax.rsqrt(ms + LN_EPS) * g.astype(jnp.float32)).astype(x.dtype)


def alibi_slopes(n):
    return jnp.asarray([2.0 ** (-8.0 * (h + 1) / n) for h in range(n)], dtype=jnp.float32)


def to_blocks(a, size):
    bn, s = a.shape[:2]
    return jnp.moveaxis(a.reshape((bn, s // size, size) + a.shape[2:]), 1, 0)


def from_blocks(a):
    n, bn, t = a.shape[:3]
    return jnp.moveaxis(a, 0, 1).reshape((bn, n * t) + a.shape[3:])


def gmlp_mixer(u, v, w_s, b_s, ln_g, ln_b):
    bn, s, _ = u.shape
    nblk = s // GMLP_BLOCK
    u = jax.nn.gelu(u)
    v = jax.nn.gelu(v).reshape(bn, nblk, GMLP_BLOCK, A_GROUPS, A_GDIM)
    v = layer_norm(v, ln_g, ln_b)
    pos_chunk = jnp.arange(GMLP_BLOCK) // CHUNK
    mask = pos_chunk[None, :] <= pos_chunk[:, None]
    w = jnp.where(mask[None], w_s, jnp.zeros_like(w_s))
    sg = jnp.einsum('gts,bnsgc->bntgc', w, v) + b_s.T[None, None, :, :, None]
    return u * sg.reshape(bn, s, A_WIDTH)


def diff_attention(q, k, v, lam, lam_init, subln_g):
    bn, s = q.shape[:2]
    nblk = s // Q_BLOCK
    scale = B_QK_DIM ** -0.5
    slopes = alibi_slopes(B_HEADS)
    kpos = jnp.arange(s)
    kchunk = kpos // CHUNK

    def block(args):
        qblk, i = args
        qpos = i * Q_BLOCK + jnp.arange(Q_BLOCK)
        allowed = kchunk[None, :] <= (qpos // CHUNK)[:, None]
        dist = jnp.abs(qpos[:, None] - kpos[None, :]).astype(jnp.float32)
        bias = -slopes[:, None, None] * dist[None]
        logits = jnp.einsum('bthmd,bshmd->bhmts', qblk, k).astype(jnp.float32) * scale
        logits = logits + bias[None, :, None]
        logits = jnp.where(allowed[None, None, None], logits, -jnp.inf)
        p = jax.nn.softmax(logits, axis=-1)
        attn = p[:, :, 0] - lam * p[:, :, 1]
        return jnp.einsum('bhts,bshe->bthe', attn.astype(v.dtype), v)

    o = from_blocks(lax.map(block, (to_blocks(q, Q_BLOCK), jnp.arange(nblk))))
    o = rms_norm(o, subln_g) * (1.0 - lam_init)
    return o.reshape(bn, s, B_WIDTH)


def dsa_attention(q, k, v, qi, ki, wi):
    bn, s = q.shape[:2]
    nblk = s // Q_BLOCK
    topk = min(TOPK_MAX, s // 4)
    scale = C_HEAD_DIM ** -0.5
    idx_scale = (IDX_HEADS ** -0.5) * (IDX_DIM ** -0.5)
    slopes = alibi_slopes(C_HEADS)
    kchunk = jnp.arange(s) // CHUNK

    def block(args):
        qblk, qiblk, wiblk, i = args
        qpos = i * Q_BLOCK + jnp.arange(Q_BLOCK)
        qchunk = qpos // CHUNK
        allowed = kchunk[None, :] <= qchunk[:, None]
        idx_logits = jnp.einsum('bthd,bsd->bths', qiblk, ki).astype(jnp.float32)
        score = jnp.einsum('bth,bths->bts', wiblk.astype(jnp.float32) * idx_scale, jax.nn.relu(idx_logits))
        score = jnp.where(allowed[None], score, -jnp.inf)
        _, sel = lax.top_k(score, topk)
        sel_ok = (sel // CHUNK) <= qchunk[None, :, None]
        k_sel = jax.vmap(lambda kb, ib: kb[ib])(k, sel)
        v_sel = jax.vmap(lambda vb, ib: vb[ib])(v, sel)
        logits = jnp.einsum('bthd,btkhd->bhtk', qblk, k_sel).astype(jnp.float32) * scale
        dist = jnp.abs(qpos[None, :, None] - sel).astype(jnp.float32)
        logits = logits - slopes[None, :, None, None] * dist[:, None]
        logits = jnp.where(sel_ok[:, None], logits, -jnp.inf)
        p = jax.nn.softmax(logits, axis=-1)
        return jnp.einsum('bhtk,btkhd->bthd', p.astype(v_sel.dtype), v_sel)

    xs = (to_blocks(q, Q_BLOCK), to_blocks(qi, Q_BLOCK), to_blocks(wi, Q_BLOCK), jnp.arange(nblk))
    o = from_blocks(lax.map(block, xs))
    return o.reshape(bn, s, C_WIDTH)


def hybrid_layer(x, layer_idx, w_in, w_s, b_s, a_ln_g, a_ln_b, lam_q1, lam_k1, lam_q2, lam_k2,
                 subln_g, w_out, ln1_g, ln1_b, w_gu, w_down, ln2_g, ln2_b):
    bn, s, _ = x.shape
    h = jnp.einsum('bsd,de->bse', x, w_in)
    a_u = h[..., OFF_A_U:OFF_A_V]
    a_v = h[..., OFF_A_V:OFF_B_Q]
    b_q = h[..., OFF_B_Q:OFF_B_K].reshape(bn, s, B_HEADS, 2, B_QK_DIM)
    b_k = h[..., OFF_B_K:OFF_B_V].reshape(bn, s, B_HEADS, 2, B_QK_DIM)
    b_v = h[..., OFF_B_V:OFF_C_Q].reshape(bn, s, B_HEADS, B_V_DIM)
    c_q = h[..., OFF_C_Q:OFF_C_K].reshape(bn, s, C_HEADS, C_HEAD_DIM)
    c_k = h[..., OFF_C_K:OFF_C_V].reshape(bn, s, C_HEADS, C_HEAD_DIM)
    c_v = h[..., OFF_C_V:OFF_I_Q].reshape(bn, s, C_HEADS, C_HEAD_DIM)
    i_q = h[..., OFF_I_Q:OFF_I_K].reshape(bn, s, IDX_HEADS, IDX_DIM)
    i_k = h[..., OFF_I_K:OFF_I_W]
    i_w = h[..., OFF_I_W:IN_WIDTH]

    out_a = gmlp_mixer(a_u, a_v, w_s, b_s, a_ln_g, a_ln_b)

    lam_init = 0.8 - 0.6 * math.exp(-0.3 * layer_idx)
    lam = (jnp.exp(jnp.sum(lam_q1.astype(jnp.float32) * lam_k1.astype(jnp.float32)))
           - jnp.exp(jnp.sum(lam_q2.astype(jnp.float32) * lam_k2.astype(jnp.float32))) + lam_init)
    out_b = diff_attention(b_q, b_k, b_v, lam, lam_init, subln_g)

    out_c = dsa_attention(c_q, c_k, c_v, i_q, i_k, i_w)

    mix = jnp.concatenate([out_a, out_b, out_c], axis=-1)
    x = layer_norm(DEEPNORM_ALPHA * x + jnp.einsum('bse,ed->bsd', mix, w_out), ln1_g, ln1_b)

    gu = jnp.einsum('bsd,df->bsf', x, w_gu)
    gate, up = gu[..., :FFN_HIDDEN], gu[..., FFN_HIDDEN:]
    ffn = jnp.einsum('bsf,fd->bsd', jax.nn.silu(gate) * up, w_down)
    return layer_norm(DEEPNORM_ALPHA * x + ffn, ln2_g, ln2_b)


def setup_inputs(seed: int = 0) -> dict:
    key = jax.random.key(seed)
    ks = jax.random.split(key, 20)
    f32 = jnp.float32
    nrm = lambda k, shape: jax.random.normal(k, shape, dtype=f32)
    return {
        'x': nrm(ks[0], (BATCH, SEQ, D_MODEL)),
        'w_in': nrm(ks[1], (DEPTH, D_MODEL, IN_WIDTH)) * D_MODEL ** -0.5,
        'gmlp_w_s': nrm(ks[2], (DEPTH, A_GROUPS, GMLP_BLOCK, GMLP_BLOCK)) * GMLP_BLOCK ** -0.5,
        'gmlp_b_s': 1.0 + 0.1 * nrm(ks[3], (DEPTH, A_GROUPS, GMLP_BLOCK)),
        'gmlp_ln_g': 1.0 + 0.05 * nrm(ks[4], (DEPTH, A_GROUPS, A_GDIM)),
        'gmlp_ln_b': 0.02 * nrm(ks[5], (DEPTH, A_GROUPS, A_GDIM)),
        'lam_q1': 0.1 * nrm(ks[6], (DEPTH, B_QK_DIM)),
        'lam_k1': 0.1 * nrm(ks[7], (DEPTH, B_QK_DIM)),
        'lam_q2': 0.1 * nrm(ks[8], (DEPTH, B_QK_DIM)),
        'lam_k2': 0.1 * nrm(ks[9], (DEPTH, B_QK_DIM)),
        'diff_subln_g': 1.0 + 0.05 * nrm(ks[10], (DEPTH, B_V_DIM)),
        'w_out': nrm(ks[11], (DEPTH, MIX_WIDTH, D_MODEL)) * MIX_WIDTH ** -0.5 * DEEPNORM_BETA,
        'ln1_g': 1.0 + 0.05 * nrm(ks[12], (DEPTH, D_MODEL)),
        'ln1_b': 0.02 * nrm(ks[13], (DEPTH, D_MODEL)),
        'w_gu': nrm(ks[14], (DEPTH, D_MODEL, 2 * FFN_HIDDEN)) * D_MODEL ** -0.5,
        'w_down': nrm(ks[15], (DEPTH, FFN_HIDDEN, D_MODEL)) * FFN_HIDDEN ** -0.5 * DEEPNORM_BETA,
        'ln2_g': 1.0 + 0.05 * nrm(ks[16], (DEPTH, D_MODEL)),
        'ln2_b': 0.02 * nrm(ks[17], (DEPTH, D_MODEL)),
    }


def reference(x, w_in, gmlp_w_s, gmlp_b_s, gmlp_ln_g, gmlp_ln_b, lam_q1, lam_k1, lam_q2, lam_k2,
              diff_subln_g, w_out, ln1_g, ln1_b, w_gu, w_down, ln2_g, ln2_b):
    for l in range(DEPTH):
        x = hybrid_layer(x, l, w_in[l], gmlp_w_s[l], gmlp_b_s[l], gmlp_ln_g[l], gmlp_ln_b[l],
                         lam_q1[l], lam_k1[l], lam_q2[l], lam_k2[l], diff_subln_g[l], w_out[l],
                         ln1_g[l], ln1_b[l], w_gu[l], w_down[l], ln2_g[l], ln2_b[l])
    return x
```

```python
import math
from contextlib import ExitStack
import numpy as np
import concourse.bass as bass
import concourse.mybir as mybir
from concourse.bass_utils import run_bass_kernel_spmd

F32 = mybir.dt.float32
BF16 = mybir.dt.bfloat16
AF = mybir.ActivationFunctionType
ALU = mybir.AluOpType
AX = mybir.AxisListType

D = 1024
S = 4096
DEPTH = 2
NSEQ = 2
NT = S // 128
NG = S // 512
FF = 2816
NF = FF // 128
IN_W = 3112
OFF_A_U, OFF_A_V, OFF_B_Q, OFF_B_K, OFF_B_V = 0, 256, 512, 1024, 1536
OFF_C_Q, OFF_C_K, OFF_C_V, OFF_I_Q, OFF_I_K, OFF_I_W = 2048, 2304, 2560, 2816, 3072, 3104
ALPHA = (2 * DEPTH) ** 0.25
EPS = 1e-5
SLOPES = [2.0 ** (-8.0 * (h + 1) / 4) for h in range(4)]
WIN = [2, 9, 31, 31]
NBIS = 20
BIS_LO, BIS_W = -16.0, 32.0
TOPK = 256
NEG = -30000.0
IDX_SCALE = (8 ** -0.5) * (32 ** -0.5)


class Tok:
    __slots__ = ("sem", "val")

    def __init__(self, sem, val):
        self.sem = sem
        self.val = val


class DSem:
    def __init__(self, h):
        self.h = h
        self.total = 0


class Eng:
    def __init__(self, name, handle, sem):
        self.name = name
        self.h = handle
        self.sem = sem
        self.count = 0
        self.waited = {}


class Prog:
    def __init__(self, nc, es):
        self.nc = nc
        self.es = es
        self.eng = {}
        for name, h in (("pe", nc.tensor), ("act", nc.scalar), ("dve", nc.vector), ("pool", nc.gpsimd), ("sp", nc.sync)):
            self.eng[name] = Eng(name, h, es.enter_context(nc.semaphore("sem_" + name)))
        self.dsems = {}
        self.last_w = {}
        self.readers = {}
        self.stopped = False

    def dsem(self, name):
        if name not in self.dsems:
            self.dsems[name] = DSem(self.es.enter_context(self.nc.semaphore("dma_" + name)))
        return self.dsems[name]

    def _wait(self, e, tok):
        if isinstance(tok.sem, DSem):
            sem, val = tok.sem.h, tok.sem.total
            key = ("d", id(tok.sem))
        else:
            if tok.sem is e and e.name == "pe":
                return
            sem, val = tok.sem.sem, tok.val
            key = ("e", tok.sem.name)
        if e.waited.get(key, 0) >= val:
            return
        e.h.wait_ge(sem, val)
        e.waited[key] = val

    def op(self, eng, fn, reads=(), writes=(), inc=True, dma=None, nowaw=False):
        if self.stopped:
            return None
        e = self.eng[eng]
        deps = []
        for k in reads:
            t = self.last_w.get(k)
            if t is not None:
                deps.append(t)
        for k in writes:
            t = self.last_w.get(k)
            if t is not None and not nowaw:
                deps.append(t)
            deps.extend(self.readers.get(k, ()))
        for t in deps:
            self._wait(e, t)
        ins = fn(e.h)
        if dma is not None:
            ds = self.dsem(dma)
            ds.total += 16
            ins.then_inc(ds.h, 16)
            tok = Tok(ds, None)
        else:
            if inc:
                e.count += 1
                ins.then_inc(e.sem, 1)
                tok = Tok(e, e.count)
            else:
                tok = Tok(e, e.count + 1)
        for k in reads:
            self.readers.setdefault(k, []).append(tok)
            if len(self.readers[k]) > 24:
                self.readers[k] = self.readers[k][-24:] if False else self.readers[k]
        for k in writes:
            self.last_w[k] = tok
            self.readers[k] = []
        return ins

    def barrier(self):
        if self.stopped:
            return
        names = list(self.eng)
        for n in names:
            e = self.eng[n]
            for m in names:
                if m != n and self.eng[m].count > 0:
                    self._wait(e, Tok(self.eng[m], self.eng[m].count))
            for ds in self.dsems.values():
                if ds.total > 0:
                    self._wait(e, Tok(ds, None))
        self.last_w.clear()
        self.readers.clear()

    def final_wait(self, eng):
        e = self.eng[eng]
        for m in self.eng:
            if m != eng and self.eng[m].count > 0:
                self._wait(e, Tok(self.eng[m], self.eng[m].count))
        for ds in self.dsems.values():
            if ds.total > 0:
                self._wait(e, Tok(ds, None))


class _Stop(Exception):
    pass


def build_program(depth=DEPTH, debug=False, stop=None, dbg_groups=None):
    NGD = NG if dbg_groups is None else dbg_groups
    nc = bass.Bass("TRN2", target_bir_lowering=False)
    dt = nc.dram_tensor

    def din(name, shape):
        return dt(name, list(shape), F32, kind="ExternalInput").ap()

    x_in = din("x", [NSEQ, S, D])
    w_in = din("w_in", [DEPTH, D, IN_W])
    w_s = din("gmlp_w_s", [DEPTH, 4, 128, 128])
    b_s = din("gmlp_b_s", [DEPTH, 4, 128])
    a_g = din("gmlp_ln_g", [DEPTH, 1, 256])
    a_b = din("gmlp_ln_b", [DEPTH, 1, 256])
    lamv = din("lamv", [DEPTH, 4, 64])
    subg = din("diff_subln_g", [DEPTH, 128, 1])
    w_out = din("w_out", [DEPTH, D, D])
    ln1g = din("ln1_g", [DEPTH, 1, D])
    ln1b = din("ln1_b", [DEPTH, 1, D])
    w_gu = din("w_gu", [DEPTH, D, 2 * FF])
    w_dn = din("w_down", [DEPTH, FF, D])
    ln2g = din("ln2_g", [DEPTH, 1, D])
    ln2b = din("ln2_b", [DEPTH, 1, D])
    c_ident = din("c_ident", [128, 128])
    c_diag = din("c_diag", [128, 4, 128])
    c_alibi = din("c_alibi", [128, 4 * 34])
    c_cmask = din("c_cmask", [128, 128])
    c_gmask = din("c_gmask", [128, 128])
    c_jpos = din("c_jpos", [128, 32])
    y_out = dt("y", [NSEQ, S, D], F32, kind="ExternalOutput").ap()

    skind = "ExternalOutput" if debug else "Internal"
    xs = dt("xs", [NSEQ, S, D], F32, kind=skind).ap()
    xT_s = dt("xT_s", [NSEQ, 128, 8, S], BF16, kind=skind).ap()
    ktb_s = dt("ktb_s", [NSEQ, 128, 4, S], BF16, kind=skind).ap()
    ktc_s = dt("ktc_s", [NSEQ, 128, 2, S], BF16, kind=skind).ap()
    ki4_s = dt("ki4_s", [NSEQ, 128, S], BF16, kind=skind).ap()
    vb_s = dt("vb_s", [NSEQ, 128, NT, 512], BF16, kind=skind).ap()
    vc_s = dt("vc_s", [NSEQ, 128, NT, 256], BF16, kind=skind).ap()
    mixT_s = dt("mixT_s", [NSEQ, 128, 8, S], BF16, kind=skind).ap()
    wgu_s = dt("wgu_s", [DEPTH, NF, 128, 8, 256], BF16, kind="Internal").ap()

    with ExitStack() as es:
        E = es.enter_context
        P = Prog(nc, es)
        op = P.op
        dbgbuf = dt("dbgbuf", [128, 16384], F32, kind="ExternalOutput").ap() if debug else None
        dbgpos = {}

        def dump(name, ap2d, ncols, reads, cond=True):
            if not debug or not cond or name in dbgpos:
                return
            off = sum(v[1] for v in dbgpos.values())
            dbgpos[name] = (off, ncols)
            op("pool", lambda h: h.dma_start(out=dbgbuf[0:ap2d.shape[0], off:off + ncols], in_=ap2d), reads=reads, writes=["dbgbuf"], dma="dbg", nowaw=True)
        build_program.dbgpos = dbgpos

        uid = [0]

        def sb(name, shape, dtype, stack=es):
            uid[0] += 1
            return stack.enter_context(nc.sbuf_tensor("%s_%d" % (name, uid[0]), list(shape), dtype))

        ident = sb("ident", [128, 128], BF16)
        ones = sb("ones", [128, 128], BF16)
        diag = sb("diag", [128, 4, 128], BF16)
        alibi = sb("alibi", [128, 4 * 34], F32)
        cmask = sb("cmask", [128, 128], F32)
        gmask = sb("gmask", [128, 128], F32)
        op("pool", lambda h: h.dma_start(out=ident[:], in_=c_ident), writes=["ident"], dma="c0")
        op("pool", lambda h: h.dma_start(out=diag[:], in_=c_diag), writes=["diag"], dma="c0")
        op("sp", lambda h: h.dma_start(out=alibi[:], in_=c_alibi), writes=["alibi"], dma="c1")
        op("sp", lambda h: h.dma_start(out=cmask[:], in_=c_cmask), writes=["cmask"], dma="c1")
        op("sp", lambda h: h.dma_start(out=gmask[:], in_=c_gmask), writes=["gmask"], dma="c1")
        op("dve", lambda h: h.memset(ones[:], 1.0), writes=["ones"])
        jpos1 = sb("jpos1", [128, 32], F32)
        slmat = sb("slmat", [128, 4, 128], BF16)
        op("sp", lambda h: h.dma_start(out=jpos1[:], in_=c_jpos), writes=["jpos1"], dma="c1")
        for hh in range(4):
            op("dve", lambda h, hh=hh: h.memset(slmat[:, hh, :], 8.0 * SLOPES[hh]), writes=["slmat"])

        ps = [E(nc.psum_tensor("ps%d" % i, [128, 512], F32)) for i in range(7)]
        pst = E(nc.psum_tensor("pst", [128, 1024], BF16))
        psk = ["ps%d" % i for i in range(7)]

        for l in range(depth):
            for f in range(NF):
                for half in range(2):
                    src = w_gu[l, :, half * FF + f * 128: half * FF + (f + 1) * 128].rearrange("(c p) j -> p c j", p=128)
                    op("pool", lambda h, src=src, f=f, half=half, l=l: h.dma_start(
                        out=wgu_s[l, f, :, :, half * 128:(half + 1) * 128], in_=src),
                        writes=["wgu_s"], dma="wgucvt", nowaw=True)
        P.barrier()

        phase_ctr = [0]
        if stop == 0:
            depth_iter = []
        else:
            depth_iter = range(depth)
        try:
          for l in depth_iter:
            lam_init = 0.8 - 0.6 * math.exp(-0.3 * l)
            x_src = x_in if l == 0 else xs
            x_dst = y_out if l == depth - 1 else xs
            for s in range(NSEQ):
                with ExitStack() as ph:
                    wk = sb("wk", [128, 8, 1664], BF16, ph)
                    for c in range(8):
                        rows = w_in[l, c * 128:(c + 1) * 128, :]
                        op("pool", lambda h, c=c, rows=rows: h.dma_start(out=wk[:, c, 0:512], in_=rows[:, OFF_B_K:OFF_B_K + 512]), writes=["wk"], dma="wk", nowaw=True)
                        op("pool", lambda h, c=c, rows=rows: h.dma_start(out=wk[:, c, 512:768], in_=rows[:, OFF_C_K:OFF_C_K + 256]), writes=["wk"], dma="wk", nowaw=True)
                        for r in range(4):
                            op("pool", lambda h, c=c, rows=rows, r=r: h.dma_start(out=wk[:, c, 768 + 32 * r:800 + 32 * r], in_=rows[:, OFF_I_K:OFF_I_K + 32]), writes=["wk"], dma="wk", nowaw=True)
                        op("pool", lambda h, c=c, rows=rows: h.dma_start(out=wk[:, c, 896:1408], in_=rows[:, OFF_B_V:OFF_B_V + 512]), writes=["wk"], dma="wk", nowaw=True)
                        op("pool", lambda h, c=c, rows=rows: h.dma_start(out=wk[:, c, 1408:1664], in_=rows[:, OFF_C_V:OFF_C_V + 256]), writes=["wk"], dma="wk", nowaw=True)
                    xin = [sb("xin%d" % i, [128, 4, D], F32, ph) for i in range(2)]
                    xbf = [sb("xbf%d" % i, [128, 4, D], BF16, ph) for i in range(2)]
                    xT = [sb("xT%d" % i, [128, 8, 512], BF16, ph) for i in range(2)]
                    kst = [sb("kst%d" % i, [128, 7, 512], BF16, ph) for i in range(2)]
                    vst = [sb("vst%d" % i, [128, 4, 768], BF16, ph) for i in range(2)]
                    cnt = 0
                    for g in range(NGD):
                        b = g % 2
                        t0 = g * 512
                        op("sp", lambda h, b=b, t0=t0: h.dma_start(out=xin[b][:], in_=x_src[s, t0:t0 + 512, :].rearrange("(r p) d -> p r d", p=128)),
                           writes=["xin%d" % b], dma="xin%d" % b)
                        op("dve" if g % 2 == 0 else "pool", lambda h, b=b: h.tensor_copy(out=xbf[b][:], in_=xin[b][:]), reads=["xin%d" % b], writes=["xbf%d" % b])
                        for r in range(4):
                            for c in range(8):
                                op("pe", lambda h, b=b, r=r, c=c: h.transpose(pst[:, c * 128:(c + 1) * 128], xbf[b][:, r, c * 128:(c + 1) * 128], ident[:]),
                                   reads=["xbf%d" % b, "ident"], writes=["pst"], inc=(c == 7))
                            op("act" if r % 2 == 0 else "dve",
                               (lambda h, b=b, r=r: h.copy(out=xT[b][:, :, r * 128:(r + 1) * 128], in_=pst[:].rearrange("p (c t) -> p c t", c=8))) if r % 2 == 0 else
                               (lambda h, b=b, r=r: h.tensor_copy(out=xT[b][:, :, r * 128:(r + 1) * 128], in_=pst[:].rearrange("p (c t) -> p c t", c=8))),
                               reads=["pst"], writes=["xT%d" % b])
                        op("sp", lambda h, b=b, t0=t0: h.dma_start(out=xT_s[s, :, :, t0:t0 + 512], in_=xT[b][:]), reads=["xT%d" % b], writes=["xT_s"], dma="st_xT", nowaw=True)
                        for cc in range(7):
                            pk = cnt % 6
                            cnt += 1
                            for c in range(8):
                                op("pe", lambda h, pk=pk, cc=cc, c=c, b=b: h.matmul(ps[pk][:], wk[:, c, cc * 128:(cc + 1) * 128], xT[b][:, c, :], start=(c == 0), stop=(c == 7)),
                                   reads=["wk", "xT%d" % b], writes=[psk[pk]], inc=(c == 7))
                            if cc % 2 == 0:
                                op("act", lambda h, pk=pk, cc=cc, b=b: h.copy(out=kst[b][:, cc, :], in_=ps[pk][:]), reads=[psk[pk]], writes=["kst%d" % b])
                            else:
                                op("dve", lambda h, pk=pk, cc=cc, b=b: h.tensor_copy(out=kst[b][:, cc, :], in_=ps[pk][:]), reads=[psk[pk]], writes=["kst%d" % b])
                        op("sp", lambda h, b=b, t0=t0: h.dma_start(out=ktb_s[s, :, :, t0:t0 + 512], in_=kst[b][:, 0:4, :]), reads=["kst%d" % b], writes=["ktb_s"], dma="st_k", nowaw=True)
                        op("sp", lambda h, b=b, t0=t0: h.dma_start(out=ktc_s[s, :, :, t0:t0 + 512], in_=kst[b][:, 4:6, :]), reads=["kst%d" % b], writes=["ktc_s"], dma="st_k", nowaw=True)
                        op("sp", lambda h, b=b, t0=t0: h.dma_start(out=ki4_s[s, :, t0:t0 + 512], in_=kst[b][:, 6, :]), reads=["kst%d" % b], writes=["ki4_s"], dma="st_k", nowaw=True)
                        for r in range(4):
                            pk = cnt % 6
                            cnt += 1
                            for c in range(8):
                                op("pe", lambda h, pk=pk, r=r, c=c, b=b: h.matmul(ps[pk][:], xT[b][:, c, r * 128:(r + 1) * 128], wk[:, c, 896:1408], start=(c == 0), stop=(c == 7)),
                                   reads=["wk", "xT%d" % b], writes=[psk[pk]], inc=(c == 7))
                            op("act", lambda h, pk=pk, r=r, b=b: h.copy(out=vst[b][:, r, 0:512], in_=ps[pk][:]), reads=[psk[pk]], writes=["vst%d" % b])
                            pk = cnt % 6
                            cnt += 1
                            for c in range(8):
                                op("pe", lambda h, pk=pk, r=r, c=c, b=b: h.matmul(ps[pk][:, 0:256], xT[b][:, c, r * 128:(r + 1) * 128], wk[:, c, 1408:1664], start=(c == 0), stop=(c == 7)),
                                   reads=["wk", "xT%d" % b], writes=[psk[pk]], inc=(c == 7))
                            op("dve", lambda h, pk=pk, r=r, b=b: h.tensor_copy(out=vst[b][:, r, 512:768], in_=ps[pk][:, 0:256]), reads=[psk[pk]], writes=["vst%d" % b])
                        op("sp", lambda h, b=b, g=g: h.dma_start(out=vb_s[s, :, 4 * g:4 * g + 4, :], in_=vst[b][:, :, 0:512]), reads=["vst%d" % b], writes=["vb_s"], dma="st_v", nowaw=True)
                        op("sp", lambda h, b=b, g=g: h.dma_start(out=vc_s[s, :, 4 * g:4 * g + 4, :], in_=vst[b][:, :, 512:768]), reads=["vst%d" % b], writes=["vc_s"], dma="st_v", nowaw=True)
                    P.barrier()
                    phase_ctr[0] += 1
                    if stop is not None and phase_ctr[0] >= stop:
                        P.stopped = True

                with ExitStack() as ph:
                    ktc = sb("ktc", [128, 2, S], BF16, ph)
                    vc = sb("vc", [128, NT, 256], BF16, ph)
                    ki4 = sb("ki4", [128, S], BF16, ph)
                    op("sp", lambda h: h.dma_start(out=ktc[:], in_=ktc_s[s]), writes=["ktc"], dma="ldkv")
                    op("sp", lambda h: h.dma_start(out=vc[:], in_=vc_s[s]), writes=["vc"], dma="ldkv")
                    op("sp", lambda h: h.dma_start(out=ki4[:], in_=ki4_s[s]), writes=["ki4"], dma="ldkv")
                    wq = sb("wq", [128, 8, 768], BF16, ph)
                    wv = sb("wv", [128, 8, 264], BF16, ph)
                    for c in range(8):
                        rows = w_in[l, c * 128:(c + 1) * 128, :]
                        op("pool", lambda h, c=c, rows=rows: h.dma_start(out=wq[:, c, 0:256], in_=rows[:, OFF_A_U:OFF_A_U + 256]), writes=["wq"], dma="wq", nowaw=True)
                        op("pool", lambda h, c=c, rows=rows: h.dma_start(out=wq[:, c, 256:512], in_=rows[:, OFF_C_Q:OFF_C_Q + 256]), writes=["wq"], dma="wq", nowaw=True)
                        op("pool", lambda h, c=c, rows=rows: h.dma_start(out=wq[:, c, 512:768], in_=rows[:, OFF_I_Q:OFF_I_Q + 256]), writes=["wq"], dma="wq", nowaw=True)
                        op("pool", lambda h, c=c, rows=rows: h.dma_start(out=wv[:, c, 0:256], in_=rows[:, OFF_A_V:OFF_A_V + 256]), writes=["wv"], dma="wq", nowaw=True)
                        op("pool", lambda h, c=c, rows=rows: h.dma_start(out=wv[:, c, 256:264], in_=rows[:, OFF_I_W:OFF_I_W + 8]), writes=["wv"], dma="wq", nowaw=True)
                    wmT = sb("wmT", [128, 4, 128], BF16, ph)
                    wmf = sb("wmf", [128, 4, 128], F32, ph)
                    bsT = sb("bsT", [128, 2, 128], F32, ph)
                    lng = sb("lng", [128, 256], F32, ph)
                    lnb = sb("lnb", [128, 256], F32, ph)
                    wmb = sb("wmb", [128, 4, 128], BF16, ph)
                    op("sp", lambda h: h.dma_start(out=wmf[:], in_=w_s[l].rearrange("g t s -> t g s")), writes=["wmf"], dma="gpw")
                    op("dve", lambda h: h.tensor_copy(out=wmb[:], in_=wmf[:]), reads=["wmf"], writes=["wmb"])
                    for gg in range(4):
                        op("pe", lambda h, gg=gg: h.transpose(pst[:, gg * 128:(gg + 1) * 128], wmb[:, gg, :], ident[:]), reads=["wmb", "ident"], writes=["pst"], inc=(gg == 3))
                    for gg in range(4):
                        op("sp", lambda h, gg=gg: h.dma_start(out=bsT[(gg % 2) * 64:(gg % 2) * 64 + 64, gg // 2, :], in_=b_s[l, gg:gg + 1, :].partition_broadcast(64)),
                           writes=["bsT"], dma="gp", nowaw=True)
                    op("sp", lambda h: h.dma_start(out=lng[:], in_=a_g[l].partition_broadcast(128)), writes=["lng"], dma="gp")
                    op("sp", lambda h: h.dma_start(out=lnb[:], in_=a_b[l].partition_broadcast(128)), writes=["lnb"], dma="gp")
                    for gg in range(4):
                        op("dve", lambda h, gg=gg: h.tensor_tensor(out=wmT[:, gg, :], in0=pst[:, gg * 128:(gg + 1) * 128], in1=gmask[:], op=ALU.mult), reads=["pst", "gmask"], writes=["wmT"])

                    xT = [sb("xTa%d" % i, [128, 8, 512], BF16, ph) for i in range(2)]
                    uT = sb("uT", [128, 2, 512], BF16, ph)
                    qcp = sb("qcp", [128, 4, 512], BF16, ph)
                    qip = sb("qip", [128, 8, 512], BF16, ph)
                    vtm = sb("vtm", [128, 264], F32, ph)
                    vg = sb("vg", [128, 256], F32, ph)
                    vsq = sb("vsq", [128, 256], F32, ph)
                    st4 = sb("st4", [128, 16], F32, ph)
                    vn = sb("vn", [128, 256], BF16, ph)
                    wab = sb("wab", [128, 8], F32, ph)
                    sgn = sb("sgn", [128, 8], F32, ph)
                    dsg = sb("dsg", [128, 8, 128], BF16, ph)
                    score = sb("score", [128, S], F32, ph)
                    mb = sb("mb", [128, S], BF16, ph)
                    mbT = sb("mbT", [128, NT, 128], BF16, ph)
                    rl = [sb("rl%d" % i, [128, 512], BF16, ph) for i in range(4)]
                    pT = [sb("pTa%d" % i, [128, 4, 128], BF16, ph) for i in range(3)]
                    bis = sb("bis", [128, 8], F32, ph)
                    mixT = [sb("mixa%d" % i, [128, 4, 512], BF16, ph) for i in range(2)]
                    tmpa = sb("tmpa", [128, 128], F32, ph)
                    rcp = sb("rcp", [128, 128], F32, ph)
                    op("dve", lambda h: h.memset(qcp[:], 0.0), writes=["qcp"])
                    op("pool", lambda h: h.memset(qip[:], 0.0), writes=["qip"])
                    op("pool", lambda h: h.memset(mbT[:], 0.0), writes=["mbT"])
                    dblk = sb("dblk", [128, 128], BF16, ph)
                    dT = sb("dT", [128, 128], BF16, ph)
                    anys = sb("anys", [128, 64], F32, ph)
                    op("dve", lambda h: h.memset(dblk[:], 0.0), writes=["dblk"])
                    op("dve", lambda h: h.memset(dT[:], 0.0), writes=["dT"])

                    rlc = 0
                    ptc = 0
                    for g in range(NGD):
                        b = g % 2
                        t0 = g * 512
                        mx = mixT[b]
                        mxk = "mixa%d" % b
                        op("sp", lambda h, b=b, t0=t0: h.dma_start(out=xT[b][:], in_=xT_s[s, :, :, t0:t0 + 512]), writes=["xTa%d" % b], dma="ldxT%d" % b)
                        for cc in range(6):
                            pk = cc % 2
                            for c in range(8):
                                op("pe", lambda h, pk=pk, cc=cc, c=c, b=b: h.matmul(ps[pk][:], wq[:, c, cc * 128:(cc + 1) * 128], xT[b][:, c, :], start=(c == 0), stop=(c == 7)),
                                   reads=["wq", "xTa%d" % b], writes=[psk[pk]], inc=(c == 7))
                            if cc < 2:
                                op("act", lambda h, pk=pk, cc=cc: h.activation(out=uT[:, cc, :], in_=ps[pk][:], func=AF.Gelu_apprx_tanh), reads=[psk[pk]], writes=["uT"])
                            elif cc < 4:
                                for k in range(2):
                                    hh = (cc - 2) * 2 + k
                                    op("dve", lambda h, pk=pk, hh=hh, k=k: h.tensor_copy(out=qcp[64 * k:64 * k + 64, hh, :], in_=ps[pk][64 * k:64 * k + 64, :]), reads=[psk[pk]], writes=["qcp"])
                            else:
                                for k in range(4):
                                    hh = (cc - 4) * 4 + k
                                    op("dve" if k % 2 == 0 else "act",
                                       (lambda h, pk=pk, hh=hh, k=k: h.tensor_copy(out=qip[32 * k:32 * k + 32, hh, :], in_=ps[pk][32 * k:32 * k + 32, :])) if k % 2 == 0 else
                                       (lambda h, pk=pk, hh=hh, k=k: h.copy(out=qip[32 * k:32 * k + 32, hh, :], in_=ps[pk][32 * k:32 * k + 32, :])),
                                       reads=[psk[pk]], writes=["qip"])
                        for r in range(4):
                            i = 4 * g + r
                            q0 = r * 128
                            for c in range(8):
                                op("pe", lambda h, c=c, b=b, q0=q0: h.matmul(ps[4][:, 0:264], xT[b][:, c, q0:q0 + 128], wv[:, c, :], start=(c == 0), stop=(c == 7)),
                                   reads=["wv", "xTa%d" % b], writes=[psk[4]], inc=(c == 7))
                            op("act", lambda h: h.activation(out=vg[:], in_=ps[4][:, 0:256], func=AF.Gelu_apprx_tanh), reads=[psk[4]], writes=["vg"])
                            op("dve", lambda h: h.tensor_copy(out=vtm[:, 256:264], in_=ps[4][:, 256:264]), reads=[psk[4]], writes=["vtm"])
                            op("dve", lambda h: h.tensor_scalar(out=wab[:], in0=vtm[:, 256:264], scalar1=-1.0, scalar2=None, op0=ALU.mult), reads=["vtm"], writes=["wab"])
                            op("dve", lambda h: h.tensor_tensor(out=wab[:], in0=wab[:], in1=vtm[:, 256:264], op=ALU.max), reads=["vtm", "wab"], writes=["wab"])
                            op("dve", lambda h: h.tensor_scalar(out=wab[:], in0=wab[:], scalar1=IDX_SCALE, scalar2=None, op0=ALU.mult), reads=["wab"], writes=["wab"])
                            op("dve", lambda h: h.tensor_scalar(out=sgn[:], in0=vtm[:, 256:264], scalar1=0.0, scalar2=2.0, op0=ALU.is_ge, op1=ALU.mult), reads=["vtm"], writes=["sgn"])
                            op("dve", lambda h: h.tensor_scalar(out=sgn[:], in0=sgn[:], scalar1=-1.0, scalar2=None, op0=ALU.add), reads=["sgn"], writes=["sgn"])
                            for hh in range(8):
                                op("dve", lambda h, hh=hh: h.tensor_scalar(out=dsg[:, hh, :], in0=ident[:], scalar1=sgn[:, hh:hh + 1], scalar2=None, op0=ALU.mult),
                                   reads=["ident", "sgn"], writes=["dsg%d" % hh])
                            v3 = vg[:].rearrange("p (g c) -> p g c", c=64)
                            op("dve", lambda h: h.tensor_reduce(out=st4[:, 0:4], in_=v3, axis=AX.X, op=ALU.add), reads=["vg"], writes=["st4"])
                            op("dve", lambda h: h.tensor_tensor(out=vsq[:], in0=vg[:], in1=vg[:], op=ALU.mult), reads=["vg"], writes=["vsq"])
                            op("dve", lambda h: h.tensor_reduce(out=st4[:, 4:8], in_=vsq[:].rearrange("p (g c) -> p g c", c=64), axis=AX.X, op=ALU.add), reads=["vsq"], writes=["st4"])
                            op("dve", lambda h: h.tensor_scalar(out=st4[:, 0:4], in0=st4[:, 0:4], scalar1=1.0 / 64, scalar2=None, op0=ALU.mult), reads=["st4"], writes=["st4"])
                            op("dve", lambda h: h.tensor_tensor(out=st4[:, 8:12], in0=st4[:, 0:4], in1=st4[:, 0:4], op=ALU.mult), reads=["st4"], writes=["st4"])
                            op("dve", lambda h: h.scalar_tensor_tensor(out=st4[:, 4:8], in0=st4[:, 4:8], scalar=1.0 / 64, in1=st4[:, 8:12], op0=ALU.mult, op1=ALU.subtract), reads=["st4"], writes=["st4"])
                            op("act", lambda h: h.activation(out=st4[:, 8:12], in_=st4[:, 4:8], func=AF.Ln, bias=EPS), reads=["st4"], writes=["st4"])
                            op("act", lambda h: h.activation(out=st4[:, 12:16], in_=st4[:, 8:12], func=AF.Exp, scale=-0.5), reads=["st4"], writes=["st4"])
                            for gg in range(4):
                                op("dve", lambda h, gg=gg: h.tensor_scalar(out=vsq[:, gg * 64:(gg + 1) * 64], in0=vg[:, gg * 64:(gg + 1) * 64], scalar1=st4[:, gg:gg + 1], scalar2=st4[:, 12 + gg:13 + gg],
                                                                    op0=ALU.subtract, op1=ALU.mult), reads=["vg", "st4"], writes=["vsq"])
                            op("dve", lambda h: h.tensor_tensor(out=vsq[:], in0=vsq[:], in1=lng[:], op=ALU.mult), reads=["vsq", "lng"], writes=["vsq"])
                            op("dve", lambda h: h.tensor_tensor(out=vn[:], in0=vsq[:], in1=lnb[:], op=ALU.add), reads=["vsq", "lnb"], writes=["vn"])
                            for gg in range(4):
                                ck = gg // 2
                                op("pe", lambda h, gg=gg, ck=ck: h.matmul(ps[5][:, (gg % 2) * 128:(gg % 2) * 128 + 128], vn[:, ck * 128:(ck + 1) * 128], wmT[:, gg, :], start=True, stop=True),
                                   reads=["vn", "wmT"], writes=[psk[5]], inc=True)
                                rs = slice((gg % 2) * 64, (gg % 2) * 64 + 64)
                                op("dve", lambda h, gg=gg, ck=ck, rs=rs: h.tensor_tensor(out=tmpa[rs, :], in0=ps[5][rs, (gg % 2) * 128:(gg % 2) * 128 + 128], in1=bsT[rs, ck, :], op=ALU.add),
                                   reads=[psk[5], "bsT"], writes=["tmpa"])
                                op("dve", lambda h, gg=gg, ck=ck, rs=rs, q0=q0: h.tensor_tensor(out=mx[rs, ck, q0:q0 + 128], in0=tmpa[rs, :], in1=uT[rs, ck, q0:q0 + 128], op=ALU.mult),
                                   reads=["tmpa", "uT"], writes=[mxk])
                            nk = 128 * (i + 1)
                            if i >= 2:
                                for c0 in range(0, nk, 512):
                                    cw = min(512, nk - c0)
                                    def emit_L(hh, c0=c0, cw=cw, q0=q0):
                                        pk = 4 + hh % 2
                                        op("pe", lambda h: h.matmul(ps[pk][:, 0:cw], qip[:, hh, q0:q0 + 128], ki4[:, c0:c0 + cw], start=True, stop=True),
                                           reads=["qip", "ki4"], writes=[psk[pk]], inc=True)
                                    emit_L(0)
                                    for hh in range(8):
                                        pk = 4 + hh % 2
                                        if hh + 1 < 8:
                                            emit_L(hh + 1)
                                        rb = rlc % 4
                                        rlc += 1
                                        op("act", lambda h, pk=pk, hh=hh, cw=cw, rb=rb: h.activation(out=rl[rb][:, 0:cw], in_=ps[pk][:, 0:cw], func=AF.Relu, scale=wab[:, hh:hh + 1]),
                                           reads=[psk[pk], "wab"], writes=["rl%d" % rb])
                                        op("pe", lambda h, hh=hh, cw=cw, rb=rb: h.matmul(ps[6][:, 0:cw], dsg[:, hh, :], rl[rb][:, 0:cw], start=(hh == 0), stop=(hh == 7)),
                                           reads=["dsg%d" % hh, "rl%d" % rb], writes=[psk[6]], inc=(hh == 7))
                                    if c0 + cw == nk:
                                        if cw > 128:
                                            op("dve", lambda h, c0=c0, cw=cw: h.tensor_copy(out=score[:, c0:c0 + cw - 128], in_=ps[6][:, 0:cw - 128]), reads=[psk[6]], writes=["score"])
                                        op("dve", lambda h, c0=c0, cw=cw: h.tensor_tensor(out=score[:, c0 + cw - 128:c0 + cw], in0=ps[6][:, cw - 128:cw], in1=cmask[:], op=ALU.add),
                                           reads=[psk[6], "cmask"], writes=["score"])
                                    else:
                                        op("dve", lambda h, c0=c0, cw=cw: h.tensor_copy(out=score[:, c0:c0 + cw], in_=ps[6][:, 0:cw]), reads=[psk[6]], writes=["score"])
                                op("dve", lambda h: h.memset(bis[:, 0:1], BIS_LO), writes=["bis"])
                                for it in range(NBIS):
                                    cst = BIS_W / (2.0 ** (it + 1))
                                    op("dve", lambda h, cst=cst: h.tensor_scalar(out=bis[:, 1:2], in0=bis[:, 0:1], scalar1=cst, scalar2=None, op0=ALU.add), reads=["bis"], writes=["bis"])
                                    op("dve", lambda h, nk=nk: h.tensor_scalar(out=mb[:, 0:nk], in0=score[:, 0:nk], scalar1=bis[:, 1:2], scalar2=None, op0=ALU.is_ge, op1=ALU.add, accum_out=bis[:, 2:3]),
                                       reads=["score", "bis"], writes=["mb", "bis"])
                                    op("dve", lambda h, cst=cst: h.tensor_scalar(out=bis[:, 3:4], in0=bis[:, 2:3], scalar1=float(TOPK), scalar2=cst, op0=ALU.is_ge, op1=ALU.mult), reads=["bis"], writes=["bis"])
                                    op("dve", lambda h: h.tensor_tensor(out=bis[:, 0:1], in0=bis[:, 0:1], in1=bis[:, 3:4], op=ALU.add), reads=["bis"], writes=["bis"])
                                op("dve", lambda h, nk=nk: h.tensor_scalar(out=mb[:, 0:nk], in0=score[:, 0:nk], scalar1=bis[:, 0:1], scalar2=NEG, op0=ALU.is_lt, op1=ALU.mult), reads=["score", "bis"], writes=["mb"])
                                for j0 in range(0, i + 1, 8):
                                    nj = min(8, i + 1 - j0)
                                    for jj in range(nj):
                                        op("pe", lambda h, j0=j0, jj=jj: h.transpose(pst[:, jj * 128:(jj + 1) * 128], mb[:, (j0 + jj) * 128:(j0 + jj + 1) * 128], ident[:]),
                                           reads=["mb", "ident"], writes=["pst"], inc=(jj == nj - 1))
                                    op("act" if (j0 // 8) % 2 == 0 else "dve",
                                       (lambda h, j0=j0, nj=nj: h.copy(out=mbT[:, j0:j0 + nj, :], in_=pst[:, 0:nj * 128].rearrange("p (j t) -> p j t", t=128))) if (j0 // 8) % 2 == 0 else
                                       (lambda h, j0=j0, nj=nj: h.tensor_copy(out=mbT[:, j0:j0 + nj, :], in_=pst[:, 0:nj * 128].rearrange("p (j t) -> p j t", t=128))),
                                       reads=["pst"], writes=["mbT"])
                                nb = i + 1
                                op("dve", lambda h, nk=nk, nb=nb: h.tensor_reduce(out=anys[:, 0:nb], in_=mb[:, 0:nk].rearrange("p (j k) -> p j k", k=128), axis=AX.X, op=ALU.max), reads=["mb"], writes=["anys"])
                                op("dve", lambda h, nb=nb: h.tensor_scalar(out=anys[:, 0:nb], in0=anys[:, 0:nb], scalar1=-1.0, scalar2=None, op0=ALU.is_ge), reads=["anys"], writes=["anys"])
                                op("dve", lambda h, nb=nb: h.tensor_tensor(out=anys[:, 0:nb], in0=anys[:, 0:nb], in1=jpos1[:, 0:nb], op=ALU.mult), reads=["anys", "jpos1"], writes=["anys"])
                                op("dve", lambda h, nb=nb: h.tensor_reduce(out=anys[:, 32:33], in_=anys[:, 0:nb], axis=AX.X, op=ALU.max), reads=["anys"], writes=["anys"])
                                op("dve", lambda h, nb=nb: h.tensor_scalar(out=dblk[:, 0:1], in0=anys[:, 32:33], scalar1=-128.0, scalar2=128.0 * nb, op0=ALU.mult, op1=ALU.add), reads=["anys"], writes=["dblk"])
                                op("pe", lambda h: h.transpose(pst[:, 0:128], dblk[:], ident[:]), reads=["dblk", "ident"], writes=["pst"], inc=True)
                                op("dve", lambda h: h.tensor_copy(out=dT[:], in_=pst[:, 0:128]), reads=["pst"], writes=["dT"])
                            else:
                                if i == 0 and g == 0:
                                    pass
                            if debug and l == 0 and s == 0 and i == 2:
                                dump("vn", vn[:], 256, ["vn"])
                                dump("wmT", wmT[:].rearrange("p a b -> p (a b)"), 512, ["wmT"])
                                dump("st4", st4[:], 16, ["st4"])
                                dump("tmpa", tmpa[:], 128, ["tmpa"])
                                dump("vg", vg[:], 256, ["vg"])
                                dump("uT", uT[:].rearrange("p a b -> p (a b)")[:, 0:512], 512, ["uT"])
                                dump("score", score[:, 0:384], 384, ["score"])
                                dump("bis", bis[:], 8, ["bis"])
                                dump("mb", mb[:, 0:384], 384, ["mb"])
                                dump("mbT", mbT[:].rearrange("p a b -> p (a b)")[:, 0:384], 384, ["mbT"])
                                dump("anys", anys[:], 64, ["anys"])
                                dump("dT", dT[:], 128, ["dT"])
                                dump("wab", wab[:], 8, ["wab"])
                                dump("sgn", sgn[:], 8, ["sgn"])
                                dump("bsT", bsT[:].rearrange("p a b -> p (a b)"), 256, ["bsT"])
                            pend = []

                            def emit_pv_c(item, i=i):
                                j, pslot = item
                                first, last = (j == 0), (j == i)
                                pk_ = "pTa%d" % pslot
                                for hh in range(4):
                                    ab = hh // 2
                                    co = (hh % 2) * 256
                                    op("pe", lambda h: h.matmul(ps[ab][:, co:co + 128], vc[:, j, ab * 128:(ab + 1) * 128], pT[pslot][:, hh, :], start=(first and hh % 2 == 0), stop=last, skip_group_check=True),
                                       reads=["vc", pk_], writes=[psk[ab]], inc=False)
                                    op("pe", lambda h: h.matmul(ps[ab][:, co + 128:co + 256], ones[:], pT[pslot][:, hh, :], start=False, stop=last, skip_group_check=True),
                                       reads=["ones", pk_], writes=[psk[ab]], inc=(hh == 3))

                            for j in range(0, i + 1):
                                sbk = 2 + (ptc % 4)
                                pslot = ptc % 3
                                ptc += 1
                                dg = (j == i)
                                for hh in range(4):
                                    ck = hh // 2
                                    cs = hh * 128
                                    op("pe", lambda h, sbk=sbk, cs=cs, hh=hh, j=j, ck=ck: h.matmul(ps[sbk][:, cs:cs + 128], ktc[:, ck, j * 128:(j + 1) * 128], qcp[:, hh, q0:q0 + 128], start=(hh == 0), stop=False, skip_group_check=True),
                                       reads=["ktc", "qcp"], writes=[psk[sbk]], inc=False)
                                    op("pe", lambda h, sbk=sbk, cs=cs, j=j: h.matmul(ps[sbk][:, cs:cs + 128], ident[:], mbT[:, j, :], start=False, stop=False, skip_group_check=True),
                                       reads=["ident", "mbT"], writes=[psk[sbk]], inc=False)
                                    op("pe", lambda h, sbk=sbk, cs=cs, hh=hh, dg=dg: h.matmul(ps[sbk][:, cs:cs + 128], slmat[:, hh, :], dT[:], start=False, stop=(not dg), skip_group_check=True),
                                       reads=["slmat", "dT"], writes=[psk[sbk]], inc=((not dg) and hh == 3))
                                    if dg:
                                        op("pe", lambda h, sbk=sbk, cs=cs, hh=hh: h.matmul(ps[sbk][:, cs:cs + 128], ident[:], diag[:, hh, :], start=False, stop=True, skip_group_check=True),
                                           reads=["ident", "diag"], writes=[psk[sbk]], inc=(hh == 3))
                                if dg:
                                    op("act", lambda h, sbk=sbk, pslot=pslot: h.activation(out=pT[pslot][:].rearrange("p a b -> p (a b)"), in_=ps[sbk][:], func=AF.Exp, scale=0.125),
                                       reads=[psk[sbk]], writes=["pTa%d" % pslot])
                                else:
                                    kk = i - j + 1
                                    for hh in range(4):
                                        op("act", lambda h, sbk=sbk, pslot=pslot, hh=hh, kk=kk: h.activation(out=pT[pslot][:, hh, :], in_=ps[sbk][:, hh * 128:(hh + 1) * 128], func=AF.Exp, scale=0.125,
                                                                                                    bias=alibi[:, hh * 34 + kk:hh * 34 + kk + 1]),
                                           reads=[psk[sbk], "alibi"], writes=["pTa%d" % pslot])
                                pend.append((j, pslot))
                                if len(pend) > 1:
                                    emit_pv_c(pend.pop(0))
                            while pend:
                                emit_pv_c(pend.pop(0))
                            for hh in range(4):
                                ab = hh // 2
                                co = (hh % 2) * 256
                                rs = slice(64 * (hh % 2), 64 * (hh % 2) + 64)
                                op("dve", lambda h, ab=ab, co=co, rs=rs: h.reciprocal(out=rcp[rs, :], in_=ps[ab][rs, co + 128:co + 256]), reads=[psk[ab]], writes=["rcp"])
                                op("dve", lambda h, ab=ab, co=co, rs=rs, q0=q0: h.tensor_tensor(out=mx[rs, 2 + ab, q0:q0 + 128], in0=ps[ab][rs, co:co + 128], in1=rcp[rs, :], op=ALU.mult),
                                   reads=[psk[ab], "rcp"], writes=[mxk])
                        op("sp", lambda h, b=b, t0=t0: h.dma_start(out=mixT_s[s, :, 0:2, t0:t0 + 512], in_=mixT[b][:, 0:2, :]), reads=[mxk], writes=["mixT_s"], dma="st_mx", nowaw=True)
                        op("sp", lambda h, b=b, t0=t0: h.dma_start(out=mixT_s[s, :, 6:8, t0:t0 + 512], in_=mixT[b][:, 2:4, :]), reads=[mxk], writes=["mixT_s"], dma="st_mx", nowaw=True)
                    P.barrier()
                    phase_ctr[0] += 1
                    if stop is not None and phase_ctr[0] >= stop:
                        P.stopped = True

                with ExitStack() as ph:
                    ktb = sb("ktb", [128, 4, S], BF16, ph)
                    vb = sb("vb", [128, NT, 512], BF16, ph)
                    op("sp", lambda h: h.dma_start(out=ktb[:], in_=ktb_s[s]), writes=["ktb"], dma="ldkv")
                    op("sp", lambda h: h.dma_start(out=vb[:], in_=vb_s[s]), writes=["vb"], dma="ldkv")
                    wq = sb("wqb", [128, 8, 512], BF16, ph)
                    for c in range(8):
                        op("pool", lambda h, c=c: h.dma_start(out=wq[:, c, :], in_=w_in[l, c * 128:(c + 1) * 128, OFF_B_Q:OFF_B_Q + 512]), writes=["wqb"], dma="wq", nowaw=True)
                    lv = sb("lv", [128, 4, 64], F32, ph)
                    lsm = sb("lsm", [128, 8], F32, ph)
                    gcol = sb("gcol", [128, 1], F32, ph)
                    op("sp", lambda h: h.dma_start(out=lv[:].rearrange("p a b -> p (a b)"), in_=lamv[l:l + 1].rearrange("o a b -> o (a b)").partition_broadcast(128)), writes=["lv"], dma="gp")
                    op("sp", lambda h: h.dma_start(out=gcol[:], in_=subg[l]), writes=["gcol"], dma="gp")
                    op("dve", lambda h: h.tensor_tensor(out=lv[:, 0, :], in0=lv[:, 0, :], in1=lv[:, 1, :], op=ALU.mult), reads=["lv"], writes=["lv"])
                    op("dve", lambda h: h.tensor_tensor(out=lv[:, 2, :], in0=lv[:, 2, :], in1=lv[:, 3, :], op=ALU.mult), reads=["lv"], writes=["lv"])
                    op("dve", lambda h: h.tensor_reduce(out=lsm[:, 0:1], in_=lv[:, 0, :], axis=AX.X, op=ALU.add), reads=["lv"], writes=["lsm"])
                    op("dve", lambda h: h.tensor_reduce(out=lsm[:, 1:2], in_=lv[:, 2, :], axis=AX.X, op=ALU.add), reads=["lv"], writes=["lsm"])
                    op("act", lambda h: h.activation(out=lsm[:, 2:4], in_=lsm[:, 0:2], func=AF.Exp), reads=["lsm"], writes=["lsm2"])
                    op("dve", lambda h: h.tensor_tensor(out=lsm[:, 4:5], in0=lsm[:, 3:4], in1=lsm[:, 2:3], op=ALU.subtract), reads=["lsm2"], writes=["lsm3"])
                    op("dve", lambda h: h.tensor_scalar(out=lsm[:, 5:6], in0=lsm[:, 4:5], scalar1=-lam_init, scalar2=None, op0=ALU.add), reads=["lsm3"], writes=["neglam"])
                    xT = [sb("xTb%d" % i, [128, 8, 512], BF16, ph) for i in range(2)]
                    qbp = sb("qbp", [128, 8, 512], BF16, ph)
                    pT = [sb("pTb%d" % i, [128, 4, 128], BF16, ph) for i in range(3)]
                    mixT = [sb("mixb%d" % i, [128, 4, 512], BF16, ph) for i in range(2)]
                    r1 = sb("r1", [128, 256], F32, ph)
                    oa = sb("oa", [128, 128], F32, ph)
                    ob = sb("ob", [128, 128], F32, ph)
                    oo = sb("oo", [128, 128], F32, ph)
                    osq = sb("osq", [128, 128], BF16, ph)
                    rsd = sb("rsd", [128, 128], F32, ph)
                    op("dve", lambda h: h.memset(qbp[:], 0.0), writes=["qbp"])
                    ptc = 0
                    for g in range(NGD):
                        b = g % 2
                        t0 = g * 512
                        mx = mixT[b]
                        mxk = "mixb%d" % b
                        op("sp", lambda h, b=b, t0=t0: h.dma_start(out=xT[b][:], in_=xT_s[s, :, :, t0:t0 + 512]), writes=["xTb%d" % b], dma="ldxT%d" % b)
                        for cc in range(4):
                            pk = cc % 2
                            for c in range(8):
                                op("pe", lambda h, pk=pk, cc=cc, c=c, b=b: h.matmul(ps[pk][:], wq[:, c, cc * 128:(cc + 1) * 128], xT[b][:, c, :], start=(c == 0), stop=(c == 7)),
                                   reads=["wqb", "xTb%d" % b], writes=[psk[pk]], inc=(c == 7))
                            op("dve", lambda h, pk=pk, cc=cc: h.tensor_copy(out=qbp[0:64, 2 * cc, :], in_=ps[pk][0:64, :]), reads=[psk[pk]], writes=["qbp"])
                            op("act", lambda h, pk=pk, cc=cc: h.copy(out=qbp[64:128, 2 * cc + 1, :], in_=ps[pk][64:128, :]), reads=[psk[pk]], writes=["qbp"])
                        for r in range(4):
                            i = 4 * g + r
                            q0 = r * 128
                            for pp in range(2):
                                hA, hB = 2 * pp, 2 * pp + 1
                                jlo = max(0, i - max(WIN[hA], WIN[hB]))
                                pend = []

                                def emit_pv_b(item, i=i):
                                    j, heads, pslot = item
                                    pk_ = "pTb%d" % pslot
                                    last = (j == i)
                                    for hi_, hh in enumerate(heads):
                                        ab = hh % 2
                                        first = (j == max(0, i - WIN[hh]))
                                        for m in range(2):
                                            blk = (hh % 2) * 2 + m
                                            op("pe", lambda h: h.matmul(ps[ab][:, m * 256:m * 256 + 128], vb[:, j, hh * 128:(hh + 1) * 128], pT[pslot][:, blk, :], start=(first and m == 0), stop=last, skip_group_check=True),
                                               reads=["vb", pk_], writes=[psk[ab]], inc=False)
                                            op("pe", lambda h: h.matmul(ps[ab][:, m * 256 + 128:m * 256 + 256], ones[:], pT[pslot][:, blk, :], start=False, stop=last, skip_group_check=True),
                                               reads=["ones", pk_], writes=[psk[ab]], inc=(hi_ == len(heads) - 1 and m == 1))

                                for j in range(jlo, i + 1):
                                    heads = [hh for hh in (hA, hB) if j >= i - WIN[hh]]
                                    sbk = 2 + (ptc % 3)
                                    pslot = ptc % 3
                                    ptc += 1
                                    dg = (j == i)
                                    firstmm = True
                                    nmm = len(heads) * 2
                                    cnt_ = 0
                                    for hh in heads:
                                        for m in range(2):
                                            cs = ((hh % 2) * 2 + m) * 128
                                            cnt_ += 1
                                            lastmm = (cnt_ == nmm)
                                            op("pe", lambda h, sbk=sbk, cs=cs, hh=hh, m=m, j=j, dg=dg, fm=firstmm: h.matmul(ps[sbk][:, cs:cs + 128], ktb[:, hh, j * 128:(j + 1) * 128], qbp[:, 2 * hh + m, q0:q0 + 128], start=fm, stop=(not dg), skip_group_check=True),
                                               reads=["ktb", "qbp"], writes=[psk[sbk]], inc=((not dg) and lastmm))
                                            firstmm = False
                                            if dg:
                                                op("pe", lambda h, sbk=sbk, cs=cs, hh=hh: h.matmul(ps[sbk][:, cs:cs + 128], ident[:], diag[:, hh, :], start=False, stop=True, skip_group_check=True),
                                                   reads=["ident", "diag"], writes=[psk[sbk]], inc=lastmm)
                                    c_lo = (heads[0] % 2) * 256
                                    c_hi = (heads[-1] % 2) * 256 + 256
                                    if dg:
                                        op("act", lambda h, sbk=sbk, pslot=pslot, c_lo=c_lo, c_hi=c_hi: h.activation(out=pT[pslot][:].rearrange("p a b -> p (a b)")[:, c_lo:c_hi], in_=ps[sbk][:, c_lo:c_hi], func=AF.Exp, scale=0.125),
                                           reads=[psk[sbk]], writes=["pTb%d" % pslot])
                                    else:
                                        kk = i - j + 1
                                        for hh in heads:
                                            cl = (hh % 2) * 256
                                            op("act", lambda h, sbk=sbk, pslot=pslot, hh=hh, kk=kk, cl=cl: h.activation(out=pT[pslot][:].rearrange("p a b -> p (a b)")[:, cl:cl + 256], in_=ps[sbk][:, cl:cl + 256], func=AF.Exp, scale=0.125,
                                                                                                               bias=alibi[:, hh * 34 + kk:hh * 34 + kk + 1]),
                                               reads=[psk[sbk], "alibi"], writes=["pTb%d" % pslot])
                                    pend.append((j, heads, pslot))
                                    if len(pend) > 1:
                                        emit_pv_b(pend.pop(0))
                                while pend:
                                    emit_pv_b(pend.pop(0))
                                for hh in (hA, hB):
                                    pa = hh % 2
                                    A = ps[pa]
                                    op("dve", lambda h, A=A: h.reciprocal(out=r1[:, 0:128], in_=A[:, 128:256]), reads=[psk[pa]], writes=["r1"])
                                    op("dve", lambda h, A=A: h.reciprocal(out=r1[:, 128:256], in_=A[:, 384:512]), reads=[psk[pa]], writes=["r1"])
                                    op("dve", lambda h, A=A: h.tensor_tensor(out=oa[:], in0=A[:, 0:128], in1=r1[:, 0:128], op=ALU.mult), reads=[psk[pa], "r1"], writes=["oa"])
                                    op("dve", lambda h, A=A: h.tensor_tensor(out=ob[:], in0=A[:, 256:384], in1=r1[:, 128:256], op=ALU.mult), reads=[psk[pa], "r1"], writes=["ob"])
                                    op("dve", lambda h: h.scalar_tensor_tensor(out=oo[:], in0=ob[:], scalar=lsm[:, 5:6], in1=oa[:], op0=ALU.mult, op1=ALU.add), reads=["oa", "ob", "neglam"], writes=["oo"])
                                    op("pool", lambda h: h.tensor_tensor(out=osq[:], in0=oo[:], in1=oo[:], op=ALU.mult), reads=["oo"], writes=["osq"])
                                    op("pe", lambda h: h.matmul(ps[6][:, 0:128], ones[:], osq[:], start=True, stop=True), reads=["ones", "osq"], writes=[psk[6]], inc=True)
                                    op("act", lambda h: h.activation(out=rsd[:], in_=ps[6][:, 0:128], func=AF.Ln, scale=1.0 / 128, bias=EPS), reads=[psk[6]], writes=["rsd"])
                                    op("act", lambda h: h.activation(out=rsd[:], in_=rsd[:], func=AF.Exp, scale=-0.5), reads=["rsd"], writes=["rsd"])
                                    op("dve", lambda h: h.tensor_tensor(out=oo[:], in0=oo[:], in1=rsd[:], op=ALU.mult), reads=["oo", "rsd"], writes=["oo"])
                                    op("dve", lambda h, hh=hh, q0=q0: h.tensor_scalar(out=mx[:, hh, q0:q0 + 128], in0=oo[:], scalar1=gcol[:, 0:1], scalar2=(1.0 - lam_init), op0=ALU.mult, op1=ALU.mult),
                                       reads=["oo", "gcol"], writes=[mxk])
                        op("sp", lambda h, b=b, t0=t0: h.dma_start(out=mixT_s[s, :, 2:6, t0:t0 + 512], in_=mixT[b][:]), reads=[mxk], writes=["mixT_s"], dma="st_mx", nowaw=True)
                    P.barrier()
                    phase_ctr[0] += 1
                    if stop is not None and phase_ctr[0] >= stop:
                        P.stopped = True

            with ExitStack() as ph:
                wo = sb("wo", [128, 8, D], BF16, ph)
                wd = sb("wd", [128, NF, D], BF16, ph)
                for c in range(8):
                    op("pool", lambda h, c=c: h.dma_start(out=wo[:, c, :], in_=w_out[l, c * 128:(c + 1) * 128, :]), writes=["wo"], dma="wq", nowaw=True)
                for f in range(NF):
                    op("pool", lambda h, f=f: h.dma_start(out=wd[:, f, :], in_=w_dn[l, f * 128:(f + 1) * 128, :]), writes=["wd"], dma="wq", nowaw=True)
                lnp = sb("lnp", [128, 4, D], F32, ph)
                for k, src in enumerate((ln1g, ln1b, ln2g, ln2b)):
                    op("sp", lambda h, k=k, src=src: h.dma_start(out=lnp[:, k, :], in_=src[l].partition_broadcast(128)), writes=["lnp"], dma="gp", nowaw=True)
                wg = [sb("wg%d" % i, [128, 8, 256], BF16, ph) for i in range(3)]
                mxT = [sb("mxT%d" % i, [128, 8, 512], BF16, ph) for i in range(2)]
                xin = [sb("xr%d" % i, [128, D], F32, ph) for i in range(2)]
                x1 = sb("x1", [128, 4, D], F32, ph)
                x1b = sb("x1b", [128, D], BF16, ph)
                x1T = sb("x1T", [128, 8, 512], BF16, ph)
                actT = sb("actT", [128, NF, 512], BF16, ph)
                sg = [sb("sg%d" % i, [128, 512], F32, ph) for i in range(2)]
                zt = sb("zt", [128, D], F32, ph)
                yo = [sb("yo%d" % i, [128, D], F32, ph) for i in range(2)]
                bst = sb("bst", [128, 16], F32, ph)

                def layer_norm(zin, zkey, gk, bk, out_ap, out_key):
                    op("dve", lambda h: h.bn_stats(out=bst[:, 0:6], in_=zin[:, 0:512]), reads=[zkey], writes=["bst"])
                    op("dve", lambda h: h.bn_stats(out=bst[:, 6:12], in_=zin[:, 512:1024]), reads=[zkey], writes=["bst"])
                    op("dve", lambda h: h.bn_aggr(out=bst[:, 12:14], in_=bst[:, 0:12]), reads=["bst"], writes=["bst2"])
                    op("act", lambda h: h.activation(out=bst[:, 14:15], in_=bst[:, 13:14], func=AF.Ln, bias=EPS), reads=["bst2"], writes=["bst3"])
                    op("act", lambda h: h.activation(out=bst[:, 15:16], in_=bst[:, 14:15], func=AF.Exp, scale=-0.5), reads=["bst3"], writes=["bst4"])
                    op("dve", lambda h: h.tensor_scalar(out=zin[:], in0=zin[:], scalar1=bst[:, 12:13], scalar2=bst[:, 15:16], op0=ALU.subtract, op1=ALU.mult), reads=[zkey, "bst2", "bst4"], writes=[zkey])
                    op("pool", lambda h: h.tensor_tensor(out=zin[:], in0=zin[:], in1=lnp[:, gk, :], op=ALU.mult), reads=[zkey, "lnp"], writes=[zkey])
                    op("dve", lambda h: h.tensor_tensor(out=out_ap, in0=zin[:], in1=lnp[:, bk, :], op=ALU.add), reads=[zkey, "lnp"], writes=[out_key])

                gi = 0
                xc = 0
                wc = 0
                yc = 0
                for s in range(NSEQ):
                    for g in range(NGD):
                        b = gi % 2
                        gi += 1
                        t0 = g * 512
                        op("sp", lambda h, b=b, t0=t0, s=s: h.dma_start(out=mxT[b][:], in_=mixT_s[s, :, :, t0:t0 + 512]), writes=["mxT%d" % b], dma="ldmx%d" % b)
                        for r in range(4):
                            xb = xc % 2
                            xc += 1
                            tt = t0 + r * 128
                            op("sp", lambda h, xb=xb, tt=tt, s=s: h.dma_start(out=xin[xb][:], in_=x_src[s, tt:tt + 128, :]), writes=["xr%d" % xb], dma="ldxr%d" % xb)
                            for hf in range(2):
                                for c in range(8):
                                    op("pe", lambda h, hf=hf, c=c, b=b, r=r: h.matmul(ps[hf][:], mxT[b][:, c, r * 128:(r + 1) * 128], wo[:, c, hf * 512:(hf + 1) * 512], start=(c == 0), stop=(c == 7)),
                                       reads=["mxT%d" % b, "wo"], writes=[psk[hf]], inc=(c == 7))
                                op("dve", lambda h, hf=hf, xb=xb: h.scalar_tensor_tensor(out=zt[:, hf * 512:(hf + 1) * 512], in0=xin[xb][:, hf * 512:(hf + 1) * 512], scalar=ALPHA, in1=ps[hf][:], op0=ALU.mult, op1=ALU.add),
                                   reads=["xr%d" % xb, psk[hf]], writes=["zt"])
                            layer_norm(zt, "zt", 0, 1, x1[:, r, :], "x1_%d" % r)
                            op("act", lambda h, r=r: h.copy(out=x1b[:], in_=x1[:, r, :]), reads=["x1_%d" % r], writes=["x1b"])
                            for c in range(8):
                                op("pe", lambda h, c=c: h.transpose(pst[:, c * 128:(c + 1) * 128], x1b[:, c * 128:(c + 1) * 128], ident[:]), reads=["x1b", "ident"], writes=["pst"], inc=(c == 7))
                            op("act", lambda h, r=r: h.copy(out=x1T[:, :, r * 128:(r + 1) * 128], in_=pst[:].rearrange("p (c t) -> p c t", c=8)), reads=["pst"], writes=["x1T"])
                        for f in range(NF):
                            wb_ = wc % 3
                            wc += 1
                            op("sp", lambda h, wb_=wb_, f=f: h.dma_start(out=wg[wb_][:], in_=wgu_s[l, f]), writes=["wg%d" % wb_], dma="ldwg%d" % wb_)
                            pg, pu = 2 + 2 * (f % 2), 3 + 2 * (f % 2)
                            for c in range(8):
                                op("pe", lambda h, pg=pg, c=c, wb_=wb_: h.matmul(ps[pg][:], wg[wb_][:, c, 0:128], x1T[:, c, :], start=(c == 0), stop=(c == 7)),
                                   reads=["wg%d" % wb_, "x1T"], writes=[psk[pg]], inc=(c == 7))
                            for c in range(8):
                                op("pe", lambda h, pu=pu, c=c, wb_=wb_: h.matmul(ps[pu][:], wg[wb_][:, c, 128:256], x1T[:, c, :], start=(c == 0), stop=(c == 7)),
                                   reads=["wg%d" % wb_, "x1T"], writes=[psk[pu]], inc=(c == 7))
                            sb_ = f % 2
                            op("act", lambda h, pg=pg, sb_=sb_: h.activation(out=sg[sb_][:], in_=ps[pg][:], func=AF.Silu), reads=[psk[pg]], writes=["sg%d" % sb_])
                            op("dve", lambda h, pu=pu, sb_=sb_, f=f: h.tensor_tensor(out=actT[:, f, :], in0=sg[sb_][:], in1=ps[pu][:], op=ALU.mult), reads=["sg%d" % sb_, psk[pu]], writes=["actT"])
                        for r in range(4):
                            tt = t0 + r * 128
                            for hf in range(2):
                                for f in range(NF):
                                    op("pe", lambda h, hf=hf, f=f, r=r: h.matmul(ps[hf][:], actT[:, f, r * 128:(r + 1) * 128], wd[:, f, hf * 512:(hf + 1) * 512], start=(f == 0), stop=(f == NF - 1)),
                                       reads=["actT", "wd"], writes=[psk[hf]], inc=(f == NF - 1))
                                op("dve", lambda h, hf=hf, r=r: h.scalar_tensor_tensor(out=zt[:, hf * 512:(hf + 1) * 512], in0=x1[:, r, hf * 512:(hf + 1) * 512], scalar=ALPHA, in1=ps[hf][:], op0=ALU.mult, op1=ALU.add),
                                   reads=["x1_%d" % r, psk[hf]], writes=["zt"])
                            yb = yc % 2
                            yc += 1
                            layer_norm(zt, "zt", 2, 3, yo[yb][:], "yo%d" % yb)
                            op("sp", lambda h, yb=yb, tt=tt, s=s: h.dma_start(out=x_dst[s, tt:tt + 128, :], in_=yo[yb][:]), reads=["yo%d" % yb], writes=["xdst"], dma="st_y", nowaw=True)
                P.barrier()
                phase_ctr[0] += 1
                if stop is not None and phase_ctr[0] >= stop:
                    P.stopped = True
        except _Stop:
            pass
        P.final_wait("sp")
    return nc


def _consts():
    ident = np.eye(128, dtype=np.float32)
    so = np.arange(128)[:, None]
    to = np.arange(128)[None, :]
    diag = np.zeros((128, 4, 128), np.float32)
    for h in range(4):
        v = (-np.abs(to - so) + (to - 128)).astype(np.float32) * SLOPES[h] * 8.0
        v = np.where((so // 64) > (to // 64), NEG * 8.0, v)
        diag[:, h, :] = v
    alibi = np.zeros((128, 4 * 34), np.float32)
    for h in range(4):
        for k in range(34):
            alibi[:, h * 34 + k] = SLOPES[h] * (np.arange(128) - 128.0 * k)
    cmask = np.where((to // 64) > (so // 64), -1e30, 0.0).astype(np.float32)
    gmask = ((so // 64) <= (to // 64)).astype(np.float32)
    jpos = np.tile(np.arange(1, 33, dtype=np.float32)[None, :], (128, 1))
    return ident, diag, alibi, cmask, gmask, jpos


_CACHE = {}


def kernel(**inputs):
    n = 8
    f = lambda a: np.ascontiguousarray(np.asarray(a, dtype=np.float32))
    x = f(inputs["x"])
    ident, diag, alibi, cmask, gmask, jpos = _consts()
    lamv = np.stack([f(inputs["lam_q1"]), f(inputs["lam_k1"]), f(inputs["lam_q2"]), f(inputs["lam_k2"])], axis=1)
    shared = {
        "w_in": f(inputs["w_in"]),
        "gmlp_w_s": f(inputs["gmlp_w_s"]),
        "gmlp_b_s": f(inputs["gmlp_b_s"]),
        "gmlp_ln_g": f(inputs["gmlp_ln_g"]).reshape(DEPTH, 1, 256),
        "gmlp_ln_b": f(inputs["gmlp_ln_b"]).reshape(DEPTH, 1, 256),
        "lamv": np.ascontiguousarray(lamv),
        "diff_subln_g": f(inputs["diff_subln_g"]).reshape(DEPTH, 128, 1),
        "w_out": f(inputs["w_out"]),
        "ln1_g": f(inputs["ln1_g"]).reshape(DEPTH, 1, D),
        "ln1_b": f(inputs["ln1_b"]).reshape(DEPTH, 1, D),
        "w_gu": f(inputs["w_gu"]),
        "w_down": f(inputs["w_down"]),
        "ln2_g": f(inputs["ln2_g"]).reshape(DEPTH, 1, D),
        "ln2_b": f(inputs["ln2_b"]).reshape(DEPTH, 1, D),
        "c_ident": ident, "c_diag": diag, "c_alibi": alibi, "c_cmask": cmask, "c_gmask": gmask, "c_jpos": jpos,
    }
    if "nc" not in _CACHE:
        _CACHE["nc"] = build_program()
    nc = _CACHE["nc"]
    in_maps = []
    for c in range(n):
        m = dict(shared)
        m["x"] = np.ascontiguousarray(x[NSEQ * c:NSEQ * (c + 1)])
        in_maps.append(m)
    res = run_bass_kernel_spmd(nc, in_maps, core_ids=list(range(n)))
    out = np.concatenate([np.asarray(r["y"], dtype=np.float32) for r in res.results], axis=0)
    return out
```

```python
import math
from contextlib import ExitStack
import numpy as np
import concourse.bass as bass
import concourse.mybir as mybir
from concourse.bass_utils import run_bass_kernel_spmd

F32 = mybir.dt.float32
BF16 = mybir.dt.bfloat16
AF = mybir.ActivationFunctionType
ALU = mybir.AluOpType
AX = mybir.AxisListType

D = 1024
S = 4096
DEPTH = 2
NSEQ = 2
NT = S // 128
NG = S // 512
FF = 2816
NF = FF // 128
IN_W = 3112
OFF_A_U, OFF_A_V, OFF_B_Q, OFF_B_K, OFF_B_V = 0, 256, 512, 1024, 1536
OFF_C_Q, OFF_C_K, OFF_C_V, OFF_I_Q, OFF_I_K, OFF_I_W = 2048, 2304, 2560, 2816, 3072, 3104
ALPHA = (2 * DEPTH) ** 0.25
EPS = 1e-5
SLOPES = [2.0 ** (-8.0 * (h + 1) / 4) for h in range(4)]
WIN = [2, 9, 31, 31]
NBIS = 18
BIS_LO, BIS_W = -8.0, 16.0
ACCB = [0, 1, 5, 6]
TOPK = 256
NEG = -30000.0
IDX_SCALE = (8 ** -0.5) * (32 ** -0.5)


class Tok:
    __slots__ = ("sem", "val")

    def __init__(self, sem, val):
        self.sem = sem
        self.val = val


class DSem:
    def __init__(self, h):
        self.h = h
        self.total = 0


class Eng:
    def __init__(self, name, handle, sem):
        self.name = name
        self.h = handle
        self.sem = sem
        self.count = 0
        self.waited = {}


class Prog:
    def __init__(self, nc, es):
        self.nc = nc
        self.es = es
        self.eng = {}
        for name, h in (("pe", nc.tensor), ("act", nc.scalar), ("dve", nc.vector), ("pool", nc.gpsimd), ("sp", nc.sync)):
            self.eng[name] = Eng(name, h, es.enter_context(nc.semaphore("sem_" + name)))
        self.dsems = {}
        self.last_w = {}
        self.readers = {}
        self.stopped = False

    def dsem(self, name):
        if name not in self.dsems:
            self.dsems[name] = DSem(self.es.enter_context(self.nc.semaphore("dma_" + name)))
        return self.dsems[name]

    def _wait(self, e, tok):
        if isinstance(tok.sem, DSem):
            sem, val = tok.sem.h, tok.sem.total
            key = ("d", id(tok.sem))
        else:
            if tok.sem is e and e.name == "pe":
                return
            sem, val = tok.sem.sem, tok.val
            key = ("e", tok.sem.name)
        if e.waited.get(key, 0) >= val:
            return
        e.h.wait_ge(sem, val)
        e.waited[key] = val

    def op(self, eng, fn, reads=(), writes=(), inc=True, dma=None, nowaw=False):
        if self.stopped:
            return None
        e = self.eng[eng]
        deps = []
        for k in reads:
            t = self.last_w.get(k)
            if t is not None:
                deps.append(t)
        for k in writes:
            t = self.last_w.get(k)
            if t is not None and not nowaw:
                deps.append(t)
            deps.extend(self.readers.get(k, ()))
        for t in deps:
            self._wait(e, t)
        ins = fn(e.h)
        if dma is not None:
            ds = self.dsem(dma)
            ds.total += 16
            ins.then_inc(ds.h, 16)
            tok = Tok(ds, None)
        else:
            if inc:
                e.count += 1
                ins.then_inc(e.sem, 1)
                tok = Tok(e, e.count)
            else:
                tok = Tok(e, e.count + 1)
        for k in reads:
            self.readers.setdefault(k, []).append(tok)
            if len(self.readers[k]) > 24:
                self.readers[k] = self.readers[k][-24:] if False else self.readers[k]
        for k in writes:
            self.last_w[k] = tok
            self.readers[k] = []
        return ins

    def barrier(self):
        if self.stopped:
            return
        names = list(self.eng)
        for n in names:
            e = self.eng[n]
            for m in names:
                if m != n and self.eng[m].count > 0:
                    self._wait(e, Tok(self.eng[m], self.eng[m].count))
            for ds in self.dsems.values():
                if ds.total > 0:
                    self._wait(e, Tok(ds, None))
        self.last_w.clear()
        self.readers.clear()

    def final_wait(self, eng):
        e = self.eng[eng]
        for m in self.eng:
            if m != eng and self.eng[m].count > 0:
                self._wait(e, Tok(self.eng[m], self.eng[m].count))
        for ds in self.dsems.values():
            if ds.total > 0:
                self._wait(e, Tok(ds, None))


class _Stop(Exception):
    pass


def build_program(depth=DEPTH, debug=False, stop=None, dbg_groups=None):
    NGD = NG if dbg_groups is None else dbg_groups
    nc = bass.Bass("TRN2", target_bir_lowering=False)
    dt = nc.dram_tensor

    def din(name, shape):
        return dt(name, list(shape), F32, kind="ExternalInput").ap()

    x_in = din("x", [NSEQ, S, D])
    w_in = din("w_in", [DEPTH, D, IN_W])
    w_s = din("gmlp_w_s", [DEPTH, 4, 128, 128])
    b_s = din("gmlp_b_s", [DEPTH, 4, 128])
    a_g = din("gmlp_ln_g", [DEPTH, 1, 256])
    a_b = din("gmlp_ln_b", [DEPTH, 1, 256])
    lamv = din("lamv", [DEPTH, 4, 64])
    subg = din("diff_subln_g", [DEPTH, 128, 1])
    w_out = din("w_out", [DEPTH, D, D])
    ln1g = din("ln1_g", [DEPTH, 1, D])
    ln1b = din("ln1_b", [DEPTH, 1, D])
    w_gu = din("w_gu", [DEPTH, D, 2 * FF])
    w_dn = din("w_down", [DEPTH, FF, D])
    ln2g = din("ln2_g", [DEPTH, 1, D])
    ln2b = din("ln2_b", [DEPTH, 1, D])
    c_ident = din("c_ident", [128, 128])
    c_diag = din("c_diag", [128, 4, 128])
    c_alibi = din("c_alibi", [128, 4 * 34])
    c_cmask = din("c_cmask", [128, 128])
    c_gmask = din("c_gmask", [128, 128])
    c_jpos = din("c_jpos", [128, 32])
    y_out = dt("y", [NSEQ, S, D], F32, kind="ExternalOutput").ap()

    skind = "ExternalOutput" if debug else "Internal"
    xs = dt("xs", [NSEQ, S, D], F32, kind=skind).ap()
    xT_s = dt("xT_s", [NSEQ, 128, 8, S], BF16, kind=skind).ap()
    ktb_s = dt("ktb_s", [NSEQ, 128, 4, S], BF16, kind=skind).ap()
    ktc_s = dt("ktc_s", [NSEQ, 128, 2, S], BF16, kind=skind).ap()
    ki4_s = dt("ki4_s", [NSEQ, 128, S], BF16, kind=skind).ap()
    vb_s = dt("vb_s", [NSEQ, 128, NT, 512], BF16, kind=skind).ap()
    vc_s = dt("vc_s", [NSEQ, 128, NT, 256], BF16, kind=skind).ap()
    mixT_s = dt("mixT_s", [NSEQ, 128, 8, S], BF16, kind=skind).ap()
    wgu_s = dt("wgu_s", [DEPTH, NF, 128, 8, 256], BF16, kind="Internal").ap()

    with ExitStack() as es:
        E = es.enter_context
        P = Prog(nc, es)
        op = P.op
        dbgbuf = dt("dbgbuf", [128, 16384], F32, kind="ExternalOutput").ap() if debug else None
        dbgpos = {}

        def dump(name, ap2d, ncols, reads, cond=True):
            if not debug or not cond or name in dbgpos:
                return
            off = sum(v[1] for v in dbgpos.values())
            dbgpos[name] = (off, ncols)
            op("pool", lambda h: h.dma_start(out=dbgbuf[0:ap2d.shape[0], off:off + ncols], in_=ap2d), reads=reads, writes=["dbgbuf"], dma="dbg", nowaw=True)
        build_program.dbgpos = dbgpos

        uid = [0]

        def sb(name, shape, dtype, stack=es):
            uid[0] += 1
            return stack.enter_context(nc.sbuf_tensor("%s_%d" % (name, uid[0]), list(shape), dtype))

        ident = sb("ident", [128, 128], BF16)
        ones = sb("ones", [128, 128], BF16)
        diag = sb("diag", [128, 4, 128], BF16)
        alibi = sb("alibi", [128, 4 * 34], F32)
        cmask = sb("cmask", [128, 128], F32)
        gmask = sb("gmask", [128, 128], F32)
        op("pool", lambda h: h.dma_start(out=ident[:], in_=c_ident), writes=["ident"], dma="c0")
        op("pool", lambda h: h.dma_start(out=diag[:], in_=c_diag), writes=["diag"], dma="c0")
        op("sp", lambda h: h.dma_start(out=alibi[:], in_=c_alibi), writes=["alibi"], dma="c1")
        op("sp", lambda h: h.dma_start(out=cmask[:], in_=c_cmask), writes=["cmask"], dma="c1")
        op("sp", lambda h: h.dma_start(out=gmask[:], in_=c_gmask), writes=["gmask"], dma="c1")
        op("dve", lambda h: h.memset(ones[:], 1.0), writes=["ones"])
        jpos1 = sb("jpos1", [128, 32], F32)
        slmat = sb("slmat", [128, 4, 128], BF16)
        op("sp", lambda h: h.dma_start(out=jpos1[:], in_=c_jpos), writes=["jpos1"], dma="c1")
        for hh in range(4):
            op("dve", lambda h, hh=hh: h.memset(slmat[:, hh, :], 8.0 * SLOPES[hh]), writes=["slmat"])

        ps = [E(nc.psum_tensor("ps%d" % i, [128, 512], F32)) for i in range(7)]
        pst = E(nc.psum_tensor("pst", [128, 1024], BF16))
        psk = ["ps%d" % i for i in range(7)]

        for l in range(depth):
            for f in range(NF):
                for half in range(2):
                    src = w_gu[l, :, half * FF + f * 128: half * FF + (f + 1) * 128].rearrange("(c p) j -> p c j", p=128)
                    op("pool", lambda h, src=src, f=f, half=half, l=l: h.dma_start(
                        out=wgu_s[l, f, :, :, half * 128:(half + 1) * 128], in_=src),
                        writes=["wgu_s"], dma="wgucvt", nowaw=True)
        P.barrier()

        phase_ctr = [0]
        if stop == 0:
            depth_iter = []
        else:
            depth_iter = range(depth)
        try:
          for l in depth_iter:
            lam_init = 0.8 - 0.6 * math.exp(-0.3 * l)
            x_src = x_in if l == 0 else xs
            x_dst = y_out if l == depth - 1 else xs
            for s in range(NSEQ):
                with ExitStack() as ph:
                    wk = sb("wk", [128, 8, 1664], BF16, ph)
                    for c in range(8):
                        rows = w_in[l, c * 128:(c + 1) * 128, :]
                        op("pool", lambda h, c=c, rows=rows: h.dma_start(out=wk[:, c, 0:512], in_=rows[:, OFF_B_K:OFF_B_K + 512]), writes=["wk"], dma="wk", nowaw=True)
                        op("pool", lambda h, c=c, rows=rows: h.dma_start(out=wk[:, c, 512:768], in_=rows[:, OFF_C_K:OFF_C_K + 256]), writes=["wk"], dma="wk", nowaw=True)
                        for r in range(4):
                            op("pool", lambda h, c=c, rows=rows, r=r: h.dma_start(out=wk[:, c, 768 + 32 * r:800 + 32 * r], in_=rows[:, OFF_I_K:OFF_I_K + 32]), writes=["wk"], dma="wk", nowaw=True)
                        op("pool", lambda h, c=c, rows=rows: h.dma_start(out=wk[:, c, 896:1408], in_=rows[:, OFF_B_V:OFF_B_V + 512]), writes=["wk"], dma="wk", nowaw=True)
                        op("pool", lambda h, c=c, rows=rows: h.dma_start(out=wk[:, c, 1408:1664], in_=rows[:, OFF_C_V:OFF_C_V + 256]), writes=["wk"], dma="wk", nowaw=True)
                    xin = [sb("xin%d" % i, [128, 4, D], F32, ph) for i in range(2)]
                    xbf = [sb("xbf%d" % i, [128, 4, D], BF16, ph) for i in range(2)]
                    xT = [sb("xT%d" % i, [128, 8, 512], BF16, ph) for i in range(2)]
                    kst = [sb("kst%d" % i, [128, 7, 512], BF16, ph) for i in range(2)]
                    vst = [sb("vst%d" % i, [128, 4, 768], BF16, ph) for i in range(2)]
                    cnt = 0
                    for g in range(NGD):
                        b = g % 2
                        t0 = g * 512
                        op("sp", lambda h, b=b, t0=t0: h.dma_start(out=xin[b][:], in_=x_src[s, t0:t0 + 512, :].rearrange("(r p) d -> p r d", p=128)),
                           writes=["xin%d" % b], dma="xin%d" % b)
                        op("dve" if g % 2 == 0 else "pool", lambda h, b=b: h.tensor_copy(out=xbf[b][:], in_=xin[b][:]), reads=["xin%d" % b], writes=["xbf%d" % b])
                        for r in range(4):
                            for c in range(8):
                                op("pe", lambda h, b=b, r=r, c=c: h.transpose(pst[:, c * 128:(c + 1) * 128], xbf[b][:, r, c * 128:(c + 1) * 128], ident[:]),
                                   reads=["xbf%d" % b, "ident"], writes=["pst"], inc=(c == 7))
                            op("act" if r % 2 == 0 else "dve",
                               (lambda h, b=b, r=r: h.copy(out=xT[b][:, :, r * 128:(r + 1) * 128], in_=pst[:].rearrange("p (c t) -> p c t", c=8))) if r % 2 == 0 else
                               (lambda h, b=b, r=r: h.tensor_copy(out=xT[b][:, :, r * 128:(r + 1) * 128], in_=pst[:].rearrange("p (c t) -> p c t", c=8))),
                               reads=["pst"], writes=["xT%d" % b])
                        op("sp", lambda h, b=b, t0=t0: h.dma_start(out=xT_s[s, :, :, t0:t0 + 512], in_=xT[b][:]), reads=["xT%d" % b], writes=["xT_s"], dma="st_xT", nowaw=True)
                        for cc in range(7):
                            pk = cnt % 6
                            cnt += 1
                            for c in range(8):
                                op("pe", lambda h, pk=pk, cc=cc, c=c, b=b: h.matmul(ps[pk][:], wk[:, c, cc * 128:(cc + 1) * 128], xT[b][:, c, :], start=(c == 0), stop=(c == 7)),
                                   reads=["wk", "xT%d" % b], writes=[psk[pk]], inc=(c == 7))
                            if cc % 2 == 0:
                                op("act", lambda h, pk=pk, cc=cc, b=b: h.copy(out=kst[b][:, cc, :], in_=ps[pk][:]), reads=[psk[pk]], writes=["kst%d" % b])
                            else:
                                op("dve", lambda h, pk=pk, cc=cc, b=b: h.tensor_copy(out=kst[b][:, cc, :], in_=ps[pk][:]), reads=[psk[pk]], writes=["kst%d" % b])
                        op("sp", lambda h, b=b, t0=t0: h.dma_start(out=ktb_s[s, :, :, t0:t0 + 512], in_=kst[b][:, 0:4, :]), reads=["kst%d" % b], writes=["ktb_s"], dma="st_k", nowaw=True)
                        op("sp", lambda h, b=b, t0=t0: h.dma_start(out=ktc_s[s, :, :, t0:t0 + 512], in_=kst[b][:, 4:6, :]), reads=["kst%d" % b], writes=["ktc_s"], dma="st_k", nowaw=True)
                        op("sp", lambda h, b=b, t0=t0: h.dma_start(out=ki4_s[s, :, t0:t0 + 512], in_=kst[b][:, 6, :]), reads=["kst%d" % b], writes=["ki4_s"], dma="st_k", nowaw=True)
                        for r in range(4):
                            pk = cnt % 6
                            cnt += 1
                            for c in range(8):
                                op("pe", lambda h, pk=pk, r=r, c=c, b=b: h.matmul(ps[pk][:], xT[b][:, c, r * 128:(r + 1) * 128], wk[:, c, 896:1408], start=(c == 0), stop=(c == 7)),
                                   reads=["wk", "xT%d" % b], writes=[psk[pk]], inc=(c == 7))
                            op("act", lambda h, pk=pk, r=r, b=b: h.copy(out=vst[b][:, r, 0:512], in_=ps[pk][:]), reads=[psk[pk]], writes=["vst%d" % b])
                            pk = cnt % 6
                            cnt += 1
                            for c in range(8):
                                op("pe", lambda h, pk=pk, r=r, c=c, b=b: h.matmul(ps[pk][:, 0:256], xT[b][:, c, r * 128:(r + 1) * 128], wk[:, c, 1408:1664], start=(c == 0), stop=(c == 7)),
                                   reads=["wk", "xT%d" % b], writes=[psk[pk]], inc=(c == 7))
                            op("dve", lambda h, pk=pk, r=r, b=b: h.tensor_copy(out=vst[b][:, r, 512:768], in_=ps[pk][:, 0:256]), reads=[psk[pk]], writes=["vst%d" % b])
                        op("sp", lambda h, b=b, g=g: h.dma_start(out=vb_s[s, :, 4 * g:4 * g + 4, :], in_=vst[b][:, :, 0:512]), reads=["vst%d" % b], writes=["vb_s"], dma="st_v", nowaw=True)
                        op("sp", lambda h, b=b, g=g: h.dma_start(out=vc_s[s, :, 4 * g:4 * g + 4, :], in_=vst[b][:, :, 512:768]), reads=["vst%d" % b], writes=["vc_s"], dma="st_v", nowaw=True)
                    P.barrier()
                    phase_ctr[0] += 1
                    if stop is not None and phase_ctr[0] >= stop:
                        P.stopped = True

                with ExitStack() as ph:
                    ktc = sb("ktc", [128, 2, S], BF16, ph)
                    vc = sb("vc", [128, NT, 256], BF16, ph)
                    ki4 = sb("ki4", [128, S], BF16, ph)
                    op("sp", lambda h: h.dma_start(out=ktc[:], in_=ktc_s[s]), writes=["ktc"], dma="ldkv")
                    op("sp", lambda h: h.dma_start(out=vc[:], in_=vc_s[s]), writes=["vc"], dma="ldkv")
                    op("sp", lambda h: h.dma_start(out=ki4[:], in_=ki4_s[s]), writes=["ki4"], dma="ldkv")
                    wq = sb("wq", [128, 8, 768], BF16, ph)
                    wv = sb("wv", [128, 8, 264], BF16, ph)
                    for c in range(8):
                        rows = w_in[l, c * 128:(c + 1) * 128, :]
                        op("pool", lambda h, c=c, rows=rows: h.dma_start(out=wq[:, c, 0:256], in_=rows[:, OFF_A_U:OFF_A_U + 256]), writes=["wq"], dma="wq", nowaw=True)
                        op("pool", lambda h, c=c, rows=rows: h.dma_start(out=wq[:, c, 256:512], in_=rows[:, OFF_C_Q:OFF_C_Q + 256]), writes=["wq"], dma="wq", nowaw=True)
                        op("pool", lambda h, c=c, rows=rows: h.dma_start(out=wq[:, c, 512:768], in_=rows[:, OFF_I_Q:OFF_I_Q + 256]), writes=["wq"], dma="wq", nowaw=True)
                        op("pool", lambda h, c=c, rows=rows: h.dma_start(out=wv[:, c, 0:256], in_=rows[:, OFF_A_V:OFF_A_V + 256]), writes=["wv"], dma="wq", nowaw=True)
                        op("pool", lambda h, c=c, rows=rows: h.dma_start(out=wv[:, c, 256:264], in_=rows[:, OFF_I_W:OFF_I_W + 8]), writes=["wv"], dma="wq", nowaw=True)
                    wmT = sb("wmT", [128, 4, 128], BF16, ph)
                    wmf = sb("wmf", [128, 4, 128], F32, ph)
                    bsT = sb("bsT", [128, 2, 128], F32, ph)
                    lng = sb("lng", [128, 256], F32, ph)
                    lnb = sb("lnb", [128, 256], F32, ph)
                    wmb = sb("wmb", [128, 4, 128], BF16, ph)
                    op("sp", lambda h: h.dma_start(out=wmf[:], in_=w_s[l].rearrange("g t s -> t g s")), writes=["wmf"], dma="gpw")
                    op("dve", lambda h: h.tensor_copy(out=wmb[:], in_=wmf[:]), reads=["wmf"], writes=["wmb"])
                    for gg in range(4):
                        op("pe", lambda h, gg=gg: h.transpose(pst[:, gg * 128:(gg + 1) * 128], wmb[:, gg, :], ident[:]), reads=["wmb", "ident"], writes=["pst"], inc=(gg == 3))
                    for gg in range(4):
                        op("sp", lambda h, gg=gg: h.dma_start(out=bsT[(gg % 2) * 64:(gg % 2) * 64 + 64, gg // 2, :], in_=b_s[l, gg:gg + 1, :].partition_broadcast(64)),
                           writes=["bsT"], dma="gp", nowaw=True)
                    op("sp", lambda h: h.dma_start(out=lng[:], in_=a_g[l].partition_broadcast(128)), writes=["lng"], dma="gp")
                    op("sp", lambda h: h.dma_start(out=lnb[:], in_=a_b[l].partition_broadcast(128)), writes=["lnb"], dma="gp")
                    for gg in range(4):
                        op("dve", lambda h, gg=gg: h.tensor_tensor(out=wmT[:, gg, :], in0=pst[:, gg * 128:(gg + 1) * 128], in1=gmask[:], op=ALU.mult), reads=["pst", "gmask"], writes=["wmT"])

                    xT = [sb("xTa%d" % i, [128, 8, 512], BF16, ph) for i in range(2)]
                    uT = sb("uT", [128, 2, 512], BF16, ph)
                    qcp = sb("qcp", [128, 4, 512], BF16, ph)
                    qip = sb("qip", [128, 8, 512], BF16, ph)
                    vtm = sb("vtm", [128, 264], F32, ph)
                    vg = sb("vg", [128, 256], F32, ph)
                    vsq = sb("vsq", [128, 256], F32, ph)
                    st4 = sb("st4", [128, 16], F32, ph)
                    vn = sb("vn", [128, 256], BF16, ph)
                    wab = sb("wab", [128, 8], F32, ph)
                    sgn = sb("sgn", [128, 8], F32, ph)
                    dsg = sb("dsg", [128, 8, 128], BF16, ph)
                    score = sb("score", [128, S], F32, ph)
                    mb = sb("mb", [128, S], BF16, ph)
                    mbT = sb("mbT", [128, NT, 128], BF16, ph)
                    rl = [sb("rl%d" % i, [128, 512], BF16, ph) for i in range(4)]
                    pT = [sb("pTa%d" % i, [128, 4, 128], BF16, ph) for i in range(3)]
                    bis = sb("bis", [128, 8], F32, ph)
                    mixT = [sb("mixa%d" % i, [128, 4, 512], BF16, ph) for i in range(2)]
                    tmpa = sb("tmpa", [128, 128], F32, ph)
                    rcp = sb("rcp", [128, 128], F32, ph)
                    qcp2 = sb("qcp2", [128, 4, 512], BF16, ph)
                    op("dve", lambda h: h.memset(qcp[:], 0.0), writes=["qcp0"])
                    op("dve", lambda h: h.memset(qcp2[:], 0.0), writes=["qcp1"])
                    op("pool", lambda h: h.memset(qip[:], 0.0), writes=["qip"])
                    op("pool", lambda h: h.memset(mbT[:], 0.0), writes=["mbT"])
                    dblk = sb("dblk", [128, 128], BF16, ph)
                    dT = sb("dT", [128, 128], BF16, ph)
                    anys = sb("anys", [128, 64], F32, ph)
                    op("dve", lambda h: h.memset(dblk[:], 0.0), writes=["dblk"])
                    op("dve", lambda h: h.memset(dT[:], 0.0), writes=["dT"])

                    rlc = 0
                    ptc = 0
                    qcpb = [qcp, qcp2]

                    def prologue(g):
                        nonlocal rlc, ptc
                        b = g % 2
                        t0 = g * 512
                        mx = mixT[b]
                        mxk = "mixa%d" % b
                        op("sp", lambda h, b=b, t0=t0: h.dma_start(out=xT[b][:], in_=xT_s[s, :, :, t0:t0 + 512]), writes=["xTa%d" % b], dma="ldxT%d" % b)
                        for cc in range(6):
                            pk = cc % 2
                            for c in range(8):
                                op("pe", lambda h, pk=pk, cc=cc, c=c, b=b: h.matmul(ps[pk][:], wq[:, c, cc * 128:(cc + 1) * 128], xT[b][:, c, :], start=(c == 0), stop=(c == 7)),
                                   reads=["wq", "xTa%d" % b], writes=[psk[pk]], inc=(c == 7))
                            if cc < 2:
                                op("act", lambda h, pk=pk, cc=cc: h.activation(out=uT[:, cc, :], in_=ps[pk][:], func=AF.Gelu_apprx_tanh), reads=[psk[pk]], writes=["uT"])
                            elif cc < 4:
                                for k in range(2):
                                    hh = (cc - 2) * 2 + k
                                    op("dve", lambda h, pk=pk, hh=hh, k=k: h.tensor_copy(out=qcpb[g % 2][64 * k:64 * k + 64, hh, :], in_=ps[pk][64 * k:64 * k + 64, :]), reads=[psk[pk]], writes=["qcp%d" % (g % 2)])
                            else:
                                for k in range(4):
                                    hh = (cc - 4) * 4 + k
                                    op("dve" if k % 2 == 0 else "act",
                                       (lambda h, pk=pk, hh=hh, k=k: h.tensor_copy(out=qip[32 * k:32 * k + 32, hh, :], in_=ps[pk][32 * k:32 * k + 32, :])) if k % 2 == 0 else
                                       (lambda h, pk=pk, hh=hh, k=k: h.copy(out=qip[32 * k:32 * k + 32, hh, :], in_=ps[pk][32 * k:32 * k + 32, :])),
                                       reads=[psk[pk]], writes=["qip"])

                    def stageA(g, r):
                        nonlocal rlc, ptc
                        if True:
                            b = g % 2
                            t0 = g * 512
                            mx = mixT[b]
                            mxk = "mixa%d" % b
                            i = 4 * g + r
                            q0 = r * 128
                            nk = 128 * (i + 1)
                            for c in range(8):
                                op("pe", lambda h, c=c, b=b, q0=q0: h.matmul(ps[4][:, 0:264], xT[b][:, c, q0:q0 + 128], wv[:, c, :], start=(c == 0), stop=(c == 7)),
                                   reads=["wv", "xTa%d" % b], writes=[psk[4]], inc=(c == 7))
                            op("act", lambda h: h.activation(out=vg[:], in_=ps[4][:, 0:256], func=AF.Gelu_apprx_tanh), reads=[psk[4]], writes=["vg"])
                            op("dve", lambda h: h.tensor_copy(out=vtm[:, 256:264], in_=ps[4][:, 256:264]), reads=[psk[4]], writes=["vtm"])
                            op("dve", lambda h: h.tensor_scalar(out=wab[:], in0=vtm[:, 256:264], scalar1=-1.0, scalar2=None, op0=ALU.mult), reads=["vtm"], writes=["wab"])
                            op("dve", lambda h: h.tensor_tensor(out=wab[:], in0=wab[:], in1=vtm[:, 256:264], op=ALU.max), reads=["vtm", "wab"], writes=["wab"])
                            op("dve", lambda h: h.tensor_scalar(out=wab[:], in0=wab[:], scalar1=IDX_SCALE, scalar2=None, op0=ALU.mult), reads=["wab"], writes=["wab"])
                            op("dve", lambda h: h.tensor_scalar(out=sgn[:], in0=vtm[:, 256:264], scalar1=0.0, scalar2=2.0, op0=ALU.is_ge, op1=ALU.mult), reads=["vtm"], writes=["sgn"])
                            op("dve", lambda h: h.tensor_scalar(out=sgn[:], in0=sgn[:], scalar1=-1.0, scalar2=None, op0=ALU.add), reads=["sgn"], writes=["sgn"])
                            for hh in range(8):
                                op("dve", lambda h, hh=hh: h.tensor_scalar(out=dsg[:, hh, :], in0=ident[:], scalar1=sgn[:, hh:hh + 1], scalar2=None, op0=ALU.mult),
                                   reads=["ident", "sgn"], writes=["dsg%d" % hh])
                            v3 = vg[:].rearrange("p (g c) -> p g c", c=64)
                            op("dve", lambda h: h.tensor_reduce(out=st4[:, 0:4], in_=v3, axis=AX.X, op=ALU.add), reads=["vg"], writes=["st4"])
                            op("dve", lambda h: h.tensor_tensor(out=vsq[:], in0=vg[:], in1=vg[:], op=ALU.mult), reads=["vg"], writes=["vsq"])
                            op("dve", lambda h: h.tensor_reduce(out=st4[:, 4:8], in_=vsq[:].rearrange("p (g c) -> p g c", c=64), axis=AX.X, op=ALU.add), reads=["vsq"], writes=["st4"])
                            op("dve", lambda h: h.tensor_scalar(out=st4[:, 0:4], in0=st4[:, 0:4], scalar1=1.0 / 64, scalar2=None, op0=ALU.mult), reads=["st4"], writes=["st4"])
                            op("dve", lambda h: h.tensor_tensor(out=st4[:, 8:12], in0=st4[:, 0:4], in1=st4[:, 0:4], op=ALU.mult), reads=["st4"], writes=["st4"])
                            op("dve", lambda h: h.scalar_tensor_tensor(out=st4[:, 4:8], in0=st4[:, 4:8], scalar=1.0 / 64, in1=st4[:, 8:12], op0=ALU.mult, op1=ALU.subtract), reads=["st4"], writes=["st4"])
                            op("act", lambda h: h.activation(out=st4[:, 8:12], in_=st4[:, 4:8], func=AF.Ln, bias=EPS), reads=["st4"], writes=["st4"])
                            op("act", lambda h: h.activation(out=st4[:, 12:16], in_=st4[:, 8:12], func=AF.Exp, scale=-0.5), reads=["st4"], writes=["st4"])
                            for gg in range(4):
                                op("dve", lambda h, gg=gg: h.tensor_scalar(out=vsq[:, gg * 64:(gg + 1) * 64], in0=vg[:, gg * 64:(gg + 1) * 64], scalar1=st4[:, gg:gg + 1], scalar2=st4[:, 12 + gg:13 + gg],
                                                                    op0=ALU.subtract, op1=ALU.mult), reads=["vg", "st4"], writes=["vsq"])
                            op("dve", lambda h: h.tensor_tensor(out=vsq[:], in0=vsq[:], in1=lng[:], op=ALU.mult), reads=["vsq", "lng"], writes=["vsq"])
                            op("dve", lambda h: h.tensor_tensor(out=vn[:], in0=vsq[:], in1=lnb[:], op=ALU.add), reads=["vsq", "lnb"], writes=["vn"])
                            for gg in range(4):
                                ck = gg // 2
                                op("pe", lambda h, gg=gg, ck=ck: h.matmul(ps[5][:, (gg % 2) * 128:(gg % 2) * 128 + 128], vn[:, ck * 128:(ck + 1) * 128], wmT[:, gg, :], start=True, stop=True),
                                   reads=["vn", "wmT"], writes=[psk[5]], inc=True)
                                rs = slice((gg % 2) * 64, (gg % 2) * 64 + 64)
                                op("dve", lambda h, gg=gg, ck=ck, rs=rs: h.tensor_tensor(out=tmpa[rs, :], in0=ps[5][rs, (gg % 2) * 128:(gg % 2) * 128 + 128], in1=bsT[rs, ck, :], op=ALU.add),
                                   reads=[psk[5], "bsT"], writes=["tmpa"])
                                op("dve", lambda h, gg=gg, ck=ck, rs=rs, q0=q0: h.tensor_tensor(out=mx[rs, ck, q0:q0 + 128], in0=tmpa[rs, :], in1=uT[rs, ck, q0:q0 + 128], op=ALU.mult),
                                   reads=["tmpa", "uT"], writes=[mxk])
                            nk = 128 * (i + 1)
                            if i >= 2:
                                for c0 in range(0, nk, 512):
                                    cw = min(512, nk - c0)
                                    def emit_L(hh, c0=c0, cw=cw, q0=q0):
                                        pk = 4 + hh % 2
                                        op("pe", lambda h: h.matmul(ps[pk][:, 0:cw], qip[:, hh, q0:q0 + 128], ki4[:, c0:c0 + cw], start=True, stop=True),
                                           reads=["qip", "ki4"], writes=[psk[pk]], inc=True)
                                    emit_L(0)
                                    for hh in range(8):
                                        pk = 4 + hh % 2
                                        if hh + 1 < 8:
                                            emit_L(hh + 1)
                                        rb = rlc % 4
                                        rlc += 1
                                        op("act", lambda h, pk=pk, hh=hh, cw=cw, rb=rb: h.activation(out=rl[rb][:, 0:cw], in_=ps[pk][:, 0:cw], func=AF.Relu, scale=wab[:, hh:hh + 1]),
                                           reads=[psk[pk], "wab"], writes=["rl%d" % rb])
                                        op("pe", lambda h, hh=hh, cw=cw, rb=rb: h.matmul(ps[6][:, 0:cw], dsg[:, hh, :], rl[rb][:, 0:cw], start=(hh == 0), stop=(hh == 7)),
                                           reads=["dsg%d" % hh, "rl%d" % rb], writes=[psk[6]], inc=(hh == 7))
                                    if c0 + cw == nk:
                                        if cw > 128:
                                            op("dve", lambda h, c0=c0, cw=cw: h.tensor_copy(out=score[:, c0:c0 + cw - 128], in_=ps[6][:, 0:cw - 128]), reads=[psk[6]], writes=["score"])
                                        op("dve", lambda h, c0=c0, cw=cw: h.tensor_tensor(out=score[:, c0 + cw - 128:c0 + cw], in0=ps[6][:, cw - 128:cw], in1=cmask[:], op=ALU.add),
                                           reads=[psk[6], "cmask"], writes=["score"])
                                    else:
                                        op("dve", lambda h, c0=c0, cw=cw: h.tensor_copy(out=score[:, c0:c0 + cw], in_=ps[6][:, 0:cw]), reads=[psk[6]], writes=["score"])
                                op("dve", lambda h: h.memset(bis[:, 0:1], BIS_LO), writes=["bis"])
                                for it in range(NBIS):
                                    cst = BIS_W / (2.0 ** (it + 1))
                                    op("dve", lambda h, cst=cst: h.tensor_scalar(out=bis[:, 1:2], in0=bis[:, 0:1], scalar1=cst, scalar2=None, op0=ALU.add), reads=["bis"], writes=["bis"])
                                    op("dve", lambda h, nk=nk: h.tensor_scalar(out=mb[:, 0:nk], in0=score[:, 0:nk], scalar1=bis[:, 1:2], scalar2=None, op0=ALU.is_ge, op1=ALU.add, accum_out=bis[:, 2:3]),
                                       reads=["score", "bis"], writes=["mb", "bis"])
                                    op("dve", lambda h, cst=cst: h.tensor_scalar(out=bis[:, 3:4], in0=bis[:, 2:3], scalar1=float(TOPK), scalar2=cst, op0=ALU.is_ge, op1=ALU.mult), reads=["bis"], writes=["bis"])
                                    op("dve", lambda h: h.tensor_tensor(out=bis[:, 0:1], in0=bis[:, 0:1], in1=bis[:, 3:4], op=ALU.add), reads=["bis"], writes=["bis"])
                                op("dve", lambda h, nk=nk: h.tensor_scalar(out=mb[:, 0:nk], in0=score[:, 0:nk], scalar1=bis[:, 0:1], scalar2=NEG, op0=ALU.is_lt, op1=ALU.mult), reads=["score", "bis"], writes=["mb"])
                                nb = i + 1
                                op("dve", lambda h, nk=nk, nb=nb: h.tensor_reduce(out=anys[:, 0:nb], in_=mb[:, 0:nk].rearrange("p (j k) -> p j k", k=128), axis=AX.X, op=ALU.max), reads=["mb"], writes=["anys"])
                                op("dve", lambda h, nb=nb: h.tensor_scalar(out=anys[:, 0:nb], in0=anys[:, 0:nb], scalar1=-1.0, scalar2=None, op0=ALU.is_ge), reads=["anys"], writes=["anys"])
                                op("dve", lambda h, nb=nb: h.tensor_tensor(out=anys[:, 0:nb], in0=anys[:, 0:nb], in1=jpos1[:, 0:nb], op=ALU.mult), reads=["anys", "jpos1"], writes=["anys"])
                                op("dve", lambda h, nb=nb: h.tensor_reduce(out=anys[:, 32:33], in_=anys[:, 0:nb], axis=AX.X, op=ALU.max), reads=["anys"], writes=["anys"])
                                op("dve", lambda h, nb=nb: h.tensor_scalar(out=dblk[:, 0:1], in0=anys[:, 32:33], scalar1=-128.0, scalar2=128.0 * nb, op0=ALU.mult, op1=ALU.add), reads=["anys"], writes=["dblk"])

                    def stageB1(g, r):
                        nonlocal rlc, ptc
                        if True:
                            b = g % 2
                            t0 = g * 512
                            mx = mixT[b]
                            mxk = "mixa%d" % b
                            i = 4 * g + r
                            q0 = r * 128
                            nk = 128 * (i + 1)
                            if i >= 2:
                                for j0 in range(0, i + 1, 8):
                                    nj = min(8, i + 1 - j0)
                                    for jj in range(nj):
                                        op("pe", lambda h, j0=j0, jj=jj: h.transpose(pst[:, jj * 128:(jj + 1) * 128], mb[:, (j0 + jj) * 128:(j0 + jj + 1) * 128], ident[:]),
                                           reads=["mb", "ident"], writes=["pst"], inc=(jj == nj - 1))
                                    op("act" if (j0 // 8) % 2 == 0 else "dve",
                                       (lambda h, j0=j0, nj=nj: h.copy(out=mbT[:, j0:j0 + nj, :], in_=pst[:, 0:nj * 128].rearrange("p (j t) -> p j t", t=128))) if (j0 // 8) % 2 == 0 else
                                       (lambda h, j0=j0, nj=nj: h.tensor_copy(out=mbT[:, j0:j0 + nj, :], in_=pst[:, 0:nj * 128].rearrange("p (j t) -> p j t", t=128))),
                                       reads=["pst"], writes=["mbT"])
                                op("pe", lambda h: h.transpose(pst[:, 0:128], dblk[:], ident[:]), reads=["dblk", "ident"], writes=["pst"], inc=True)
                                op("dve", lambda h: h.tensor_copy(out=dT[:], in_=pst[:, 0:128]), reads=["pst"], writes=["dT"])

                    def stageB2(g, r):
                        nonlocal rlc, ptc
                        if True:
                            b = g % 2
                            t0 = g * 512
                            mx = mixT[b]
                            mxk = "mixa%d" % b
                            i = 4 * g + r
                            q0 = r * 128
                            nk = 128 * (i + 1)
                            if debug and l == 0 and s == 0 and i == 2:
                                dump("vn", vn[:], 256, ["vn"])
                                dump("wmT", wmT[:].rearrange("p a b -> p (a b)"), 512, ["wmT"])
                                dump("st4", st4[:], 16, ["st4"])
                                dump("tmpa", tmpa[:], 128, ["tmpa"])
                                dump("vg", vg[:], 256, ["vg"])
                                dump("uT", uT[:].rearrange("p a b -> p (a b)")[:, 0:512], 512, ["uT"])
                                dump("score", score[:, 0:384], 384, ["score"])
                                dump("bis", bis[:], 8, ["bis"])
                                dump("mb", mb[:, 0:384], 384, ["mb"])
                                dump("mbT", mbT[:].rearrange("p a b -> p (a b)")[:, 0:384], 384, ["mbT"])
                                dump("anys", anys[:], 64, ["anys"])
                                dump("dT", dT[:], 128, ["dT"])
                                dump("wab", wab[:], 8, ["wab"])
                                dump("sgn", sgn[:], 8, ["sgn"])
                                dump("bsT", bsT[:].rearrange("p a b -> p (a b)"), 256, ["bsT"])
                            pend = []

                            def emit_pv_c(item, i=i):
                                j, pslot = item
                                first, last = (j == 0), (j == i)
                                pk_ = "pTa%d" % pslot
                                for hh in range(4):
                                    ab = hh // 2
                                    co = (hh % 2) * 256
                                    op("pe", lambda h: h.matmul(ps[ab][:, co:co + 128], vc[:, j, ab * 128:(ab + 1) * 128], pT[pslot][:, hh, :], start=(first and hh % 2 == 0), stop=last, skip_group_check=True),
                                       reads=["vc", pk_], writes=[psk[ab]], inc=False)
                                    op("pe", lambda h: h.matmul(ps[ab][:, co + 128:co + 256], ones[:], pT[pslot][:, hh, :], start=False, stop=last, skip_group_check=True),
                                       reads=["ones", pk_], writes=[psk[ab]], inc=(hh == 3))

                            for j in range(0, i + 1):
                                sbk = 2 + (ptc % 4)
                                pslot = ptc % 3
                                ptc += 1
                                dg = (j == i)
                                for hh in range(4):
                                    ck = hh // 2
                                    cs = hh * 128
                                    op("pe", lambda h, sbk=sbk, cs=cs, hh=hh, j=j, ck=ck: h.matmul(ps[sbk][:, cs:cs + 128], ktc[:, ck, j * 128:(j + 1) * 128], qcpb[g % 2][:, hh, q0:q0 + 128], start=(hh == 0), stop=False, skip_group_check=True),
                                       reads=["ktc", "qcp%d" % (g % 2)], writes=[psk[sbk]], inc=False)
                                    op("pe", lambda h, sbk=sbk, cs=cs, j=j: h.matmul(ps[sbk][:, cs:cs + 128], ident[:], mbT[:, j, :], start=False, stop=False, skip_group_check=True),
                                       reads=["ident", "mbT"], writes=[psk[sbk]], inc=False)
                                    op("pe", lambda h, sbk=sbk, cs=cs, hh=hh, dg=dg: h.matmul(ps[sbk][:, cs:cs + 128], slmat[:, hh, :], dT[:], start=False, stop=(not dg), skip_group_check=True),
                                       reads=["slmat", "dT"], writes=[psk[sbk]], inc=((not dg) and hh == 3))
                                    if dg:
                                        op("pe", lambda h, sbk=sbk, cs=cs, hh=hh: h.matmul(ps[sbk][:, cs:cs + 128], ident[:], diag[:, hh, :], start=False, stop=True, skip_group_check=True),
                                           reads=["ident", "diag"], writes=[psk[sbk]], inc=(hh == 3))
                                if dg:
                                    op("act", lambda h, sbk=sbk, pslot=pslot: h.activation(out=pT[pslot][:].rearrange("p a b -> p (a b)"), in_=ps[sbk][:], func=AF.Exp, scale=0.125),
                                       reads=[psk[sbk]], writes=["pTa%d" % pslot])
                                else:
                                    kk = i - j + 1
                                    for hh in range(4):
                                        op("act", lambda h, sbk=sbk, pslot=pslot, hh=hh, kk=kk: h.activation(out=pT[pslot][:, hh, :], in_=ps[sbk][:, hh * 128:(hh + 1) * 128], func=AF.Exp, scale=0.125,
                                                                                                    bias=alibi[:, hh * 34 + kk:hh * 34 + kk + 1]),
                                           reads=[psk[sbk], "alibi"], writes=["pTa%d" % pslot])
                                pend.append((j, pslot))
                                if len(pend) > 1:
                                    emit_pv_c(pend.pop(0))
                            while pend:
                                emit_pv_c(pend.pop(0))
                            for hh in range(4):
                                ab = hh // 2
                                co = (hh % 2) * 256
                                rs = slice(64 * (hh % 2), 64 * (hh % 2) + 64)
                                op("dve", lambda h, ab=ab, co=co, rs=rs: h.reciprocal(out=rcp[rs, :], in_=ps[ab][rs, co + 128:co + 256]), reads=[psk[ab]], writes=["rcp"])
                                op("dve", lambda h, ab=ab, co=co, rs=rs, q0=q0: h.tensor_tensor(out=mx[rs, 2 + ab, q0:q0 + 128], in0=ps[ab][rs, co:co + 128], in1=rcp[rs, :], op=ALU.mult),
                                   reads=[psk[ab], "rcp"], writes=[mxk])
                            if r == 3:
                                op("sp", lambda h, b=b, t0=t0: h.dma_start(out=mixT_s[s, :, 0:2, t0:t0 + 512], in_=mixT[b][:, 0:2, :]), reads=[mxk], writes=["mixT_s"], dma="st_mx", nowaw=True)
                                op("sp", lambda h, b=b, t0=t0: h.dma_start(out=mixT_s[s, :, 6:8, t0:t0 + 512], in_=mixT[b][:, 2:4, :]), reads=[mxk], writes=["mixT_s"], dma="st_mx", nowaw=True)

                    tiles = [(g_, r_) for g_ in range(NGD) for r_ in range(4)]
                    prologue(0)
                    stageA(0, 0)
                    for ti, (g_, r_) in enumerate(tiles):
                        stageB1(g_, r_)
                        if ti + 1 < len(tiles):
                            gn, rn = tiles[ti + 1]
                            if rn == 0:
                                prologue(gn)
                            stageA(gn, rn)
                        stageB2(g_, r_)
                    P.barrier()
                    phase_ctr[0] += 1
                    if stop is not None and phase_ctr[0] >= stop:
                        P.stopped = True

                with ExitStack() as ph:
                    ktb = sb("ktb", [128, 4, S], BF16, ph)
                    vb = sb("vb", [128, NT, 512], BF16, ph)
                    op("sp", lambda h: h.dma_start(out=ktb[:], in_=ktb_s[s]), writes=["ktb"], dma="ldkv")
                    op("sp", lambda h: h.dma_start(out=vb[:], in_=vb_s[s]), writes=["vb"], dma="ldkv")
                    wq = sb("wqb", [128, 8, 512], BF16, ph)
                    for c in range(8):
                        op("pool", lambda h, c=c: h.dma_start(out=wq[:, c, :], in_=w_in[l, c * 128:(c + 1) * 128, OFF_B_Q:OFF_B_Q + 512]), writes=["wqb"], dma="wq", nowaw=True)
                    lv = sb("lv", [128, 4, 64], F32, ph)
                    lsm = sb("lsm", [128, 8], F32, ph)
                    gcol = sb("gcol", [128, 1], F32, ph)
                    op("sp", lambda h: h.dma_start(out=lv[:].rearrange("p a b -> p (a b)"), in_=lamv[l:l + 1].rearrange("o a b -> o (a b)").partition_broadcast(128)), writes=["lv"], dma="gp")
                    op("sp", lambda h: h.dma_start(out=gcol[:], in_=subg[l]), writes=["gcol"], dma="gp")
                    op("dve", lambda h: h.tensor_tensor(out=lv[:, 0, :], in0=lv[:, 0, :], in1=lv[:, 1, :], op=ALU.mult), reads=["lv"], writes=["lv"])
                    op("dve", lambda h: h.tensor_tensor(out=lv[:, 2, :], in0=lv[:, 2, :], in1=lv[:, 3, :], op=ALU.mult), reads=["lv"], writes=["lv"])
                    op("dve", lambda h: h.tensor_reduce(out=lsm[:, 0:1], in_=lv[:, 0, :], axis=AX.X, op=ALU.add), reads=["lv"], writes=["lsm"])
                    op("dve", lambda h: h.tensor_reduce(out=lsm[:, 1:2], in_=lv[:, 2, :], axis=AX.X, op=ALU.add), reads=["lv"], writes=["lsm"])
                    op("act", lambda h: h.activation(out=lsm[:, 2:4], in_=lsm[:, 0:2], func=AF.Exp), reads=["lsm"], writes=["lsm2"])
                    op("dve", lambda h: h.tensor_tensor(out=lsm[:, 4:5], in0=lsm[:, 3:4], in1=lsm[:, 2:3], op=ALU.subtract), reads=["lsm2"], writes=["lsm3"])
                    op("dve", lambda h: h.tensor_scalar(out=lsm[:, 5:6], in0=lsm[:, 4:5], scalar1=-lam_init, scalar2=None, op0=ALU.add), reads=["lsm3"], writes=["neglam"])
                    xT = [sb("xTb%d" % i, [128, 8, 512], BF16, ph) for i in range(2)]
                    qbp = sb("qbp", [128, 8, 512], BF16, ph)
                    pT = [sb("pTb%d" % i, [128, 4, 128], BF16, ph) for i in range(3)]
                    mixT = [sb("mixb%d" % i, [128, 4, 512], BF16, ph) for i in range(2)]
                    r1 = sb("r1", [128, 256], F32, ph)
                    oa = sb("oa", [128, 128], F32, ph)
                    ob = sb("ob", [128, 128], F32, ph)
                    oo = sb("oo", [128, 128], F32, ph)
                    osq = sb("osq", [128, 128], BF16, ph)
                    rsd = sb("rsd", [128, 128], F32, ph)
                    op("dve", lambda h: h.memset(qbp[:], 0.0), writes=["qbp"])
                    ptc = 0
                    for g in range(NGD):
                        b = g % 2
                        t0 = g * 512
                        mx = mixT[b]
                        mxk = "mixb%d" % b
                        op("sp", lambda h, b=b, t0=t0: h.dma_start(out=xT[b][:], in_=xT_s[s, :, :, t0:t0 + 512]), writes=["xTb%d" % b], dma="ldxT%d" % b)
                        for cc in range(4):
                            pk = cc % 2
                            for c in range(8):
                                op("pe", lambda h, pk=pk, cc=cc, c=c, b=b: h.matmul(ps[pk][:], wq[:, c, cc * 128:(cc + 1) * 128], xT[b][:, c, :], start=(c == 0), stop=(c == 7)),
                                   reads=["wqb", "xTb%d" % b], writes=[psk[pk]], inc=(c == 7))
                            op("dve", lambda h, pk=pk, cc=cc: h.tensor_copy(out=qbp[0:64, 2 * cc, :], in_=ps[pk][0:64, :]), reads=[psk[pk]], writes=["qbp"])
                            op("act", lambda h, pk=pk, cc=cc: h.copy(out=qbp[64:128, 2 * cc + 1, :], in_=ps[pk][64:128, :]), reads=[psk[pk]], writes=["qbp"])
                        for r in range(4):
                            i = 4 * g + r
                            q0 = r * 128
                            for pp in range(2):
                                hA, hB = 2 * pp, 2 * pp + 1
                                jlo = max(0, i - max(WIN[hA], WIN[hB]))
                                pend = []

                                def emit_pv_b(item, i=i):
                                    j, heads, pslot = item
                                    pk_ = "pTb%d" % pslot
                                    last = (j == i)
                                    for hi_, hh in enumerate(heads):
                                        ab = ACCB[hh]
                                        first = (j == max(0, i - WIN[hh]))
                                        for m in range(2):
                                            blk = (hh % 2) * 2 + m
                                            op("pe", lambda h: h.matmul(ps[ab][:, m * 256:m * 256 + 128], vb[:, j, hh * 128:(hh + 1) * 128], pT[pslot][:, blk, :], start=(first and m == 0), stop=last, skip_group_check=True),
                                               reads=["vb", pk_], writes=[psk[ab]], inc=False)
                                            op("pe", lambda h: h.matmul(ps[ab][:, m * 256 + 128:m * 256 + 256], ones[:], pT[pslot][:, blk, :], start=False, stop=last, skip_group_check=True),
                                               reads=["ones", pk_], writes=[psk[ab]], inc=(hi_ == len(heads) - 1 and m == 1))

                                for j in range(jlo, i + 1):
                                    heads = [hh for hh in (hA, hB) if j >= i - WIN[hh]]
                                    sbk = 2 + (ptc % 3)
                                    pslot = ptc % 3
                                    ptc += 1
                                    dg = (j == i)
                                    firstmm = True
                                    nmm = len(heads) * 2
                                    cnt_ = 0
                                    for hh in heads:
                                        for m in range(2):
                                            cs = ((hh % 2) * 2 + m) * 128
                                            cnt_ += 1
                                            lastmm = (cnt_ == nmm)
                                            op("pe", lambda h, sbk=sbk, cs=cs, hh=hh, m=m, j=j, dg=dg, fm=firstmm: h.matmul(ps[sbk][:, cs:cs + 128], ktb[:, hh, j * 128:(j + 1) * 128], qbp[:, 2 * hh + m, q0:q0 + 128], start=fm, stop=(not dg), skip_group_check=True),
                                               reads=["ktb", "qbp"], writes=[psk[sbk]], inc=((not dg) and lastmm))
                                            firstmm = False
                                            if dg:
                                                op("pe", lambda h, sbk=sbk, cs=cs, hh=hh: h.matmul(ps[sbk][:, cs:cs + 128], ident[:], diag[:, hh, :], start=False, stop=True, skip_group_check=True),
                                                   reads=["ident", "diag"], writes=[psk[sbk]], inc=lastmm)
                                    c_lo = (heads[0] % 2) * 256
                                    c_hi = (heads[-1] % 2) * 256 + 256
                                    if dg:
                                        op("act", lambda h, sbk=sbk, pslot=pslot, c_lo=c_lo, c_hi=c_hi: h.activation(out=pT[pslot][:].rearrange("p a b -> p (a b)")[:, c_lo:c_hi], in_=ps[sbk][:, c_lo:c_hi], func=AF.Exp, scale=0.125),
                                           reads=[psk[sbk]], writes=["pTb%d" % pslot])
                                    else:
                                        kk = i - j + 1
                                        for hh in heads:
                                            cl = (hh % 2) * 256
                                            op("act", lambda h, sbk=sbk, pslot=pslot, hh=hh, kk=kk, cl=cl: h.activation(out=pT[pslot][:].rearrange("p a b -> p (a b)")[:, cl:cl + 256], in_=ps[sbk][:, cl:cl + 256], func=AF.Exp, scale=0.125,
                                                                                                               bias=alibi[:, hh * 34 + kk:hh * 34 + kk + 1]),
                                               reads=[psk[sbk], "alibi"], writes=["pTb%d" % pslot])
                                    pend.append((j, heads, pslot))
                                    if len(pend) > 1:
                                        emit_pv_b(pend.pop(0))
                                while pend:
                                    emit_pv_b(pend.pop(0))
                                for hh in (hA, hB):
                                    pa = ACCB[hh]
                                    A = ps[pa]
                                    op("dve", lambda h, A=A: h.reciprocal(out=r1[:, 0:128], in_=A[:, 128:256]), reads=[psk[pa]], writes=["r1"])
                                    op("dve", lambda h, A=A: h.reciprocal(out=r1[:, 128:256], in_=A[:, 384:512]), reads=[psk[pa]], writes=["r1"])
                                    op("dve", lambda h, A=A: h.tensor_tensor(out=oa[:], in0=A[:, 0:128], in1=r1[:, 0:128], op=ALU.mult), reads=[psk[pa], "r1"], writes=["oa"])
                                    op("dve", lambda h, A=A: h.tensor_tensor(out=ob[:], in0=A[:, 256:384], in1=r1[:, 128:256], op=ALU.mult), reads=[psk[pa], "r1"], writes=["ob"])
                                    op("dve", lambda h: h.scalar_tensor_tensor(out=oo[:], in0=ob[:], scalar=lsm[:, 5:6], in1=oa[:], op0=ALU.mult, op1=ALU.add), reads=["oa", "ob", "neglam"], writes=["oo"])
                                    op("pool", lambda h: h.tensor_tensor(out=osq[:], in0=oo[:], in1=oo[:], op=ALU.mult), reads=["oo"], writes=["osq"])
                                    op("pe", lambda h, pa=pa: h.matmul(ps[pa][:, 0:128], ones[:], osq[:], start=True, stop=True), reads=["ones", "osq"], writes=[psk[pa]], inc=True)
                                    op("act", lambda h, pa=pa: h.activation(out=rsd[:], in_=ps[pa][:, 0:128], func=AF.Ln, scale=1.0 / 128, bias=EPS), reads=[psk[pa]], writes=["rsd"])
                                    op("act", lambda h: h.activation(out=rsd[:], in_=rsd[:], func=AF.Exp, scale=-0.5), reads=["rsd"], writes=["rsd"])
                                    op("dve", lambda h: h.tensor_tensor(out=oo[:], in0=oo[:], in1=rsd[:], op=ALU.mult), reads=["oo", "rsd"], writes=["oo"])
                                    op("dve", lambda h, hh=hh, q0=q0: h.tensor_scalar(out=mx[:, hh, q0:q0 + 128], in0=oo[:], scalar1=gcol[:, 0:1], scalar2=(1.0 - lam_init), op0=ALU.mult, op1=ALU.mult),
                                       reads=["oo", "gcol"], writes=[mxk])
                        op("sp", lambda h, b=b, t0=t0: h.dma_start(out=mixT_s[s, :, 2:6, t0:t0 + 512], in_=mixT[b][:]), reads=[mxk], writes=["mixT_s"], dma="st_mx", nowaw=True)
                    P.barrier()
                    phase_ctr[0] += 1
                    if stop is not None and phase_ctr[0] >= stop:
                        P.stopped = True

            with ExitStack() as ph:
                wo = sb("wo", [128, 8, D], BF16, ph)
                wd = sb("wd", [128, NF, D], BF16, ph)
                for c in range(8):
                    op("pool", lambda h, c=c: h.dma_start(out=wo[:, c, :], in_=w_out[l, c * 128:(c + 1) * 128, :]), writes=["wo"], dma="wq", nowaw=True)
                for f in range(NF):
                    op("pool", lambda h, f=f: h.dma_start(out=wd[:, f, :], in_=w_dn[l, f * 128:(f + 1) * 128, :]), writes=["wd"], dma="wq", nowaw=True)
                lnp = sb("lnp", [128, 4, D], F32, ph)
                for k, src in enumerate((ln1g, ln1b, ln2g, ln2b)):
                    op("sp", lambda h, k=k, src=src: h.dma_start(out=lnp[:, k, :], in_=src[l].partition_broadcast(128)), writes=["lnp"], dma="gp", nowaw=True)
                wg = [sb("wg%d" % i, [128, 8, 256], BF16, ph) for i in range(3)]
                mxT = [sb("mxT%d" % i, [128, 8, 512], BF16, ph) for i in range(2)]
                xin = [sb("xr%d" % i, [128, D], F32, ph) for i in range(2)]
                x1 = sb("x1", [128, 4, D], F32, ph)
                x1b = sb("x1b", [128, D], BF16, ph)
                x1T = sb("x1T", [128, 8, 512], BF16, ph)
                actT = sb("actT", [128, NF, 512], BF16, ph)
                sg = [sb("sg%d" % i, [128, 512], F32, ph) for i in range(2)]
                zt = sb("zt", [128, D], F32, ph)
                yo = [sb("yo%d" % i, [128, D], F32, ph) for i in range(2)]
                bst = sb("bst", [128, 16], F32, ph)

                def layer_norm(zin, zkey, gk, bk, out_ap, out_key):
                    op("dve", lambda h: h.bn_stats(out=bst[:, 0:6], in_=zin[:, 0:512]), reads=[zkey], writes=["bst"])
                    op("dve", lambda h: h.bn_stats(out=bst[:, 6:12], in_=zin[:, 512:1024]), reads=[zkey], writes=["bst"])
                    op("dve", lambda h: h.bn_aggr(out=bst[:, 12:14], in_=bst[:, 0:12]), reads=["bst"], writes=["bst2"])
                    op("act", lambda h: h.activation(out=bst[:, 14:15], in_=bst[:, 13:14], func=AF.Ln, bias=EPS), reads=["bst2"], writes=["bst3"])
                    op("act", lambda h: h.activation(out=bst[:, 15:16], in_=bst[:, 14:15], func=AF.Exp, scale=-0.5), reads=["bst3"], writes=["bst4"])
                    op("dve", lambda h: h.tensor_scalar(out=zin[:], in0=zin[:], scalar1=bst[:, 12:13], scalar2=bst[:, 15:16], op0=ALU.subtract, op1=ALU.mult), reads=[zkey, "bst2", "bst4"], writes=[zkey])
                    op("pool", lambda h: h.tensor_tensor(out=zin[:], in0=zin[:], in1=lnp[:, gk, :], op=ALU.mult), reads=[zkey, "lnp"], writes=[zkey])
                    op("dve", lambda h: h.tensor_tensor(out=out_ap, in0=zin[:], in1=lnp[:, bk, :], op=ALU.add), reads=[zkey, "lnp"], writes=[out_key])

                gi = 0
                xc = 0
                wc = 0
                yc = 0
                for s in range(NSEQ):
                    for g in range(NGD):
                        b = gi % 2
                        gi += 1
                        t0 = g * 512
                        op("sp", lambda h, b=b, t0=t0, s=s: h.dma_start(out=mxT[b][:], in_=mixT_s[s, :, :, t0:t0 + 512]), writes=["mxT%d" % b], dma="ldmx%d" % b)
                        for r in range(4):
                            xb = xc % 2
                            xc += 1
                            tt = t0 + r * 128
                            op("sp", lambda h, xb=xb, tt=tt, s=s: h.dma_start(out=xin[xb][:], in_=x_src[s, tt:tt + 128, :]), writes=["xr%d" % xb], dma="ldxr%d" % xb)
                            for hf in range(2):
                                for c in range(8):
                                    op("pe", lambda h, hf=hf, c=c, b=b, r=r: h.matmul(ps[hf][:], mxT[b][:, c, r * 128:(r + 1) * 128], wo[:, c, hf * 512:(hf + 1) * 512], start=(c == 0), stop=(c == 7)),
                                       reads=["mxT%d" % b, "wo"], writes=[psk[hf]], inc=(c == 7))
                                op("dve", lambda h, hf=hf, xb=xb: h.scalar_tensor_tensor(out=zt[:, hf * 512:(hf + 1) * 512], in0=xin[xb][:, hf * 512:(hf + 1) * 512], scalar=ALPHA, in1=ps[hf][:], op0=ALU.mult, op1=ALU.add),
                                   reads=["xr%d" % xb, psk[hf]], writes=["zt"])
                            layer_norm(zt, "zt", 0, 1, x1[:, r, :], "x1_%d" % r)
                            op("act", lambda h, r=r: h.copy(out=x1b[:], in_=x1[:, r, :]), reads=["x1_%d" % r], writes=["x1b"])
                            for c in range(8):
                                op("pe", lambda h, c=c: h.transpose(pst[:, c * 128:(c + 1) * 128], x1b[:, c * 128:(c + 1) * 128], ident[:]), reads=["x1b", "ident"], writes=["pst"], inc=(c == 7))
                            op("act", lambda h, r=r: h.copy(out=x1T[:, :, r * 128:(r + 1) * 128], in_=pst[:].rearrange("p (c t) -> p c t", c=8)), reads=["pst"], writes=["x1T"])
                        for f in range(NF):
                            wb_ = wc % 3
                            wc += 1
                            op("sp", lambda h, wb_=wb_, f=f: h.dma_start(out=wg[wb_][:], in_=wgu_s[l, f]), writes=["wg%d" % wb_], dma="ldwg%d" % wb_)
                            pg, pu = 2 + 2 * (f % 2), 3 + 2 * (f % 2)
                            for c in range(8):
                                op("pe", lambda h, pg=pg, c=c, wb_=wb_: h.matmul(ps[pg][:], wg[wb_][:, c, 0:128], x1T[:, c, :], start=(c == 0), stop=(c == 7)),
                                   reads=["wg%d" % wb_, "x1T"], writes=[psk[pg]], inc=(c == 7))
                            for c in range(8):
                                op("pe", lambda h, pu=pu, c=c, wb_=wb_: h.matmul(ps[pu][:], wg[wb_][:, c, 128:256], x1T[:, c, :], start=(c == 0), stop=(c == 7)),
                                   reads=["wg%d" % wb_, "x1T"], writes=[psk[pu]], inc=(c == 7))
                            sb_ = f % 2
                            op("act", lambda h, pg=pg, sb_=sb_: h.activation(out=sg[sb_][:], in_=ps[pg][:], func=AF.Silu), reads=[psk[pg]], writes=["sg%d" % sb_])
                            op("dve", lambda h, pu=pu, sb_=sb_, f=f: h.tensor_tensor(out=actT[:, f, :], in0=sg[sb_][:], in1=ps[pu][:], op=ALU.mult), reads=["sg%d" % sb_, psk[pu]], writes=["actT"])
                        for r in range(4):
                            tt = t0 + r * 128
                            for hf in range(2):
                                for f in range(NF):
                                    op("pe", lambda h, hf=hf, f=f, r=r: h.matmul(ps[hf][:], actT[:, f, r * 128:(r + 1) * 128], wd[:, f, hf * 512:(hf + 1) * 512], start=(f == 0), stop=(f == NF - 1)),
                                       reads=["actT", "wd"], writes=[psk[hf]], inc=(f == NF - 1))
                                op("dve", lambda h, hf=hf, r=r: h.scalar_tensor_tensor(out=zt[:, hf * 512:(hf + 1) * 512], in0=x1[:, r, hf * 512:(hf + 1) * 512], scalar=ALPHA, in1=ps[hf][:], op0=ALU.mult, op1=ALU.add),
                                   reads=["x1_%d" % r, psk[hf]], writes=["zt"])
                            yb = yc % 2
                            yc += 1
                            layer_norm(zt, "zt", 2, 3, yo[yb][:], "yo%d" % yb)
                            op("sp", lambda h, yb=yb, tt=tt, s=s: h.dma_start(out=x_dst[s, tt:tt + 128, :], in_=yo[yb][:]), reads=["yo%d" % yb], writes=["xdst"], dma="st_y", nowaw=True)
                P.barrier()
                phase_ctr[0] += 1
                if stop is not None and phase_ctr[0] >= stop:
                    P.stopped = True
        except _Stop:
            pass
        P.final_wait("sp")
    return nc


def _consts():
    ident = np.eye(128, dtype=np.float32)
    so = np.arange(128)[:, None]
    to = np.arange(128)[None, :]
    diag = np.zeros((128, 4, 128), np.float32)
    for h in range(4):
        v = (-np.abs(to - so) + (to - 128)).astype(np.float32) * SLOPES[h] * 8.0
        v = np.where((so // 64) > (to // 64), NEG * 8.0, v)
        diag[:, h, :] = v
    alibi = np.zeros((128, 4 * 34), np.float32)
    for h in range(4):
        for k in range(34):
            alibi[:, h * 34 + k] = SLOPES[h] * (np.arange(128) - 128.0 * k)
    cmask = np.where((to // 64) > (so // 64), -1e30, 0.0).astype(np.float32)
    gmask = ((so // 64) <= (to // 64)).astype(np.float32)
    jpos = np.tile(np.arange(1, 33, dtype=np.float32)[None, :], (128, 1))
    return ident, diag, alibi, cmask, gmask, jpos


_CACHE = {}


def kernel(**inputs):
    n = 8
    f = lambda a: np.ascontiguousarray(np.asarray(a, dtype=np.float32))
    x = f(inputs["x"])
    ident, diag, alibi, cmask, gmask, jpos = _consts()
    lamv = np.stack([f(inputs["lam_q1"]), f(inputs["lam_k1"]), f(inputs["lam_q2"]), f(inputs["lam_k2"])], axis=1)
    shared = {
        "w_in": f(inputs["w_in"]),
        "gmlp_w_s": f(inputs["gmlp_w_s"]),
        "gmlp_b_s": f(inputs["gmlp_b_s"]),
        "gmlp_ln_g": f(inputs["gmlp_ln_g"]).reshape(DEPTH, 1, 256),
        "gmlp_ln_b": f(inputs["gmlp_ln_b"]).reshape(DEPTH, 1, 256),
        "lamv": np.ascontiguousarray(lamv),
        "diff_subln_g": f(inputs["diff_subln_g"]).reshape(DEPTH, 128, 1),
        "w_out": f(inputs["w_out"]),
        "ln1_g": f(inputs["ln1_g"]).reshape(DEPTH, 1, D),
        "ln1_b": f(inputs["ln1_b"]).reshape(DEPTH, 1, D),
        "w_gu": f(inputs["w_gu"]),
        "w_down": f(inputs["w_down"]),
        "ln2_g": f(inputs["ln2_g"]).reshape(DEPTH, 1, D),
        "ln2_b": f(inputs["ln2_b"]).reshape(DEPTH, 1, D),
        "c_ident": ident, "c_diag": diag, "c_alibi": alibi, "c_cmask": cmask, "c_gmask": gmask, "c_jpos": jpos,
    }
    if "nc" not in _CACHE:
        _CACHE["nc"] = build_program()
    nc = _CACHE["nc"]
    in_maps = []
    for c in range(n):
        m = dict(shared)
        m["x"] = np.ascontiguousarray(x[NSEQ * c:NSEQ * (c + 1)])
        in_maps.append(m)
    res = run_bass_kernel_spmd(nc, in_maps, core_ids=list(range(n)))
    out = np.concatenate([np.asarray(r["y"], dtype=np.float32) for r in res.results], axis=0)
    return out
```

```python
import math
from contextlib import ExitStack
import numpy as np
import concourse.bass as bass
import concourse.mybir as mybir
from concourse.bass_utils import run_bass_kernel_spmd

F32 = mybir.dt.float32
BF16 = mybir.dt.bfloat16
AF = mybir.ActivationFunctionType
ALU = mybir.AluOpType
AX = mybir.AxisListType

D = 1024
S = 4096
DEPTH = 2
NSEQ = 2
NT = S // 128
NG = S // 512
FF = 2816
NF = FF // 128
IN_W = 3112
OFF_A_U, OFF_A_V, OFF_B_Q, OFF_B_K, OFF_B_V = 0, 256, 512, 1024, 1536
OFF_C_Q, OFF_C_K, OFF_C_V, OFF_I_Q, OFF_I_K, OFF_I_W = 2048, 2304, 2560, 2816, 3072, 3104
ALPHA = (2 * DEPTH) ** 0.25
EPS = 1e-5
SLOPES = [2.0 ** (-8.0 * (h + 1) / 4) for h in range(4)]
WIN = [2, 9, 31, 31]
NBIS = 18
BIS_LO, BIS_W = -8.0, 16.0
ACCB = [0, 1, 5, 6]
TOPK = 256
NEG = -30000.0
IDX_SCALE = (8 ** -0.5) * (32 ** -0.5)


class Tok:
    __slots__ = ("sem", "val")

    def __init__(self, sem, val):
        self.sem = sem
        self.val = val


class DSem:
    def __init__(self, h):
        self.h = h
        self.total = 0


class Eng:
    def __init__(self, name, handle, sem):
        self.name = name
        self.h = handle
        self.sem = sem
        self.count = 0
        self.waited = {}


class Prog:
    def __init__(self, nc, es):
        self.nc = nc
        self.es = es
        self.eng = {}
        for name, h in (("pe", nc.tensor), ("act", nc.scalar), ("dve", nc.vector), ("pool", nc.gpsimd), ("sp", nc.sync)):
            self.eng[name] = Eng(name, h, es.enter_context(nc.semaphore("sem_" + name)))
        self.dsems = {}
        self.last_w = {}
        self.readers = {}
        self.stopped = False

    def dsem(self, name):
        if name not in self.dsems:
            self.dsems[name] = DSem(self.es.enter_context(self.nc.semaphore("dma_" + name)))
        return self.dsems[name]

    def _wait(self, e, tok):
        if isinstance(tok.sem, DSem):
            sem, val = tok.sem.h, tok.sem.total
            key = ("d", id(tok.sem))
        else:
            if tok.sem is e and e.name == "pe":
                return
            sem, val = tok.sem.sem, tok.val
            key = ("e", tok.sem.name)
        if e.waited.get(key, 0) >= val:
            return
        e.h.wait_ge(sem, val)
        e.waited[key] = val

    def op(self, eng, fn, reads=(), writes=(), inc=True, dma=None, nowaw=False):
        if self.stopped:
            return None
        e = self.eng[eng]
        deps = []
        for k in reads:
            t = self.last_w.get(k)
            if t is not None:
                deps.append(t)
        for k in writes:
            t = self.last_w.get(k)
            if t is not None and not nowaw:
                deps.append(t)
            deps.extend(self.readers.get(k, ()))
        for t in deps:
            self._wait(e, t)
        ins = fn(e.h)
        if dma is not None:
            ds = self.dsem(dma)
            ds.total += 16
            ins.then_inc(ds.h, 16)
            tok = Tok(ds, None)
        else:
            if inc:
                e.count += 1
                ins.then_inc(e.sem, 1)
                tok = Tok(e, e.count)
            else:
                tok = Tok(e, e.count + 1)
        for k in reads:
            self.readers.setdefault(k, []).append(tok)
            if len(self.readers[k]) > 24:
                self.readers[k] = self.readers[k][-24:] if False else self.readers[k]
        for k in writes:
            self.last_w[k] = tok
            self.readers[k] = []
        return ins

    def barrier(self):
        if self.stopped:
            return
        names = list(self.eng)
        for n in names:
            e = self.eng[n]
            for m in names:
                if m != n and self.eng[m].count > 0:
                    self._wait(e, Tok(self.eng[m], self.eng[m].count))
            for ds in self.dsems.values():
                if ds.total > 0:
                    self._wait(e, Tok(ds, None))
        self.last_w.clear()
        self.readers.clear()

    def final_wait(self, eng):
        e = self.eng[eng]
        for m in self.eng:
            if m != eng and self.eng[m].count > 0:
                self._wait(e, Tok(self.eng[m], self.eng[m].count))
        for ds in self.dsems.values():
            if ds.total > 0:
                self._wait(e, Tok(ds, None))


class _Stop(Exception):
    pass


def build_program(depth=DEPTH, debug=False, stop=None, dbg_groups=None):
    NGD = NG if dbg_groups is None else dbg_groups
    nc = bass.Bass("TRN2", target_bir_lowering=False)
    dt = nc.dram_tensor

    def din(name, shape):
        return dt(name, list(shape), F32, kind="ExternalInput").ap()

    x_in = din("x", [NSEQ, S, D])
    w_in = din("w_in", [DEPTH, D, IN_W])
    w_s = din("gmlp_w_s", [DEPTH, 4, 128, 128])
    b_s = din("gmlp_b_s", [DEPTH, 4, 128])
    a_g = din("gmlp_ln_g", [DEPTH, 1, 256])
    a_b = din("gmlp_ln_b", [DEPTH, 1, 256])
    lamv = din("lamv", [DEPTH, 4, 64])
    subg = din("diff_subln_g", [DEPTH, 128, 1])
    w_out = din("w_out", [DEPTH, D, D])
    ln1g = din("ln1_g", [DEPTH, 1, D])
    ln1b = din("ln1_b", [DEPTH, 1, D])
    w_gu = din("w_gu", [DEPTH, D, 2 * FF])
    w_dn = din("w_down", [DEPTH, FF, D])
    ln2g = din("ln2_g", [DEPTH, 1, D])
    ln2b = din("ln2_b", [DEPTH, 1, D])
    c_ident = din("c_ident", [128, 128])
    c_diag = din("c_diag", [128, 4, 128])
    c_alibi = din("c_alibi", [128, 4 * 34])
    c_cmask = din("c_cmask", [128, 128])
    c_gmask = din("c_gmask", [128, 128])
    c_jpos = din("c_jpos", [128, 32])
    y_out = dt("y", [NSEQ, S, D], F32, kind="ExternalOutput").ap()

    skind = "ExternalOutput" if debug else "Internal"
    xs = dt("xs", [NSEQ, S, D], F32, kind=skind).ap()
    xT_s = dt("xT_s", [NSEQ, 128, 8, S], BF16, kind=skind).ap()
    ktb_s = dt("ktb_s", [NSEQ, 128, 4, S], BF16, kind=skind).ap()
    ktc_s = dt("ktc_s", [NSEQ, 128, 2, S], BF16, kind=skind).ap()
    ki4_s = dt("ki4_s", [NSEQ, 128, S], BF16, kind=skind).ap()
    vb_s = dt("vb_s", [NSEQ, 128, NT, 512], BF16, kind=skind).ap()
    vc_s = dt("vc_s", [NSEQ, 128, NT, 256], BF16, kind=skind).ap()
    mixT_s = dt("mixT_s", [NSEQ, 128, 8, S], BF16, kind=skind).ap()
    wgu_s = dt("wgu_s", [DEPTH, NF, 128, 8, 256], BF16, kind="Internal").ap()

    with ExitStack() as es:
        E = es.enter_context
        P = Prog(nc, es)
        op = P.op
        dbgbuf = dt("dbgbuf", [128, 16384], F32, kind="ExternalOutput").ap() if debug else None
        dbgpos = {}

        def dump(name, ap2d, ncols, reads, cond=True):
            if not debug or not cond or name in dbgpos:
                return
            off = sum(v[1] for v in dbgpos.values())
            dbgpos[name] = (off, ncols)
            op("pool", lambda h: h.dma_start(out=dbgbuf[0:ap2d.shape[0], off:off + ncols], in_=ap2d), reads=reads, writes=["dbgbuf"], dma="dbg", nowaw=True)
        build_program.dbgpos = dbgpos

        uid = [0]

        def sb(name, shape, dtype, stack=es):
            uid[0] += 1
            return stack.enter_context(nc.sbuf_tensor("%s_%d" % (name, uid[0]), list(shape), dtype))

        ident = sb("ident", [128, 128], BF16)
        ones = sb("ones", [128, 128], BF16)
        diag = sb("diag", [128, 4, 128], BF16)
        alibi = sb("alibi", [128, 4 * 34], F32)
        cmask = sb("cmask", [128, 128], F32)
        gmask = sb("gmask", [128, 128], F32)
        op("pool", lambda h: h.dma_start(out=ident[:], in_=c_ident), writes=["ident"], dma="c0")
        op("pool", lambda h: h.dma_start(out=diag[:], in_=c_diag), writes=["diag"], dma="c0")
        op("sp", lambda h: h.dma_start(out=alibi[:], in_=c_alibi), writes=["alibi"], dma="c1")
        op("sp", lambda h: h.dma_start(out=cmask[:], in_=c_cmask), writes=["cmask"], dma="c1")
        op("sp", lambda h: h.dma_start(out=gmask[:], in_=c_gmask), writes=["gmask"], dma="c1")
        op("dve", lambda h: h.memset(ones[:], 1.0), writes=["ones"])
        jpos1 = sb("jpos1", [128, 32], F32)
        slmat = sb("slmat", [128, 4, 128], BF16)
        op("sp", lambda h: h.dma_start(out=jpos1[:], in_=c_jpos), writes=["jpos1"], dma="c1")
        for hh in range(4):
            op("dve", lambda h, hh=hh: h.memset(slmat[:, hh, :], 8.0 * SLOPES[hh]), writes=["slmat"])

        ps = [E(nc.psum_tensor("ps%d" % i, [128, 512], F32)) for i in range(7)]
        pst = E(nc.psum_tensor("pst", [128, 1024], BF16))
        psk = ["ps%d" % i for i in range(7)]

        for l in range(depth):
            for f in range(NF):
                for half in range(2):
                    src = w_gu[l, :, half * FF + f * 128: half * FF + (f + 1) * 128].rearrange("(c p) j -> p c j", p=128)
                    op("pool", lambda h, src=src, f=f, half=half, l=l: h.dma_start(
                        out=wgu_s[l, f, :, :, half * 128:(half + 1) * 128], in_=src),
                        writes=["wgu_s"], dma="wgucvt", nowaw=True)
        P.barrier()

        phase_ctr = [0]
        if stop == 0:
            depth_iter = []
        else:
            depth_iter = range(depth)
        try:
          for l in depth_iter:
            lam_init = 0.8 - 0.6 * math.exp(-0.3 * l)
            x_src = x_in if l == 0 else xs
            x_dst = y_out if l == depth - 1 else xs
            for s in range(NSEQ):
                with ExitStack() as ph:
                    wk = sb("wk", [128, 8, 1664], BF16, ph)
                    for c in range(8):
                        rows = w_in[l, c * 128:(c + 1) * 128, :]
                        op("pool", lambda h, c=c, rows=rows: h.dma_start(out=wk[:, c, 0:512], in_=rows[:, OFF_B_K:OFF_B_K + 512]), writes=["wk"], dma="wk", nowaw=True)
                        op("pool", lambda h, c=c, rows=rows: h.dma_start(out=wk[:, c, 512:768], in_=rows[:, OFF_C_K:OFF_C_K + 256]), writes=["wk"], dma="wk", nowaw=True)
                        for r in range(4):
                            op("pool", lambda h, c=c, rows=rows, r=r: h.dma_start(out=wk[:, c, 768 + 32 * r:800 + 32 * r], in_=rows[:, OFF_I_K:OFF_I_K + 32]), writes=["wk"], dma="wk", nowaw=True)
                        op("pool", lambda h, c=c, rows=rows: h.dma_start(out=wk[:, c, 896:1408], in_=rows[:, OFF_B_V:OFF_B_V + 512]), writes=["wk"], dma="wk", nowaw=True)
                        op("pool", lambda h, c=c, rows=rows: h.dma_start(out=wk[:, c, 1408:1664], in_=rows[:, OFF_C_V:OFF_C_V + 256]), writes=["wk"], dma="wk", nowaw=True)
                    xin = [sb("xin%d" % i, [128, 4, D], F32, ph) for i in range(2)]
                    xbf = [sb("xbf%d" % i, [128, 4, D], BF16, ph) for i in range(2)]
                    xT = [sb("xT%d" % i, [128, 8, 512], BF16, ph) for i in range(2)]
                    kst = [sb("kst%d" % i, [128, 7, 512], BF16, ph) for i in range(2)]
                    vst = [sb("vst%d" % i, [128, 4, 768], BF16, ph) for i in range(2)]
                    cnt = 0
                    for g in range(NGD):
                        b = g % 2
                        t0 = g * 512
                        op("sp", lambda h, b=b, t0=t0: h.dma_start(out=xin[b][:], in_=x_src[s, t0:t0 + 512, :].rearrange("(r p) d -> p r d", p=128)),
                           writes=["xin%d" % b], dma="xin%d" % b)
                        op("dve" if g % 2 == 0 else "pool", lambda h, b=b: h.tensor_copy(out=xbf[b][:], in_=xin[b][:]), reads=["xin%d" % b], writes=["xbf%d" % b])
                        for r in range(4):
                            for c in range(8):
                                op("pe", lambda h, b=b, r=r, c=c: h.transpose(pst[:, c * 128:(c + 1) * 128], xbf[b][:, r, c * 128:(c + 1) * 128], ident[:]),
                                   reads=["xbf%d" % b, "ident"], writes=["pst"], inc=(c == 7))
                            op("act" if r % 2 == 0 else "dve",
                               (lambda h, b=b, r=r: h.copy(out=xT[b][:, :, r * 128:(r + 1) * 128], in_=pst[:].rearrange("p (c t) -> p c t", c=8))) if r % 2 == 0 else
                               (lambda h, b=b, r=r: h.tensor_copy(out=xT[b][:, :, r * 128:(r + 1) * 128], in_=pst[:].rearrange("p (c t) -> p c t", c=8))),
                               reads=["pst"], writes=["xT%d" % b])
                        op("sp", lambda h, b=b, t0=t0: h.dma_start(out=xT_s[s, :, :, t0:t0 + 512], in_=xT[b][:]), reads=["xT%d" % b], writes=["xT_s"], dma="st_xT", nowaw=True)
                        for cc in range(7):
                            pk = cnt % 6
                            cnt += 1
                            for c in range(8):
                                op("pe", lambda h, pk=pk, cc=cc, c=c, b=b: h.matmul(ps[pk][:], wk[:, c, cc * 128:(cc + 1) * 128], xT[b][:, c, :], start=(c == 0), stop=(c == 7)),
                                   reads=["wk", "xT%d" % b], writes=[psk[pk]], inc=(c == 7))
                            if cc % 2 == 0:
                                op("act", lambda h, pk=pk, cc=cc, b=b: h.copy(out=kst[b][:, cc, :], in_=ps[pk][:]), reads=[psk[pk]], writes=["kst%d" % b])
                            else:
                                op("dve", lambda h, pk=pk, cc=cc, b=b: h.tensor_copy(out=kst[b][:, cc, :], in_=ps[pk][:]), reads=[psk[pk]], writes=["kst%d" % b])
                        op("sp", lambda h, b=b, t0=t0: h.dma_start(out=ktb_s[s, :, :, t0:t0 + 512], in_=kst[b][:, 0:4, :]), reads=["kst%d" % b], writes=["ktb_s"], dma="st_k", nowaw=True)
                        op("sp", lambda h, b=b, t0=t0: h.dma_start(out=ktc_s[s, :, :, t0:t0 + 512], in_=kst[b][:, 4:6, :]), reads=["kst%d" % b], writes=["ktc_s"], dma="st_k", nowaw=True)
                        op("sp", lambda h, b=b, t0=t0: h.dma_start(out=ki4_s[s, :, t0:t0 + 512], in_=kst[b][:, 6, :]), reads=["kst%d" % b], writes=["ki4_s"], dma="st_k", nowaw=True)
                        for r in range(4):
                            pk = cnt % 6
                            cnt += 1
                            for c in range(8):
                                op("pe", lambda h, pk=pk, r=r, c=c, b=b: h.matmul(ps[pk][:], xT[b][:, c, r * 128:(r + 1) * 128], wk[:, c, 896:1408], start=(c == 0), stop=(c == 7)),
                                   reads=["wk", "xT%d" % b], writes=[psk[pk]], inc=(c == 7))
                            op("act", lambda h, pk=pk, r=r, b=b: h.copy(out=vst[b][:, r, 0:512], in_=ps[pk][:]), reads=[psk[pk]], writes=["vst%d" % b])
                            pk = cnt % 6
                            cnt += 1
                            for c in range(8):
                                op("pe", lambda h, pk=pk, r=r, c=c, b=b: h.matmul(ps[pk][:, 0:256], xT[b][:, c, r * 128:(r + 1) * 128], wk[:, c, 1408:1664], start=(c == 0), stop=(c == 7)),
                                   reads=["wk", "xT%d" % b], writes=[psk[pk]], inc=(c == 7))
                            op("dve", lambda h, pk=pk, r=r, b=b: h.tensor_copy(out=vst[b][:, r, 512:768], in_=ps[pk][:, 0:256]), reads=[psk[pk]], writes=["vst%d" % b])
                        op("sp", lambda h, b=b, g=g: h.dma_start(out=vb_s[s, :, 4 * g:4 * g + 4, :], in_=vst[b][:, :, 0:512]), reads=["vst%d" % b], writes=["vb_s"], dma="st_v", nowaw=True)
                        op("sp", lambda h, b=b, g=g: h.dma_start(out=vc_s[s, :, 4 * g:4 * g + 4, :], in_=vst[b][:, :, 512:768]), reads=["vst%d" % b], writes=["vc_s"], dma="st_v", nowaw=True)
                    P.barrier()
                    phase_ctr[0] += 1
                    if stop is not None and phase_ctr[0] >= stop:
                        P.stopped = True

                with ExitStack() as ph:
                    ktc = sb("ktc", [128, 2, S], BF16, ph)
                    vc = sb("vc", [128, NT, 256], BF16, ph)
                    ki4 = sb("ki4", [128, S], BF16, ph)
                    op("sp", lambda h: h.dma_start(out=ktc[:], in_=ktc_s[s]), writes=["ktc"], dma="ldkv")
                    op("sp", lambda h: h.dma_start(out=vc[:], in_=vc_s[s]), writes=["vc"], dma="ldkv")
                    op("sp", lambda h: h.dma_start(out=ki4[:], in_=ki4_s[s]), writes=["ki4"], dma="ldkv")
                    wq = sb("wq", [128, 8, 768], BF16, ph)
                    wv = sb("wv", [128, 8, 264], BF16, ph)
                    for c in range(8):
                        rows = w_in[l, c * 128:(c + 1) * 128, :]
                        op("pool", lambda h, c=c, rows=rows: h.dma_start(out=wq[:, c, 0:256], in_=rows[:, OFF_A_U:OFF_A_U + 256]), writes=["wq"], dma="wq", nowaw=True)
                        op("pool", lambda h, c=c, rows=rows: h.dma_start(out=wq[:, c, 256:512], in_=rows[:, OFF_C_Q:OFF_C_Q + 256]), writes=["wq"], dma="wq", nowaw=True)
                        op("pool", lambda h, c=c, rows=rows: h.dma_start(out=wq[:, c, 512:768], in_=rows[:, OFF_I_Q:OFF_I_Q + 256]), writes=["wq"], dma="wq", nowaw=True)
                        op("pool", lambda h, c=c, rows=rows: h.dma_start(out=wv[:, c, 0:256], in_=rows[:, OFF_A_V:OFF_A_V + 256]), writes=["wv"], dma="wq", nowaw=True)
                        op("pool", lambda h, c=c, rows=rows: h.dma_start(out=wv[:, c, 256:264], in_=rows[:, OFF_I_W:OFF_I_W + 8]), writes=["wv"], dma="wq", nowaw=True)
                    wmT = sb("wmT", [128, 4, 128], BF16, ph)
                    wmf = sb("wmf", [128, 4, 128], F32, ph)
                    bsT = sb("bsT", [128, 2, 128], F32, ph)
                    lng = sb("lng", [128, 256], F32, ph)
                    lnb = sb("lnb", [128, 256], F32, ph)
                    wmb = sb("wmb", [128, 4, 128], BF16, ph)
                    op("sp", lambda h: h.dma_start(out=wmf[:], in_=w_s[l].rearrange("g t s -> t g s")), writes=["wmf"], dma="gpw")
                    op("dve", lambda h: h.tensor_copy(out=wmb[:], in_=wmf[:]), reads=["wmf"], writes=["wmb"])
                    for gg in range(4):
                        op("pe", lambda h, gg=gg: h.transpose(pst[:, gg * 128:(gg + 1) * 128], wmb[:, gg, :], ident[:]), reads=["wmb", "ident"], writes=["pst"], inc=(gg == 3))
                    for gg in range(4):
                        op("sp", lambda h, gg=gg: h.dma_start(out=bsT[(gg % 2) * 64:(gg % 2) * 64 + 64, gg // 2, :], in_=b_s[l, gg:gg + 1, :].partition_broadcast(64)),
                           writes=["bsT"], dma="gp", nowaw=True)
                    op("sp", lambda h: h.dma_start(out=lng[:], in_=a_g[l].partition_broadcast(128)), writes=["lng"], dma="gp")
                    op("sp", lambda h: h.dma_start(out=lnb[:], in_=a_b[l].partition_broadcast(128)), writes=["lnb"], dma="gp")
                    for gg in range(4):
                        op("dve", lambda h, gg=gg: h.tensor_tensor(out=wmT[:, gg, :], in0=pst[:, gg * 128:(gg + 1) * 128], in1=gmask[:], op=ALU.mult), reads=["pst", "gmask"], writes=["wmT"])

                    xT = [sb("xTa%d" % i, [128, 8, 512], BF16, ph) for i in range(2)]
                    uT = sb("uT", [128, 2, 512], BF16, ph)
                    qcp = sb("qcp", [128, 4, 512], BF16, ph)
                    qip = sb("qip", [128, 8, 512], BF16, ph)
                    vtm = sb("vtm", [128, 264], F32, ph)
                    vg = sb("vg", [128, 256], F32, ph)
                    vsq = sb("vsq", [128, 256], F32, ph)
                    st4 = sb("st4", [128, 16], F32, ph)
                    vn = sb("vn", [128, 256], BF16, ph)
                    wab = sb("wab", [128, 8], F32, ph)
                    sgn = sb("sgn", [128, 8], F32, ph)
                    dsg = sb("dsg", [128, 8, 128], BF16, ph)
                    scoreb = [sb("score%d" % i_, [128, S], F32, ph) for i_ in range(2)]
                    cmb = sb("cmb", [128, 128], BF16, ph)
                    op("pool", lambda h: h.dma_start(out=cmb[:], in_=c_cmask), writes=["cmb"], dma="gpc")
                    mb = sb("mb", [128, S], BF16, ph)
                    mbT = sb("mbT", [128, NT, 128], BF16, ph)
                    rl = [sb("rl%d" % i, [128, 512], BF16, ph) for i in range(4)]
                    pT = [sb("pTa%d" % i, [128, 4, 128], BF16, ph) for i in range(3)]
                    bis = sb("bis", [128, 8], F32, ph)
                    mixT = [sb("mixa%d" % i, [128, 4, 512], BF16, ph) for i in range(2)]
                    tmpa = sb("tmpa", [128, 128], F32, ph)
                    rcp = sb("rcp", [128, 128], F32, ph)
                    qcp2 = sb("qcp2", [128, 4, 512], BF16, ph)
                    op("dve", lambda h: h.memset(qcp[:], 0.0), writes=["qcp0"])
                    op("dve", lambda h: h.memset(qcp2[:], 0.0), writes=["qcp1"])
                    op("pool", lambda h: h.memset(qip[:], 0.0), writes=["qip"])
                    op("pool", lambda h: h.memset(mbT[:], 0.0), writes=["mbT"])
                    dblk = sb("dblk", [128, 128], BF16, ph)
                    dT = sb("dT", [128, 128], BF16, ph)
                    anys = sb("anys", [128, 64], F32, ph)
                    op("dve", lambda h: h.memset(dblk[:], 0.0), writes=["dblk"])
                    op("dve", lambda h: h.memset(dT[:], 0.0), writes=["dT"])

                    rlc = 0
                    ptc = 0
                    qcpb = [qcp, qcp2]

                    def prologue(g):
                        nonlocal rlc, ptc
                        b = g % 2
                        t0 = g * 512
                        mx = mixT[b]
                        mxk = "mixa%d" % b
                        op("sp", lambda h, b=b, t0=t0: h.dma_start(out=xT[b][:], in_=xT_s[s, :, :, t0:t0 + 512]), writes=["xTa%d" % b], dma="ldxT%d" % b)
                        for cc in range(6):
                            pk = cc % 2
                            for c in range(8):
                                op("pe", lambda h, pk=pk, cc=cc, c=c, b=b: h.matmul(ps[pk][:], wq[:, c, cc * 128:(cc + 1) * 128], xT[b][:, c, :], start=(c == 0), stop=(c == 7)),
                                   reads=["wq", "xTa%d" % b], writes=[psk[pk]], inc=(c == 7))
                            if cc < 2:
                                op("act", lambda h, pk=pk, cc=cc: h.activation(out=uT[:, cc, :], in_=ps[pk][:], func=AF.Gelu_apprx_tanh), reads=[psk[pk]], writes=["uT"])
                            elif cc < 4:
                                for k in range(2):
                                    hh = (cc - 2) * 2 + k
                                    op("dve", lambda h, pk=pk, hh=hh, k=k: h.tensor_copy(out=qcpb[g % 2][64 * k:64 * k + 64, hh, :], in_=ps[pk][64 * k:64 * k + 64, :]), reads=[psk[pk]], writes=["qcp%d" % (g % 2)])
                            else:
                                for k in range(4):
                                    hh = (cc - 4) * 4 + k
                                    op("dve" if k % 2 == 0 else "act",
                                       (lambda h, pk=pk, hh=hh, k=k: h.tensor_copy(out=qip[32 * k:32 * k + 32, hh, :], in_=ps[pk][32 * k:32 * k + 32, :])) if k % 2 == 0 else
                                       (lambda h, pk=pk, hh=hh, k=k: h.copy(out=qip[32 * k:32 * k + 32, hh, :], in_=ps[pk][32 * k:32 * k + 32, :])),
                                       reads=[psk[pk]], writes=["qip"])

                    def stageA1a(g, r):
                        nonlocal rlc, ptc
                        if True:
                            b = g % 2
                            t0 = g * 512
                            mx = mixT[b]
                            mxk = "mixa%d" % b
                            i = 4 * g + r
                            q0 = r * 128
                            nk = 128 * (i + 1)
                            for c in range(8):
                                op("pe", lambda h, c=c, b=b, q0=q0: h.matmul(ps[4][:, 0:264], xT[b][:, c, q0:q0 + 128], wv[:, c, :], start=(c == 0), stop=(c == 7)),
                                   reads=["wv", "xTa%d" % b], writes=[psk[4]], inc=(c == 7))
                            op("act", lambda h: h.activation(out=vg[:], in_=ps[4][:, 0:256], func=AF.Gelu_apprx_tanh), reads=[psk[4]], writes=["vg"])
                            op("dve", lambda h: h.tensor_copy(out=vtm[:, 256:264], in_=ps[4][:, 256:264]), reads=[psk[4]], writes=["vtm"])
                            op("dve", lambda h: h.tensor_scalar(out=wab[:], in0=vtm[:, 256:264], scalar1=-1.0, scalar2=None, op0=ALU.mult), reads=["vtm"], writes=["wab"])
                            op("dve", lambda h: h.tensor_tensor(out=wab[:], in0=wab[:], in1=vtm[:, 256:264], op=ALU.max), reads=["vtm", "wab"], writes=["wab"])
                            op("dve", lambda h: h.tensor_scalar(out=wab[:], in0=wab[:], scalar1=IDX_SCALE, scalar2=None, op0=ALU.mult), reads=["wab"], writes=["wab"])
                            op("dve", lambda h: h.tensor_scalar(out=sgn[:], in0=vtm[:, 256:264], scalar1=0.0, scalar2=2.0, op0=ALU.is_ge, op1=ALU.mult), reads=["vtm"], writes=["sgn"])
                            op("dve", lambda h: h.tensor_scalar(out=sgn[:], in0=sgn[:], scalar1=-1.0, scalar2=None, op0=ALU.add), reads=["sgn"], writes=["sgn"])
                            for hh in range(8):
                                op("dve", lambda h, hh=hh: h.tensor_scalar(out=dsg[:, hh, :], in0=ident[:], scalar1=sgn[:, hh:hh + 1], scalar2=None, op0=ALU.mult),
                                   reads=["ident", "sgn"], writes=["dsg%d" % hh])
                            v3 = vg[:].rearrange("p (g c) -> p g c", c=64)
                            op("dve", lambda h: h.tensor_reduce(out=st4[:, 0:4], in_=v3, axis=AX.X, op=ALU.add), reads=["vg"], writes=["st4"])
                            op("dve", lambda h: h.tensor_tensor(out=vsq[:], in0=vg[:], in1=vg[:], op=ALU.mult), reads=["vg"], writes=["vsq"])
                            op("dve", lambda h: h.tensor_reduce(out=st4[:, 4:8], in_=vsq[:].rearrange("p (g c) -> p g c", c=64), axis=AX.X, op=ALU.add), reads=["vsq"], writes=["st4"])
                            op("dve", lambda h: h.tensor_scalar(out=st4[:, 0:4], in0=st4[:, 0:4], scalar1=1.0 / 64, scalar2=None, op0=ALU.mult), reads=["st4"], writes=["st4"])
                            op("dve", lambda h: h.tensor_tensor(out=st4[:, 8:12], in0=st4[:, 0:4], in1=st4[:, 0:4], op=ALU.mult), reads=["st4"], writes=["st4"])
                            op("dve", lambda h: h.scalar_tensor_tensor(out=st4[:, 4:8], in0=st4[:, 4:8], scalar=1.0 / 64, in1=st4[:, 8:12], op0=ALU.mult, op1=ALU.subtract), reads=["st4"], writes=["st4"])
                            op("act", lambda h: h.activation(out=st4[:, 8:12], in_=st4[:, 4:8], func=AF.Ln, bias=EPS), reads=["st4"], writes=["st4"])
                            op("act", lambda h: h.activation(out=st4[:, 12:16], in_=st4[:, 8:12], func=AF.Exp, scale=-0.5), reads=["st4"], writes=["st4"])
                            for gg in range(4):
                                op("dve", lambda h, gg=gg: h.tensor_scalar(out=vsq[:, gg * 64:(gg + 1) * 64], in0=vg[:, gg * 64:(gg + 1) * 64], scalar1=st4[:, gg:gg + 1], scalar2=st4[:, 12 + gg:13 + gg],
                                                                    op0=ALU.subtract, op1=ALU.mult), reads=["vg", "st4"], writes=["vsq"])
                            op("dve", lambda h: h.tensor_tensor(out=vsq[:], in0=vsq[:], in1=lng[:], op=ALU.mult), reads=["vsq", "lng"], writes=["vsq"])
                            op("dve", lambda h: h.tensor_tensor(out=vn[:], in0=vsq[:], in1=lnb[:], op=ALU.add), reads=["vsq", "lnb"], writes=["vn"])
                            for gg in range(4):
                                ck = gg // 2
                                op("pe", lambda h, gg=gg, ck=ck: h.matmul(ps[5][:, (gg % 2) * 128:(gg % 2) * 128 + 128], vn[:, ck * 128:(ck + 1) * 128], wmT[:, gg, :], start=True, stop=True),
                                   reads=["vn", "wmT"], writes=[psk[5]], inc=True)
                                rs = slice((gg % 2) * 64, (gg % 2) * 64 + 64)
                                op("dve", lambda h, gg=gg, ck=ck, rs=rs: h.tensor_tensor(out=tmpa[rs, :], in0=ps[5][rs, (gg % 2) * 128:(gg % 2) * 128 + 128], in1=bsT[rs, ck, :], op=ALU.add),
                                   reads=[psk[5], "bsT"], writes=["tmpa"])
                                op("dve", lambda h, gg=gg, ck=ck, rs=rs, q0=q0: h.tensor_tensor(out=mx[rs, ck, q0:q0 + 128], in0=tmpa[rs, :], in1=uT[rs, ck, q0:q0 + 128], op=ALU.mult),
                                   reads=["tmpa", "uT"], writes=[mxk])

                    def stageA1b(g, r):
                        nonlocal rlc, ptc
                        if True:
                            b = g % 2
                            t0 = g * 512
                            mx = mixT[b]
                            mxk = "mixa%d" % b
                            i = 4 * g + r
                            q0 = r * 128
                            nk = 128 * (i + 1)
                            score = scoreb[i % 2]
                            sck = "score%d" % (i % 2)
                            nk = 128 * (i + 1)
                            if i >= 2:
                                for c0 in range(0, nk, 512):
                                    cw = min(512, nk - c0)
                                    def emit_L(hh, c0=c0, cw=cw, q0=q0):
                                        pk = 4 + hh % 2
                                        op("pe", lambda h: h.matmul(ps[pk][:, 0:cw], qip[:, hh, q0:q0 + 128], ki4[:, c0:c0 + cw], start=True, stop=True),
                                           reads=["qip", "ki4"], writes=[psk[pk]], inc=True)
                                    emit_L(0)
                                    for hh in range(8):
                                        pk = 4 + hh % 2
                                        if hh + 1 < 8:
                                            emit_L(hh + 1)
                                        rb = rlc % 4
                                        rlc += 1
                                        op("act", lambda h, pk=pk, hh=hh, cw=cw, rb=rb: h.activation(out=rl[rb][:, 0:cw], in_=ps[pk][:, 0:cw], func=AF.Relu, scale=wab[:, hh:hh + 1]),
                                           reads=[psk[pk], "wab"], writes=["rl%d" % rb])
                                        op("pe", lambda h, hh=hh, cw=cw, rb=rb: h.matmul(ps[6][:, 0:cw], dsg[:, hh, :], rl[rb][:, 0:cw], start=(hh == 0), stop=(hh == 7)),
                                           reads=["dsg%d" % hh, "rl%d" % rb], writes=[psk[6]], inc=(hh == 7))
                                    if c0 + cw == nk:
                                        op("pe", lambda h, cw=cw: h.matmul(ps[6][:, cw - 128:cw], ident[:], cmb[:], start=False, stop=True, skip_group_check=True),
                                           reads=["ident", "cmb"], writes=[psk[6]], inc=True)
                                    op("act", lambda h, c0=c0, cw=cw: h.copy(out=score[:, c0:c0 + cw], in_=ps[6][:, 0:cw]), reads=[psk[6]], writes=[sck])

                    def stageA2(g, r):
                        nonlocal rlc, ptc
                        if True:
                            b = g % 2
                            t0 = g * 512
                            mx = mixT[b]
                            mxk = "mixa%d" % b
                            i = 4 * g + r
                            q0 = r * 128
                            nk = 128 * (i + 1)
                            score = scoreb[i % 2]
                            sck = "score%d" % (i % 2)
                            if i >= 2:
                                op("dve", lambda h: h.memset(bis[:, 0:1], BIS_LO), writes=["bis"])
                                for it in range(NBIS):
                                    cst = BIS_W / (2.0 ** (it + 1))
                                    op("dve", lambda h, cst=cst: h.tensor_scalar(out=bis[:, 1:2], in0=bis[:, 0:1], scalar1=cst, scalar2=None, op0=ALU.add), reads=["bis"], writes=["bis"])
                                    op("dve", lambda h, nk=nk: h.tensor_scalar(out=mb[:, 0:nk], in0=score[:, 0:nk], scalar1=bis[:, 1:2], scalar2=None, op0=ALU.is_ge, op1=ALU.add, accum_out=bis[:, 2:3]),
                                       reads=[sck, "bis"], writes=["mb", "bis"])
                                    op("dve", lambda h, cst=cst: h.tensor_scalar(out=bis[:, 3:4], in0=bis[:, 2:3], scalar1=float(TOPK), scalar2=cst, op0=ALU.is_ge, op1=ALU.mult), reads=["bis"], writes=["bis"])
                                    op("dve", lambda h: h.tensor_tensor(out=bis[:, 0:1], in0=bis[:, 0:1], in1=bis[:, 3:4], op=ALU.add), reads=["bis"], writes=["bis"])
                                op("dve", lambda h, nk=nk: h.tensor_scalar(out=mb[:, 0:nk], in0=score[:, 0:nk], scalar1=bis[:, 0:1], scalar2=NEG, op0=ALU.is_lt, op1=ALU.mult), reads=[sck, "bis"], writes=["mb"])
                                nb = i + 1
                                op("dve", lambda h, nk=nk, nb=nb: h.tensor_reduce(out=anys[:, 0:nb], in_=mb[:, 0:nk].rearrange("p (j k) -> p j k", k=128), axis=AX.X, op=ALU.max), reads=["mb"], writes=["anys"])
                                op("dve", lambda h, nb=nb: h.tensor_scalar(out=anys[:, 0:nb], in0=anys[:, 0:nb], scalar1=-1.0, scalar2=None, op0=ALU.is_ge), reads=["anys"], writes=["anys"])
                                op("dve", lambda h, nb=nb: h.tensor_tensor(out=anys[:, 0:nb], in0=anys[:, 0:nb], in1=jpos1[:, 0:nb], op=ALU.mult), reads=["anys", "jpos1"], writes=["anys"])
                                op("dve", lambda h, nb=nb: h.tensor_reduce(out=anys[:, 32:33], in_=anys[:, 0:nb], axis=AX.X, op=ALU.max), reads=["anys"], writes=["anys"])
                                op("dve", lambda h, nb=nb: h.tensor_scalar(out=dblk[:, 0:1], in0=anys[:, 32:33], scalar1=-128.0, scalar2=128.0 * nb, op0=ALU.mult, op1=ALU.add), reads=["anys"], writes=["dblk"])

                    def stageB1(g, r):
                        nonlocal rlc, ptc
                        if True:
                            b = g % 2
                            t0 = g * 512
                            mx = mixT[b]
                            mxk = "mixa%d" % b
                            i = 4 * g + r
                            q0 = r * 128
                            nk = 128 * (i + 1)
                            if i >= 2:
                                for j0 in range(0, i + 1, 8):
                                    nj = min(8, i + 1 - j0)
                                    for jj in range(nj):
                                        op("pe", lambda h, j0=j0, jj=jj: h.transpose(pst[:, jj * 128:(jj + 1) * 128], mb[:, (j0 + jj) * 128:(j0 + jj + 1) * 128], ident[:]),
                                           reads=["mb", "ident"], writes=["pst"], inc=(jj == nj - 1))
                                    op("act" if (j0 // 8) % 2 == 0 else "dve",
                                       (lambda h, j0=j0, nj=nj: h.copy(out=mbT[:, j0:j0 + nj, :], in_=pst[:, 0:nj * 128].rearrange("p (j t) -> p j t", t=128))) if (j0 // 8) % 2 == 0 else
                                       (lambda h, j0=j0, nj=nj: h.tensor_copy(out=mbT[:, j0:j0 + nj, :], in_=pst[:, 0:nj * 128].rearrange("p (j t) -> p j t", t=128))),
                                       reads=["pst"], writes=["mbT"])
                                op("pe", lambda h: h.transpose(pst[:, 0:128], dblk[:], ident[:]), reads=["dblk", "ident"], writes=["pst"], inc=True)
                                op("dve", lambda h: h.tensor_copy(out=dT[:], in_=pst[:, 0:128]), reads=["pst"], writes=["dT"])

                    def stageB2(g, r):
                        nonlocal rlc, ptc
                        if True:
                            b = g % 2
                            t0 = g * 512
                            mx = mixT[b]
                            mxk = "mixa%d" % b
                            i = 4 * g + r
                            q0 = r * 128
                            nk = 128 * (i + 1)
                            if debug and l == 0 and s == 0 and i == 2:
                                dump("vn", vn[:], 256, ["vn"])
                                dump("wmT", wmT[:].rearrange("p a b -> p (a b)"), 512, ["wmT"])
                                dump("st4", st4[:], 16, ["st4"])
                                dump("tmpa", tmpa[:], 128, ["tmpa"])
                                dump("vg", vg[:], 256, ["vg"])
                                dump("uT", uT[:].rearrange("p a b -> p (a b)")[:, 0:512], 512, ["uT"])
                                dump("score", scoreb[0][:, 0:384], 384, ["score0"])
                                dump("bis", bis[:], 8, ["bis"])
                                dump("mb", mb[:, 0:384], 384, ["mb"])
                                dump("mbT", mbT[:].rearrange("p a b -> p (a b)")[:, 0:384], 384, ["mbT"])
                                dump("anys", anys[:], 64, ["anys"])
                                dump("dT", dT[:], 128, ["dT"])
                                dump("wab", wab[:], 8, ["wab"])
                                dump("sgn", sgn[:], 8, ["sgn"])
                                dump("bsT", bsT[:].rearrange("p a b -> p (a b)"), 256, ["bsT"])
                            pend = []

                            def emit_pv_c(item, i=i):
                                j, pslot = item
                                first, last = (j == 0), (j == i)
                                pk_ = "pTa%d" % pslot
                                for hh in range(4):
                                    ab = hh // 2
                                    co = (hh % 2) * 256
                                    op("pe", lambda h: h.matmul(ps[ab][:, co:co + 128], vc[:, j, ab * 128:(ab + 1) * 128], pT[pslot][:, hh, :], start=(first and hh % 2 == 0), stop=last, skip_group_check=True),
                                       reads=["vc", pk_], writes=[psk[ab]], inc=False)
                                    op("pe", lambda h: h.matmul(ps[ab][:, co + 128:co + 256], ones[:], pT[pslot][:, hh, :], start=False, stop=last, skip_group_check=True),
                                       reads=["ones", pk_], writes=[psk[ab]], inc=(hh == 3))

                            for j in range(0, i + 1):
                                sbk = 2 + (ptc % 2)
                                pslot = ptc % 3
                                ptc += 1
                                dg = (j == i)
                                for hh in range(4):
                                    ck = hh // 2
                                    cs = hh * 128
                                    op("pe", lambda h, sbk=sbk, cs=cs, hh=hh, j=j, ck=ck: h.matmul(ps[sbk][:, cs:cs + 128], ktc[:, ck, j * 128:(j + 1) * 128], qcpb[g % 2][:, hh, q0:q0 + 128], start=(hh == 0), stop=False, skip_group_check=True),
                                       reads=["ktc", "qcp%d" % (g % 2)], writes=[psk[sbk]], inc=False)
                                    op("pe", lambda h, sbk=sbk, cs=cs, j=j: h.matmul(ps[sbk][:, cs:cs + 128], ident[:], mbT[:, j, :], start=False, stop=False, skip_group_check=True),
                                       reads=["ident", "mbT"], writes=[psk[sbk]], inc=False)
                                    op("pe", lambda h, sbk=sbk, cs=cs, hh=hh, dg=dg: h.matmul(ps[sbk][:, cs:cs + 128], slmat[:, hh, :], dT[:], start=False, stop=(not dg), skip_group_check=True),
                                       reads=["slmat", "dT"], writes=[psk[sbk]], inc=((not dg) and hh == 3))
                                    if dg:
                                        op("pe", lambda h, sbk=sbk, cs=cs, hh=hh: h.matmul(ps[sbk][:, cs:cs + 128], ident[:], diag[:, hh, :], start=False, stop=True, skip_group_check=True),
                                           reads=["ident", "diag"], writes=[psk[sbk]], inc=(hh == 3))
                                if dg:
                                    op("act", lambda h, sbk=sbk, pslot=pslot: h.activation(out=pT[pslot][:].rearrange("p a b -> p (a b)"), in_=ps[sbk][:], func=AF.Exp, scale=0.125),
                                       reads=[psk[sbk]], writes=["pTa%d" % pslot])
                                else:
                                    kk = i - j + 1
                                    for hh in range(4):
                                        op("act", lambda h, sbk=sbk, pslot=pslot, hh=hh, kk=kk: h.activation(out=pT[pslot][:, hh, :], in_=ps[sbk][:, hh * 128:(hh + 1) * 128], func=AF.Exp, scale=0.125,
                                                                                                    bias=alibi[:, hh * 34 + kk:hh * 34 + kk + 1]),
                                           reads=[psk[sbk], "alibi"], writes=["pTa%d" % pslot])
                                pend.append((j, pslot))
                                if len(pend) > 1:
                                    emit_pv_c(pend.pop(0))
                            while pend:
                                emit_pv_c(pend.pop(0))
                            for hh in range(4):
                                ab = hh // 2
                                co = (hh % 2) * 256
                                rs = slice(64 * (hh % 2), 64 * (hh % 2) + 64)
                                op("dve", lambda h, ab=ab, co=co, rs=rs: h.reciprocal(out=rcp[rs, :], in_=ps[ab][rs, co + 128:co + 256]), reads=[psk[ab]], writes=["rcp"])
                                op("dve", lambda h, ab=ab, co=co, rs=rs, q0=q0: h.tensor_tensor(out=mx[rs, 2 + ab, q0:q0 + 128], in0=ps[ab][rs, co:co + 128], in1=rcp[rs, :], op=ALU.mult),
                                   reads=[psk[ab], "rcp"], writes=[mxk])
                            if r == 3:
                                op("sp", lambda h, b=b, t0=t0: h.dma_start(out=mixT_s[s, :, 0:2, t0:t0 + 512], in_=mixT[b][:, 0:2, :]), reads=[mxk], writes=["mixT_s"], dma="st_mx", nowaw=True)
                                op("sp", lambda h, b=b, t0=t0: h.dma_start(out=mixT_s[s, :, 6:8, t0:t0 + 512], in_=mixT[b][:, 2:4, :]), reads=[mxk], writes=["mixT_s"], dma="st_mx", nowaw=True)

                    tiles = [(g_, r_) for g_ in range(NGD) for r_ in range(4)]
                    NTL = len(tiles)
                    prologue(0)
                    stageA1a(*tiles[0])
                    stageA1b(*tiles[0])
                    stageA2(*tiles[0])
                    if NTL > 1:
                        stageA1a(*tiles[1])
                        stageA1b(*tiles[1])
                    for ti in range(NTL):
                        stageB1(*tiles[ti])
                        if ti + 2 < NTL:
                            if tiles[ti + 2][1] == 0:
                                prologue(tiles[ti + 2][0])
                            stageA1a(*tiles[ti + 2])
                        if ti + 1 < NTL:
                            stageA2(*tiles[ti + 1])
                        if ti + 2 < NTL:
                            stageA1b(*tiles[ti + 2])
                        stageB2(*tiles[ti])
                    P.barrier()
                    phase_ctr[0] += 1
                    if stop is not None and phase_ctr[0] >= stop:
                        P.stopped = True

                with ExitStack() as ph:
                    ktb = sb("ktb", [128, 4, S], BF16, ph)
                    vb = sb("vb", [128, NT, 512], BF16, ph)
                    op("sp", lambda h: h.dma_start(out=ktb[:], in_=ktb_s[s]), writes=["ktb"], dma="ldkv")
                    op("sp", lambda h: h.dma_start(out=vb[:], in_=vb_s[s]), writes=["vb"], dma="ldkv")
                    wq = sb("wqb", [128, 8, 512], BF16, ph)
                    for c in range(8):
                        op("pool", lambda h, c=c: h.dma_start(out=wq[:, c, :], in_=w_in[l, c * 128:(c + 1) * 128, OFF_B_Q:OFF_B_Q + 512]), writes=["wqb"], dma="wq", nowaw=True)
                    lv = sb("lv", [128, 4, 64], F32, ph)
                    lsm = sb("lsm", [128, 8], F32, ph)
                    gcol = sb("gcol", [128, 1], F32, ph)
                    op("sp", lambda h: h.dma_start(out=lv[:].rearrange("p a b -> p (a b)"), in_=lamv[l:l + 1].rearrange("o a b -> o (a b)").partition_broadcast(128)), writes=["lv"], dma="gp")
                    op("sp", lambda h: h.dma_start(out=gcol[:], in_=subg[l]), writes=["gcol"], dma="gp")
                    op("dve", lambda h: h.tensor_tensor(out=lv[:, 0, :], in0=lv[:, 0, :], in1=lv[:, 1, :], op=ALU.mult), reads=["lv"], writes=["lv"])
                    op("dve", lambda h: h.tensor_tensor(out=lv[:, 2, :], in0=lv[:, 2, :], in1=lv[:, 3, :], op=ALU.mult), reads=["lv"], writes=["lv"])
                    op("dve", lambda h: h.tensor_reduce(out=lsm[:, 0:1], in_=lv[:, 0, :], axis=AX.X, op=ALU.add), reads=["lv"], writes=["lsm"])
                    op("dve", lambda h: h.tensor_reduce(out=lsm[:, 1:2], in_=lv[:, 2, :], axis=AX.X, op=ALU.add), reads=["lv"], writes=["lsm"])
                    op("act", lambda h: h.activation(out=lsm[:, 2:4], in_=lsm[:, 0:2], func=AF.Exp), reads=["lsm"], writes=["lsm2"])
                    op("dve", lambda h: h.tensor_tensor(out=lsm[:, 4:5], in0=lsm[:, 3:4], in1=lsm[:, 2:3], op=ALU.subtract), reads=["lsm2"], writes=["lsm3"])
                    op("dve", lambda h: h.tensor_scalar(out=lsm[:, 5:6], in0=lsm[:, 4:5], scalar1=-lam_init, scalar2=None, op0=ALU.add), reads=["lsm3"], writes=["neglam"])
                    xT = [sb("xTb%d" % i, [128, 8, 512], BF16, ph) for i in range(2)]
                    qbp = sb("qbp", [128, 8, 512], BF16, ph)
                    pT = [sb("pTb%d" % i, [128, 4, 128], BF16, ph) for i in range(3)]
                    mixT = [sb("mixb%d" % i, [128, 4, 512], BF16, ph) for i in range(2)]
                    r1 = sb("r1", [128, 256], F32, ph)
                    oa = sb("oa", [128, 128], F32, ph)
                    ob = sb("ob", [128, 128], F32, ph)
                    oo = sb("oo", [128, 128], F32, ph)
                    osq = sb("osq", [128, 128], BF16, ph)
                    rsd = sb("rsd", [128, 128], F32, ph)
                    op("dve", lambda h: h.memset(qbp[:], 0.0), writes=["qbp"])
                    ptc = 0
                    for g in range(NGD):
                        b = g % 2
                        t0 = g * 512
                        mx = mixT[b]
                        mxk = "mixb%d" % b
                        op("sp", lambda h, b=b, t0=t0: h.dma_start(out=xT[b][:], in_=xT_s[s, :, :, t0:t0 + 512]), writes=["xTb%d" % b], dma="ldxT%d" % b)
                        for cc in range(4):
                            pk = cc % 2
                            for c in range(8):
                                op("pe", lambda h, pk=pk, cc=cc, c=c, b=b: h.matmul(ps[pk][:], wq[:, c, cc * 128:(cc + 1) * 128], xT[b][:, c, :], start=(c == 0), stop=(c == 7)),
                                   reads=["wqb", "xTb%d" % b], writes=[psk[pk]], inc=(c == 7))
                            op("dve", lambda h, pk=pk, cc=cc: h.tensor_copy(out=qbp[0:64, 2 * cc, :], in_=ps[pk][0:64, :]), reads=[psk[pk]], writes=["qbp"])
                            op("act", lambda h, pk=pk, cc=cc: h.copy(out=qbp[64:128, 2 * cc + 1, :], in_=ps[pk][64:128, :]), reads=[psk[pk]], writes=["qbp"])
                        for r in range(4):
                            i = 4 * g + r
                            q0 = r * 128
                            for pp in range(2):
                                hA, hB = 2 * pp, 2 * pp + 1
                                jlo = max(0, i - max(WIN[hA], WIN[hB]))
                                pend = []

                                def emit_pv_b(item, i=i):
                                    j, heads, pslot = item
                                    pk_ = "pTb%d" % pslot
                                    last = (j == i)
                                    for hi_, hh in enumerate(heads):
                                        ab = ACCB[hh]
                                        first = (j == max(0, i - WIN[hh]))
                                        for m in range(2):
                                            blk = (hh % 2) * 2 + m
                                            op("pe", lambda h: h.matmul(ps[ab][:, m * 256:m * 256 + 128], vb[:, j, hh * 128:(hh + 1) * 128], pT[pslot][:, blk, :], start=(first and m == 0), stop=last, skip_group_check=True),
                                               reads=["vb", pk_], writes=[psk[ab]], inc=False)
                                            op("pe", lambda h: h.matmul(ps[ab][:, m * 256 + 128:m * 256 + 256], ones[:], pT[pslot][:, blk, :], start=False, stop=last, skip_group_check=True),
                                               reads=["ones", pk_], writes=[psk[ab]], inc=(hi_ == len(heads) - 1 and m == 1))

                                for j in range(jlo, i + 1):
                                    heads = [hh for hh in (hA, hB) if j >= i - WIN[hh]]
                                    sbk = 2 + (ptc % 3)
                                    pslot = ptc % 3
                                    ptc += 1
                                    dg = (j == i)
                                    firstmm = True
                                    nmm = len(heads) * 2
                                    cnt_ = 0
                                    for hh in heads:
                                        for m in range(2):
                                            cs = ((hh % 2) * 2 + m) * 128
                                            cnt_ += 1
                                            lastmm = (cnt_ == nmm)
                                            op("pe", lambda h, sbk=sbk, cs=cs, hh=hh, m=m, j=j, dg=dg, fm=firstmm: h.matmul(ps[sbk][:, cs:cs + 128], ktb[:, hh, j * 128:(j + 1) * 128], qbp[:, 2 * hh + m, q0:q0 + 128], start=fm, stop=(not dg), skip_group_check=True),
                                               reads=["ktb", "qbp"], writes=[psk[sbk]], inc=((not dg) and lastmm))
                                            firstmm = False
                                            if dg:
                                                op("pe", lambda h, sbk=sbk, cs=cs, hh=hh: h.matmul(ps[sbk][:, cs:cs + 128], ident[:], diag[:, hh, :], start=False, stop=True, skip_group_check=True),
                                                   reads=["ident", "diag"], writes=[psk[sbk]], inc=lastmm)
                                    c_lo = (heads[0] % 2) * 256
                                    c_hi = (heads[-1] % 2) * 256 + 256
                                    if dg:
                                        op("act", lambda h, sbk=sbk, pslot=pslot, c_lo=c_lo, c_hi=c_hi: h.activation(out=pT[pslot][:].rearrange("p a b -> p (a b)")[:, c_lo:c_hi], in_=ps[sbk][:, c_lo:c_hi], func=AF.Exp, scale=0.125),
                                           reads=[psk[sbk]], writes=["pTb%d" % pslot])
                                    else:
                                        kk = i - j + 1
                                        for hh in heads:
                                            cl = (hh % 2) * 256
                                            op("act", lambda h, sbk=sbk, pslot=pslot, hh=hh, kk=kk, cl=cl: h.activation(out=pT[pslot][:].rearrange("p a b -> p (a b)")[:, cl:cl + 256], in_=ps[sbk][:, cl:cl + 256], func=AF.Exp, scale=0.125,
                                                                                                               bias=alibi[:, hh * 34 + kk:hh * 34 + kk + 1]),
                                               reads=[psk[sbk], "alibi"], writes=["pTb%d" % pslot])
                                    pend.append((j, heads, pslot))
                                    if len(pend) > 2:
                                        emit_pv_b(pend.pop(0))
                                while pend:
                                    emit_pv_b(pend.pop(0))
                                for hh in (hA, hB):
                                    pa = ACCB[hh]
                                    A = ps[pa]
                                    op("dve", lambda h, A=A: h.reciprocal(out=r1[:, 0:128], in_=A[:, 128:256]), reads=[psk[pa]], writes=["r1"])
                                    op("dve", lambda h, A=A: h.reciprocal(out=r1[:, 128:256], in_=A[:, 384:512]), reads=[psk[pa]], writes=["r1"])
                                    op("dve", lambda h, A=A: h.tensor_tensor(out=oa[:], in0=A[:, 0:128], in1=r1[:, 0:128], op=ALU.mult), reads=[psk[pa], "r1"], writes=["oa"])
                                    op("dve", lambda h, A=A: h.tensor_tensor(out=ob[:], in0=A[:, 256:384], in1=r1[:, 128:256], op=ALU.mult), reads=[psk[pa], "r1"], writes=["ob"])
                                    op("dve", lambda h: h.scalar_tensor_tensor(out=oo[:], in0=ob[:], scalar=lsm[:, 5:6], in1=oa[:], op0=ALU.mult, op1=ALU.add), reads=["oa", "ob", "neglam"], writes=["oo"])
                                    op("pool", lambda h: h.tensor_tensor(out=osq[:], in0=oo[:], in1=oo[:], op=ALU.mult), reads=["oo"], writes=["osq"])
                                    op("pe", lambda h, pa=pa: h.matmul(ps[pa][:, 0:128], ones[:], osq[:], start=True, stop=True), reads=["ones", "osq"], writes=[psk[pa]], inc=True)
                                    op("act", lambda h, pa=pa: h.activation(out=rsd[:], in_=ps[pa][:, 0:128], func=AF.Ln, scale=1.0 / 128, bias=EPS), reads=[psk[pa]], writes=["rsd"])
                                    op("act", lambda h: h.activation(out=rsd[:], in_=rsd[:], func=AF.Exp, scale=-0.5), reads=["rsd"], writes=["rsd"])
                                    op("dve", lambda h: h.tensor_tensor(out=oo[:], in0=oo[:], in1=rsd[:], op=ALU.mult), reads=["oo", "rsd"], writes=["oo"])
                                    op("dve", lambda h, hh=hh, q0=q0: h.tensor_scalar(out=mx[:, hh, q0:q0 + 128], in0=oo[:], scalar1=gcol[:, 0:1], scalar2=(1.0 - lam_init), op0=ALU.mult, op1=ALU.mult),
                                       reads=["oo", "gcol"], writes=[mxk])
                        op("sp", lambda h, b=b, t0=t0: h.dma_start(out=mixT_s[s, :, 2:6, t0:t0 + 512], in_=mixT[b][:]), reads=[mxk], writes=["mixT_s"], dma="st_mx", nowaw=True)
                    P.barrier()
                    phase_ctr[0] += 1
                    if stop is not None and phase_ctr[0] >= stop:
                        P.stopped = True

            with ExitStack() as ph:
                wo = sb("wo", [128, 8, D], BF16, ph)
                wd = sb("wd", [128, NF, D], BF16, ph)
                for c in range(8):
                    op("pool", lambda h, c=c: h.dma_start(out=wo[:, c, :], in_=w_out[l, c * 128:(c + 1) * 128, :]), writes=["wo"], dma="wq", nowaw=True)
                for f in range(NF):
                    op("pool", lambda h, f=f: h.dma_start(out=wd[:, f, :], in_=w_dn[l, f * 128:(f + 1) * 128, :]), writes=["wd"], dma="wq", nowaw=True)
                lnp = sb("lnp", [128, 4, D], F32, ph)
                for k, src in enumerate((ln1g, ln1b, ln2g, ln2b)):
                    op("sp", lambda h, k=k, src=src: h.dma_start(out=lnp[:, k, :], in_=src[l].partition_broadcast(128)), writes=["lnp"], dma="gp", nowaw=True)
                wg = [sb("wg%d" % i, [128, 8, 256], BF16, ph) for i in range(3)]
                mxT = [sb("mxT%d" % i, [128, 8, 512], BF16, ph) for i in range(2)]
                xin = [sb("xr%d" % i, [128, D], F32, ph) for i in range(2)]
                x1 = sb("x1", [128, 4, D], F32, ph)
                x1b = sb("x1b", [128, D], BF16, ph)
                x1T = sb("x1T", [128, 8, 512], BF16, ph)
                actT = sb("actT", [128, NF, 512], BF16, ph)
                sg = [sb("sg%d" % i, [128, 512], F32, ph) for i in range(2)]
                zt = sb("zt", [128, D], F32, ph)
                yo = [sb("yo%d" % i, [128, D], F32, ph) for i in range(2)]
                bst = sb("bst", [128, 16], F32, ph)

                def layer_norm(zin, zkey, gk, bk, out_ap, out_key):
                    op("dve", lambda h: h.bn_stats(out=bst[:, 0:6], in_=zin[:, 0:512]), reads=[zkey], writes=["bst"])
                    op("dve", lambda h: h.bn_stats(out=bst[:, 6:12], in_=zin[:, 512:1024]), reads=[zkey], writes=["bst"])
                    op("dve", lambda h: h.bn_aggr(out=bst[:, 12:14], in_=bst[:, 0:12]), reads=["bst"], writes=["bst2"])
                    op("act", lambda h: h.activation(out=bst[:, 14:15], in_=bst[:, 13:14], func=AF.Ln, bias=EPS), reads=["bst2"], writes=["bst3"])
                    op("act", lambda h: h.activation(out=bst[:, 15:16], in_=bst[:, 14:15], func=AF.Exp, scale=-0.5), reads=["bst3"], writes=["bst4"])
                    op("dve", lambda h: h.tensor_scalar(out=zin[:], in0=zin[:], scalar1=bst[:, 12:13], scalar2=bst[:, 15:16], op0=ALU.subtract, op1=ALU.mult), reads=[zkey, "bst2", "bst4"], writes=[zkey])
                    op("pool", lambda h: h.tensor_tensor(out=zin[:], in0=zin[:], in1=lnp[:, gk, :], op=ALU.mult), reads=[zkey, "lnp"], writes=[zkey])
                    op("dve", lambda h: h.tensor_tensor(out=out_ap, in0=zin[:], in1=lnp[:, bk, :], op=ALU.add), reads=[zkey, "lnp"], writes=[out_key])

                gi = 0
                xc = 0
                wc = 0
                yc = 0
                for s in range(NSEQ):
                    for g in range(NGD):
                        b = gi % 2
                        gi += 1
                        t0 = g * 512
                        op("sp", lambda h, b=b, t0=t0, s=s: h.dma_start(out=mxT[b][:], in_=mixT_s[s, :, :, t0:t0 + 512]), writes=["mxT%d" % b], dma="ldmx%d" % b)
                        for r in range(4):
                            xb = xc % 2
                            xc += 1
                            tt = t0 + r * 128
                            op("sp", lambda h, xb=xb, tt=tt, s=s: h.dma_start(out=xin[xb][:], in_=x_src[s, tt:tt + 128, :]), writes=["xr%d" % xb], dma="ldxr%d" % xb)
                            for hf in range(2):
                                for c in range(8):
                                    op("pe", lambda h, hf=hf, c=c, b=b, r=r: h.matmul(ps[hf][:], mxT[b][:, c, r * 128:(r + 1) * 128], wo[:, c, hf * 512:(hf + 1) * 512], start=(c == 0), stop=(c == 7)),
                                       reads=["mxT%d" % b, "wo"], writes=[psk[hf]], inc=(c == 7))
                                op("dve", lambda h, hf=hf, xb=xb: h.scalar_tensor_tensor(out=zt[:, hf * 512:(hf + 1) * 512], in0=xin[xb][:, hf * 512:(hf + 1) * 512], scalar=ALPHA, in1=ps[hf][:], op0=ALU.mult, op1=ALU.add),
                                   reads=["xr%d" % xb, psk[hf]], writes=["zt"])
                            layer_norm(zt, "zt", 0, 1, x1[:, r, :], "x1_%d" % r)
                            op("act", lambda h, r=r: h.copy(out=x1b[:], in_=x1[:, r, :]), reads=["x1_%d" % r], writes=["x1b"])
                            for c in range(8):
                                op("pe", lambda h, c=c: h.transpose(pst[:, c * 128:(c + 1) * 128], x1b[:, c * 128:(c + 1) * 128], ident[:]), reads=["x1b", "ident"], writes=["pst"], inc=(c == 7))
                            op("act", lambda h, r=r: h.copy(out=x1T[:, :, r * 128:(r + 1) * 128], in_=pst[:].rearrange("p (c t) -> p c t", c=8)), reads=["pst"], writes=["x1T"])
                        for f in range(NF):
                            wb_ = wc % 3
                            wc += 1
                            op("sp", lambda h, wb_=wb_, f=f: h.dma_start(out=wg[wb_][:], in_=wgu_s[l, f]), writes=["wg%d" % wb_], dma="ldwg%d" % wb_)
                            pg, pu = 2 + 2 * (f % 2), 3 + 2 * (f % 2)
                            for c in range(8):
                                op("pe", lambda h, pg=pg, c=c, wb_=wb_: h.matmul(ps[pg][:], wg[wb_][:, c, 0:128], x1T[:, c, :], start=(c == 0), stop=(c == 7)),
                                   reads=["wg%d" % wb_, "x1T"], writes=[psk[pg]], inc=(c == 7))
                            for c in range(8):
                                op("pe", lambda h, pu=pu, c=c, wb_=wb_: h.matmul(ps[pu][:], wg[wb_][:, c, 128:256], x1T[:, c, :], start=(c == 0), stop=(c == 7)),
                                   reads=["wg%d" % wb_, "x1T"], writes=[psk[pu]], inc=(c == 7))
                            sb_ = f % 2
                            op("act", lambda h, pg=pg, sb_=sb_: h.activation(out=sg[sb_][:], in_=ps[pg][:], func=AF.Silu), reads=[psk[pg]], writes=["sg%d" % sb_])
                            op("dve", lambda h, pu=pu, sb_=sb_, f=f: h.tensor_tensor(out=actT[:, f, :], in0=sg[sb_][:], in1=ps[pu][:], op=ALU.mult), reads=["sg%d" % sb_, psk[pu]], writes=["actT"])
                        for r in range(4):
                            tt = t0 + r * 128
                            for hf in range(2):
                                for f in range(NF):
                                    op("pe", lambda h, hf=hf, f=f, r=r: h.matmul(ps[hf][:], actT[:, f, r * 128:(r + 1) * 128], wd[:, f, hf * 512:(hf + 1) * 512], start=(f == 0), stop=(f == NF - 1)),
                                       reads=["actT", "wd"], writes=[psk[hf]], inc=(f == NF - 1))
                                op("dve", lambda h, hf=hf, r=r: h.scalar_tensor_tensor(out=zt[:, hf * 512:(hf + 1) * 512], in0=x1[:, r, hf * 512:(hf + 1) * 512], scalar=ALPHA, in1=ps[hf][:], op0=ALU.mult, op1=ALU.add),
                                   reads=["x1_%d" % r, psk[hf]], writes=["zt"])
                            yb = yc % 2
                            yc += 1
                            layer_norm(zt, "zt", 2, 3, yo[yb][:], "yo%d" % yb)
                            op("sp", lambda h, yb=yb, tt=tt, s=s: h.dma_start(out=x_dst[s, tt:tt + 128, :], in_=yo[yb][:]), reads=["yo%d" % yb], writes=["xdst"], dma="st_y", nowaw=True)
                P.barrier()
                phase_ctr[0] += 1
                if stop is not None and phase_ctr[0] >= stop:
                    P.stopped = True
        except _Stop:
            pass
        P.final_wait("sp")
    return nc


def _consts():
    ident = np.eye(128, dtype=np.float32)
    so = np.arange(128)[:, None]
    to = np.arange(128)[None, :]
    diag = np.zeros((128, 4, 128), np.float32)
    for h in range(4):
        v = (-np.abs(to - so) + (to - 128)).astype(np.float32) * SLOPES[h] * 8.0
        v = np.where((so // 64) > (to // 64), NEG * 8.0, v)
        diag[:, h, :] = v
    alibi = np.zeros((128, 4 * 34), np.float32)
    for h in range(4):
        for k in range(34):
            alibi[:, h * 34 + k] = SLOPES[h] * (np.arange(128) - 128.0 * k)
    cmask = np.where((to // 64) > (so // 64), -1e30, 0.0).astype(np.float32)
    gmask = ((so // 64) <= (to // 64)).astype(np.float32)
    jpos = np.tile(np.arange(1, 33, dtype=np.float32)[None, :], (128, 1))
    return ident, diag, alibi, cmask, gmask, jpos


_CACHE = {}


def kernel(**inputs):
    n = 8
    f = lambda a: np.ascontiguousarray(np.asarray(a, dtype=np.float32))
    x = f(inputs["x"])
    ident, diag, alibi, cmask, gmask, jpos = _consts()
    lamv = np.stack([f(inputs["lam_q1"]), f(inputs["lam_k1"]), f(inputs["lam_q2"]), f(inputs["lam_k2"])], axis=1)
    shared = {
        "w_in": f(inputs["w_in"]),
        "gmlp_w_s": f(inputs["gmlp_w_s"]),
        "gmlp_b_s": f(inputs["gmlp_b_s"]),
        "gmlp_ln_g": f(inputs["gmlp_ln_g"]).reshape(DEPTH, 1, 256),
        "gmlp_ln_b": f(inputs["gmlp_ln_b"]).reshape(DEPTH, 1, 256),
        "lamv": np.ascontiguousarray(lamv),
        "diff_subln_g": f(inputs["diff_subln_g"]).reshape(DEPTH, 128, 1),
        "w_out": f(inputs["w_out"]),
        "ln1_g": f(inputs["ln1_g"]).reshape(DEPTH, 1, D),
        "ln1_b": f(inputs["ln1_b"]).reshape(DEPTH, 1, D),
        "w_gu": f(inputs["w_gu"]),
        "w_down": f(inputs["w_down"]),
        "ln2_g": f(inputs["ln2_g"]).reshape(DEPTH, 1, D),
        "ln2_b": f(inputs["ln2_b"]).reshape(DEPTH, 1, D),
        "c_ident": ident, "c_diag": diag, "c_alibi": alibi, "c_cmask": cmask, "c_gmask": gmask, "c_jpos": jpos,
    }
    if "nc" not in _CACHE:
        _CACHE["nc"] = build_program()
    nc = _CACHE["nc"]
    in_maps = []
    for c in range(n):
        m = dict(shared)
        m["x"] = np.ascontiguousarray(x[NSEQ * c:NSEQ * (c + 1)])
        in_maps.append(m)
    res = run_bass_kernel_spmd(nc, in_maps, core_ids=list(range(n)))
    out = np.concatenate([np.asarray(r["y"], dtype=np.float32) for r in res.results], axis=0)
    return out
```

```python
import math
from contextlib import ExitStack
import numpy as np
import concourse.bass as bass
import concourse.mybir as mybir
from concourse.bass_utils import run_bass_kernel_spmd

F32 = mybir.dt.float32
BF16 = mybir.dt.bfloat16
AF = mybir.ActivationFunctionType
ALU = mybir.AluOpType
AX = mybir.AxisListType

D = 1024
S = 4096
DEPTH = 2
NSEQ = 2
NT = S // 128
NG = S // 512
FF = 2816
NF = FF // 128
IN_W = 3112
OFF_A_U, OFF_A_V, OFF_B_Q, OFF_B_K, OFF_B_V = 0, 256, 512, 1024, 1536
OFF_C_Q, OFF_C_K, OFF_C_V, OFF_I_Q, OFF_I_K, OFF_I_W = 2048, 2304, 2560, 2816, 3072, 3104
ALPHA = (2 * DEPTH) ** 0.25
EPS = 1e-5
SLOPES = [2.0 ** (-8.0 * (h + 1) / 4) for h in range(4)]
WIN = [2, 9, 31, 31]
NBIS = 18
BIS_LO, BIS_W = -8.0, 16.0
ACCB = [0, 1, 5, 6]
TOPK = 256
NEG = -30000.0
IDX_SCALE = (8 ** -0.5) * (32 ** -0.5)


class Tok:
    __slots__ = ("sem", "val")

    def __init__(self, sem, val):
        self.sem = sem
        self.val = val


class DSem:
    def __init__(self, h):
        self.h = h
        self.total = 0


class Eng:
    def __init__(self, name, handle, sem):
        self.name = name
        self.h = handle
        self.sem = sem
        self.count = 0
        self.waited = {}


class Prog:
    def __init__(self, nc, es):
        self.nc = nc
        self.es = es
        self.eng = {}
        for name, h in (("pe", nc.tensor), ("act", nc.scalar), ("dve", nc.vector), ("pool", nc.gpsimd), ("sp", nc.sync)):
            self.eng[name] = Eng(name, h, es.enter_context(nc.semaphore("sem_" + name)))
        self.dsems = {}
        self.last_w = {}
        self.readers = {}
        self.stopped = False

    def dsem(self, name):
        if name not in self.dsems:
            self.dsems[name] = DSem(self.es.enter_context(self.nc.semaphore("dma_" + name)))
        return self.dsems[name]

    def _wait(self, e, tok):
        if isinstance(tok.sem, DSem):
            sem, val = tok.sem.h, tok.sem.total
            key = ("d", id(tok.sem))
        else:
            if tok.sem is e and e.name == "pe":
                return
            sem, val = tok.sem.sem, tok.val
            key = ("e", tok.sem.name)
        if e.waited.get(key, 0) >= val:
            return
        e.h.wait_ge(sem, val)
        e.waited[key] = val

    def op(self, eng, fn, reads=(), writes=(), inc=True, dma=None, nowaw=False):
        if self.stopped:
            return None
        e = self.eng[eng]
        deps = []
        for k in reads:
            t = self.last_w.get(k)
            if t is not None:
                deps.append(t)
        for k in writes:
            t = self.last_w.get(k)
            if t is not None and not nowaw:
                deps.append(t)
            deps.extend(self.readers.get(k, ()))
        for t in deps:
            self._wait(e, t)
        ins = fn(e.h)
        if dma is not None:
            ds = self.dsem(dma)
            ds.total += 16
            ins.then_inc(ds.h, 16)
            tok = Tok(ds, None)
        else:
            if inc:
                e.count += 1
                ins.then_inc(e.sem, 1)
                tok = Tok(e, e.count)
            else:
                tok = Tok(e, e.count + 1)
        for k in reads:
            self.readers.setdefault(k, []).append(tok)
            if len(self.readers[k]) > 24:
                self.readers[k] = self.readers[k][-24:] if False else self.readers[k]
        for k in writes:
            self.last_w[k] = tok
            self.readers[k] = []
        return ins

    def barrier(self):
        if self.stopped:
            return
        names = list(self.eng)
        for n in names:
            e = self.eng[n]
            for m in names:
                if m != n and self.eng[m].count > 0:
                    self._wait(e, Tok(self.eng[m], self.eng[m].count))
            for ds in self.dsems.values():
                if ds.total > 0:
                    self._wait(e, Tok(ds, None))
        self.last_w.clear()
        self.readers.clear()

    def final_wait(self, eng):
        e = self.eng[eng]
        for m in self.eng:
            if m != eng and self.eng[m].count > 0:
                self._wait(e, Tok(self.eng[m], self.eng[m].count))
        for ds in self.dsems.values():
            if ds.total > 0:
                self._wait(e, Tok(ds, None))


class _Stop(Exception):
    pass


def build_program(depth=DEPTH, debug=False, stop=None, dbg_groups=None):
    NGD = NG if dbg_groups is None else dbg_groups
    nc = bass.Bass("TRN2", target_bir_lowering=False)
    dt = nc.dram_tensor

    def din(name, shape):
        return dt(name, list(shape), F32, kind="ExternalInput").ap()

    x_in = din("x", [NSEQ, S, D])
    w_in = din("w_in", [DEPTH, D, IN_W])
    w_s = din("gmlp_w_s", [DEPTH, 4, 128, 128])
    b_s = din("gmlp_b_s", [DEPTH, 4, 128])
    a_g = din("gmlp_ln_g", [DEPTH, 1, 256])
    a_b = din("gmlp_ln_b", [DEPTH, 1, 256])
    lamv = din("lamv", [DEPTH, 4, 64])
    subg = din("diff_subln_g", [DEPTH, 128, 1])
    w_out = din("w_out", [DEPTH, D, D])
    ln1g = din("ln1_g", [DEPTH, 1, D])
    ln1b = din("ln1_b", [DEPTH, 1, D])
    w_gu = din("w_gu", [DEPTH, D, 2 * FF])
    w_dn = din("w_down", [DEPTH, FF, D])
    ln2g = din("ln2_g", [DEPTH, 1, D])
    ln2b = din("ln2_b", [DEPTH, 1, D])
    c_ident = din("c_ident", [128, 128])
    c_diag = din("c_diag", [128, 4, 128])
    c_alibi = din("c_alibi", [128, 4 * 34])
    c_cmask = din("c_cmask", [128, 128])
    c_gmask = din("c_gmask", [128, 128])
    c_jpos = din("c_jpos", [128, 32])
    y_out = dt("y", [NSEQ, S, D], F32, kind="ExternalOutput").ap()

    skind = "ExternalOutput" if debug else "Internal"
    xs = dt("xs", [NSEQ, S, D], F32, kind=skind).ap()
    xT_s = dt("xT_s", [NSEQ, 128, 8, S], BF16, kind=skind).ap()
    ktb_s = dt("ktb_s", [NSEQ, 128, 4, S], BF16, kind=skind).ap()
    ktc_s = dt("ktc_s", [NSEQ, 128, 2, S], BF16, kind=skind).ap()
    ki4_s = dt("ki4_s", [NSEQ, 128, S], BF16, kind=skind).ap()
    vb_s = dt("vb_s", [NSEQ, 128, NT, 512], BF16, kind=skind).ap()
    vc_s = dt("vc_s", [NSEQ, 128, NT, 256], BF16, kind=skind).ap()
    mixT_s = dt("mixT_s", [NSEQ, 128, 8, S], BF16, kind=skind).ap()
    wgu_s = dt("wgu_s", [DEPTH, NF, 128, 8, 256], BF16, kind="Internal").ap()

    with ExitStack() as es:
        E = es.enter_context
        P = Prog(nc, es)
        op = P.op
        dbgbuf = dt("dbgbuf", [128, 16384], F32, kind="ExternalOutput").ap() if debug else None
        dbgpos = {}

        def dump(name, ap2d, ncols, reads, cond=True):
            if not debug or not cond or name in dbgpos:
                return
            off = sum(v[1] for v in dbgpos.values())
            dbgpos[name] = (off, ncols)
            op("pool", lambda h: h.dma_start(out=dbgbuf[0:ap2d.shape[0], off:off + ncols], in_=ap2d), reads=reads, writes=["dbgbuf"], dma="dbg", nowaw=True)
        build_program.dbgpos = dbgpos

        uid = [0]

        def sb(name, shape, dtype, stack=es):
            uid[0] += 1
            return stack.enter_context(nc.sbuf_tensor("%s_%d" % (name, uid[0]), list(shape), dtype))

        ident = sb("ident", [128, 128], BF16)
        ones = sb("ones", [128, 128], BF16)
        diag = sb("diag", [128, 4, 128], BF16)
        alibi = sb("alibi", [128, 4 * 34], F32)
        cmask = sb("cmask", [128, 128], F32)
        gmask = sb("gmask", [128, 128], F32)
        op("pool", lambda h: h.dma_start(out=ident[:], in_=c_ident), writes=["ident"], dma="c0")
        op("pool", lambda h: h.dma_start(out=diag[:], in_=c_diag), writes=["diag"], dma="c0")
        op("sp", lambda h: h.dma_start(out=alibi[:], in_=c_alibi), writes=["alibi"], dma="c1")
        op("sp", lambda h: h.dma_start(out=cmask[:], in_=c_cmask), writes=["cmask"], dma="c1")
        op("sp", lambda h: h.dma_start(out=gmask[:], in_=c_gmask), writes=["gmask"], dma="c1")
        op("dve", lambda h: h.memset(ones[:], 1.0), writes=["ones"])
        jpos1 = sb("jpos1", [128, 32], F32)
        slmat = sb("slmat", [128, 4, 128], BF16)
        op("sp", lambda h: h.dma_start(out=jpos1[:], in_=c_jpos), writes=["jpos1"], dma="c1")
        for hh in range(4):
            op("dve", lambda h, hh=hh: h.memset(slmat[:, hh, :], 8.0 * SLOPES[hh]), writes=["slmat"])

        ps = [E(nc.psum_tensor("ps%d" % i, [128, 512], F32)) for i in range(7)]
        pst = E(nc.psum_tensor("pst", [128, 1024], BF16))
        psk = ["ps%d" % i for i in range(7)]

        for l in range(depth):
            for f in range(NF):
                for half in range(2):
                    src = w_gu[l, :, half * FF + f * 128: half * FF + (f + 1) * 128].rearrange("(c p) j -> p c j", p=128)
                    op("pool", lambda h, src=src, f=f, half=half, l=l: h.dma_start(
                        out=wgu_s[l, f, :, :, half * 128:(half + 1) * 128], in_=src),
                        writes=["wgu_s"], dma="wgucvt", nowaw=True)
        P.barrier()

        phase_ctr = [0]
        if stop == 0:
            depth_iter = []
        else:
            depth_iter = range(depth)
        try:
          for l in depth_iter:
            lam_init = 0.8 - 0.6 * math.exp(-0.3 * l)
            x_src = x_in if l == 0 else xs
            x_dst = y_out if l == depth - 1 else xs
            for s in range(NSEQ):
                with ExitStack() as ph:
                    wk = sb("wk", [128, 8, 1664], BF16, ph)
                    for c in range(8):
                        rows = w_in[l, c * 128:(c + 1) * 128, :]
                        op("pool", lambda h, c=c, rows=rows: h.dma_start(out=wk[:, c, 0:512], in_=rows[:, OFF_B_K:OFF_B_K + 512]), writes=["wk"], dma="wk", nowaw=True)
                        op("pool", lambda h, c=c, rows=rows: h.dma_start(out=wk[:, c, 512:768], in_=rows[:, OFF_C_K:OFF_C_K + 256]), writes=["wk"], dma="wk", nowaw=True)
                        for r in range(4):
                            op("pool", lambda h, c=c, rows=rows, r=r: h.dma_start(out=wk[:, c, 768 + 32 * r:800 + 32 * r], in_=rows[:, OFF_I_K:OFF_I_K + 32]), writes=["wk"], dma="wk", nowaw=True)
                        op("pool", lambda h, c=c, rows=rows: h.dma_start(out=wk[:, c, 896:1408], in_=rows[:, OFF_B_V:OFF_B_V + 512]), writes=["wk"], dma="wk", nowaw=True)
                        op("pool", lambda h, c=c, rows=rows: h.dma_start(out=wk[:, c, 1408:1664], in_=rows[:, OFF_C_V:OFF_C_V + 256]), writes=["wk"], dma="wk", nowaw=True)
                    xin = [sb("xin%d" % i, [128, 4, D], F32, ph) for i in range(2)]
                    xbf = [sb("xbf%d" % i, [128, 4, D], BF16, ph) for i in range(2)]
                    xT = [sb("xT%d" % i, [128, 8, 512], BF16, ph) for i in range(2)]
                    kst = [sb("kst%d" % i, [128, 7, 512], BF16, ph) for i in range(2)]
                    vst = [sb("vst%d" % i, [128, 4, 768], BF16, ph) for i in range(2)]
                    cnt = 0
                    for g in range(NGD):
                        b = g % 2
                        t0 = g * 512
                        op("sp", lambda h, b=b, t0=t0: h.dma_start(out=xin[b][:], in_=x_src[s, t0:t0 + 512, :].rearrange("(r p) d -> p r d", p=128)),
                           writes=["xin%d" % b], dma="xin%d" % b)
                        op("dve" if g % 2 == 0 else "pool", lambda h, b=b: h.tensor_copy(out=xbf[b][:], in_=xin[b][:]), reads=["xin%d" % b], writes=["xbf%d" % b])
                        for r in range(4):
                            for c in range(8):
                                op("pe", lambda h, b=b, r=r, c=c: h.transpose(pst[:, c * 128:(c + 1) * 128], xbf[b][:, r, c * 128:(c + 1) * 128], ident[:]),
                                   reads=["xbf%d" % b, "ident"], writes=["pst"], inc=(c == 7))
                            op("act" if r % 2 == 0 else "dve",
                               (lambda h, b=b, r=r: h.copy(out=xT[b][:, :, r * 128:(r + 1) * 128], in_=pst[:].rearrange("p (c t) -> p c t", c=8))) if r % 2 == 0 else
                               (lambda h, b=b, r=r: h.tensor_copy(out=xT[b][:, :, r * 128:(r + 1) * 128], in_=pst[:].rearrange("p (c t) -> p c t", c=8))),
                               reads=["pst"], writes=["xT%d" % b])
                        op("sp", lambda h, b=b, t0=t0: h.dma_start(out=xT_s[s, :, :, t0:t0 + 512], in_=xT[b][:]), reads=["xT%d" % b], writes=["xT_s"], dma="st_xT", nowaw=True)
                        for cc in range(7):
                            pk = cnt % 6
                            cnt += 1
                            for c in range(8):
                                op("pe", lambda h, pk=pk, cc=cc, c=c, b=b: h.matmul(ps[pk][:], wk[:, c, cc * 128:(cc + 1) * 128], xT[b][:, c, :], start=(c == 0), stop=(c == 7)),
                                   reads=["wk", "xT%d" % b], writes=[psk[pk]], inc=(c == 7))
                            if cc % 2 == 0:
                                op("act", lambda h, pk=pk, cc=cc, b=b: h.copy(out=kst[b][:, cc, :], in_=ps[pk][:]), reads=[psk[pk]], writes=["kst%d" % b])
                            else:
                                op("dve", lambda h, pk=pk, cc=cc, b=b: h.tensor_copy(out=kst[b][:, cc, :], in_=ps[pk][:]), reads=[psk[pk]], writes=["kst%d" % b])
                        op("sp", lambda h, b=b, t0=t0: h.dma_start(out=ktb_s[s, :, :, t0:t0 + 512], in_=kst[b][:, 0:4, :]), reads=["kst%d" % b], writes=["ktb_s"], dma="st_k", nowaw=True)
                        op("sp", lambda h, b=b, t0=t0: h.dma_start(out=ktc_s[s, :, :, t0:t0 + 512], in_=kst[b][:, 4:6, :]), reads=["kst%d" % b], writes=["ktc_s"], dma="st_k", nowaw=True)
                        op("sp", lambda h, b=b, t0=t0: h.dma_start(out=ki4_s[s, :, t0:t0 + 512], in_=kst[b][:, 6, :]), reads=["kst%d" % b], writes=["ki4_s"], dma="st_k", nowaw=True)
                        for r in range(4):
                            pk = cnt % 6
                            cnt += 1
                            for c in range(8):
                                op("pe", lambda h, pk=pk, r=r, c=c, b=b: h.matmul(ps[pk][:], xT[b][:, c, r * 128:(r + 1) * 128], wk[:, c, 896:1408], start=(c == 0), stop=(c == 7)),
                                   reads=["wk", "xT%d" % b], writes=[psk[pk]], inc=(c == 7))
                            op("act", lambda h, pk=pk, r=r, b=b: h.copy(out=vst[b][:, r, 0:512], in_=ps[pk][:]), reads=[psk[pk]], writes=["vst%d" % b])
                            pk = cnt % 6
                            cnt += 1
                            for c in range(8):
                                op("pe", lambda h, pk=pk, r=r, c=c, b=b: h.matmul(ps[pk][:, 0:256], xT[b][:, c, r * 128:(r + 1) * 128], wk[:, c, 1408:1664], start=(c == 0), stop=(c == 7)),
                                   reads=["wk", "xT%d" % b], writes=[psk[pk]], inc=(c == 7))
                            op("dve", lambda h, pk=pk, r=r, b=b: h.tensor_copy(out=vst[b][:, r, 512:768], in_=ps[pk][:, 0:256]), reads=[psk[pk]], writes=["vst%d" % b])
                        op("sp", lambda h, b=b, g=g: h.dma_start(out=vb_s[s, :, 4 * g:4 * g + 4, :], in_=vst[b][:, :, 0:512]), reads=["vst%d" % b], writes=["vb_s"], dma="st_v", nowaw=True)
                        op("sp", lambda h, b=b, g=g: h.dma_start(out=vc_s[s, :, 4 * g:4 * g + 4, :], in_=vst[b][:, :, 512:768]), reads=["vst%d" % b], writes=["vc_s"], dma="st_v", nowaw=True)
                    P.barrier()
                    phase_ctr[0] += 1
                    if stop is not None and phase_ctr[0] >= stop:
                        P.stopped = True

                with ExitStack() as ph:
                    ktc = sb("ktc", [128, 2, S], BF16, ph)
                    vc = sb("vc", [128, NT, 256], BF16, ph)
                    ki4 = sb("ki4", [128, S], BF16, ph)
                    op("sp", lambda h: h.dma_start(out=ktc[:], in_=ktc_s[s]), writes=["ktc"], dma="ldkv")
                    op("sp", lambda h: h.dma_start(out=vc[:], in_=vc_s[s]), writes=["vc"], dma="ldkv")
                    op("sp", lambda h: h.dma_start(out=ki4[:], in_=ki4_s[s]), writes=["ki4"], dma="ldkv")
                    wq = sb("wq", [128, 8, 768], BF16, ph)
                    wv = sb("wv", [128, 8, 264], BF16, ph)
                    for c in range(8):
                        rows = w_in[l, c * 128:(c + 1) * 128, :]
                        op("pool", lambda h, c=c, rows=rows: h.dma_start(out=wq[:, c, 0:256], in_=rows[:, OFF_A_U:OFF_A_U + 256]), writes=["wq"], dma="wq", nowaw=True)
                        op("pool", lambda h, c=c, rows=rows: h.dma_start(out=wq[:, c, 256:512], in_=rows[:, OFF_C_Q:OFF_C_Q + 256]), writes=["wq"], dma="wq", nowaw=True)
                        op("pool", lambda h, c=c, rows=rows: h.dma_start(out=wq[:, c, 512:768], in_=rows[:, OFF_I_Q:OFF_I_Q + 256]), writes=["wq"], dma="wq", nowaw=True)
                        op("pool", lambda h, c=c, rows=rows: h.dma_start(out=wv[:, c, 0:256], in_=rows[:, OFF_A_V:OFF_A_V + 256]), writes=["wv"], dma="wq", nowaw=True)
                        op("pool", lambda h, c=c, rows=rows: h.dma_start(out=wv[:, c, 256:264], in_=rows[:, OFF_I_W:OFF_I_W + 8]), writes=["wv"], dma="wq", nowaw=True)
                    wmT = sb("wmT", [128, 4, 128], BF16, ph)
                    wmf = sb("wmf", [128, 4, 128], F32, ph)
                    bsT = sb("bsT", [128, 2, 128], F32, ph)
                    lng = sb("lng", [128, 256], F32, ph)
                    lnb = sb("lnb", [128, 256], F32, ph)
                    wmb = sb("wmb", [128, 4, 128], BF16, ph)
                    op("sp", lambda h: h.dma_start(out=wmf[:], in_=w_s[l].rearrange("g t s -> t g s")), writes=["wmf"], dma="gpw")
                    op("dve", lambda h: h.tensor_copy(out=wmb[:], in_=wmf[:]), reads=["wmf"], writes=["wmb"])
                    for gg in range(4):
                        op("pe", lambda h, gg=gg: h.transpose(pst[:, gg * 128:(gg + 1) * 128], wmb[:, gg, :], ident[:]), reads=["wmb", "ident"], writes=["pst"], inc=(gg == 3))
                    for gg in range(4):
                        op("sp", lambda h, gg=gg: h.dma_start(out=bsT[(gg % 2) * 64:(gg % 2) * 64 + 64, gg // 2, :], in_=b_s[l, gg:gg + 1, :].partition_broadcast(64)),
                           writes=["bsT"], dma="gp", nowaw=True)
                    op("sp", lambda h: h.dma_start(out=lng[:], in_=a_g[l].partition_broadcast(128)), writes=["lng"], dma="gp")
                    op("sp", lambda h: h.dma_start(out=lnb[:], in_=a_b[l].partition_broadcast(128)), writes=["lnb"], dma="gp")
                    for gg in range(4):
                        op("dve", lambda h, gg=gg: h.tensor_tensor(out=wmT[:, gg, :], in0=pst[:, gg * 128:(gg + 1) * 128], in1=gmask[:], op=ALU.mult), reads=["pst", "gmask"], writes=["wmT"])

                    xT = [sb("xTa%d" % i, [128, 8, 512], BF16, ph) for i in range(2)]
                    uT = sb("uT", [128, 2, 512], BF16, ph)
                    qcp = sb("qcp", [128, 4, 512], BF16, ph)
                    qip = sb("qip", [128, 8, 512], BF16, ph)
                    vtm = sb("vtm", [128, 264], F32, ph)
                    vg = sb("vg", [128, 256], F32, ph)
                    vsq = sb("vsq", [128, 256], F32, ph)
                    st4 = sb("st4", [128, 16], F32, ph)
                    vn = sb("vn", [128, 256], BF16, ph)
                    wab = sb("wab", [128, 8], F32, ph)
                    sgn = sb("sgn", [128, 8], F32, ph)
                    dsg = sb("dsg", [128, 8, 128], BF16, ph)
                    scoreb = [sb("score%d" % i_, [128, S], F32, ph) for i_ in range(2)]
                    cmb = sb("cmb", [128, 128], BF16, ph)
                    op("pool", lambda h: h.dma_start(out=cmb[:], in_=c_cmask), writes=["cmb"], dma="gpc")
                    mb = sb("mb", [128, S], BF16, ph)
                    mbT = sb("mbT", [128, NT, 128], BF16, ph)
                    rl = [sb("rl%d" % i, [128, 512], BF16, ph) for i in range(4)]
                    pT = [sb("pTa%d" % i, [128, 4, 128], BF16, ph) for i in range(3)]
                    bis = sb("bis", [128, 8], F32, ph)
                    mixT = [sb("mixa%d" % i, [128, 4, 512], BF16, ph) for i in range(2)]
                    tmpa = sb("tmpa", [128, 128], F32, ph)
                    rcp = sb("rcp", [128, 128], F32, ph)
                    qcp2 = sb("qcp2", [128, 4, 512], BF16, ph)
                    op("dve", lambda h: h.memset(qcp[:], 0.0), writes=["qcp0"])
                    op("dve", lambda h: h.memset(qcp2[:], 0.0), writes=["qcp1"])
                    op("pool", lambda h: h.memset(qip[:], 0.0), writes=["qip"])
                    op("pool", lambda h: h.memset(mbT[:], 0.0), writes=["mbT"])
                    dblk = sb("dblk", [128, 128], BF16, ph)
                    dT = sb("dT", [128, 128], BF16, ph)
                    anys = sb("anys", [128, 64], F32, ph)
                    op("dve", lambda h: h.memset(dblk[:], 0.0), writes=["dblk"])
                    op("dve", lambda h: h.memset(dT[:], 0.0), writes=["dT"])

                    rlc = 0
                    ptc = 0
                    qcpb = [qcp, qcp2]

                    def prologue(g):
                        nonlocal rlc, ptc
                        b = g % 2
                        t0 = g * 512
                        mx = mixT[b]
                        mxk = "mixa%d" % b
                        op("sp", lambda h, b=b, t0=t0: h.dma_start(out=xT[b][:], in_=xT_s[s, :, :, t0:t0 + 512]), writes=["xTa%d" % b], dma="ldxT%d" % b)
                        for cc in range(6):
                            pk = cc % 2
                            for c in range(8):
                                op("pe", lambda h, pk=pk, cc=cc, c=c, b=b: h.matmul(ps[pk][:], wq[:, c, cc * 128:(cc + 1) * 128], xT[b][:, c, :], start=(c == 0), stop=(c == 7)),
                                   reads=["wq", "xTa%d" % b], writes=[psk[pk]], inc=(c == 7))
                            if cc < 2:
                                op("act", lambda h, pk=pk, cc=cc: h.activation(out=uT[:, cc, :], in_=ps[pk][:], func=AF.Gelu_apprx_tanh), reads=[psk[pk]], writes=["uT"])
                            elif cc < 4:
                                for k in range(2):
                                    hh = (cc - 2) * 2 + k
                                    op("dve", lambda h, pk=pk, hh=hh, k=k: h.tensor_copy(out=qcpb[g % 2][64 * k:64 * k + 64, hh, :], in_=ps[pk][64 * k:64 * k + 64, :]), reads=[psk[pk]], writes=["qcp%d" % (g % 2)])
                            else:
                                for k in range(4):
                                    hh = (cc - 4) * 4 + k
                                    op("dve" if k % 2 == 0 else "act",
                                       (lambda h, pk=pk, hh=hh, k=k: h.tensor_copy(out=qip[32 * k:32 * k + 32, hh, :], in_=ps[pk][32 * k:32 * k + 32, :])) if k % 2 == 0 else
                                       (lambda h, pk=pk, hh=hh, k=k: h.copy(out=qip[32 * k:32 * k + 32, hh, :], in_=ps[pk][32 * k:32 * k + 32, :])),
                                       reads=[psk[pk]], writes=["qip"])

                    def stageA1a(g, r):
                        nonlocal rlc, ptc
                        if True:
                            b = g % 2
                            t0 = g * 512
                            mx = mixT[b]
                            mxk = "mixa%d" % b
                            i = 4 * g + r
                            q0 = r * 128
                            nk = 128 * (i + 1)
                            for c in range(8):
                                op("pe", lambda h, c=c, b=b, q0=q0: h.matmul(ps[4][:, 0:264], xT[b][:, c, q0:q0 + 128], wv[:, c, :], start=(c == 0), stop=(c == 7)),
                                   reads=["wv", "xTa%d" % b], writes=[psk[4]], inc=(c == 7))
                            op("act", lambda h: h.activation(out=vg[:], in_=ps[4][:, 0:256], func=AF.Gelu_apprx_tanh), reads=[psk[4]], writes=["vg"])
                            op("dve", lambda h: h.tensor_copy(out=vtm[:, 256:264], in_=ps[4][:, 256:264]), reads=[psk[4]], writes=["vtm"])
                            op("dve", lambda h: h.tensor_scalar(out=wab[:], in0=vtm[:, 256:264], scalar1=-1.0, scalar2=None, op0=ALU.mult), reads=["vtm"], writes=["wab"])
                            op("dve", lambda h: h.tensor_tensor(out=wab[:], in0=wab[:], in1=vtm[:, 256:264], op=ALU.max), reads=["vtm", "wab"], writes=["wab"])
                            op("dve", lambda h: h.tensor_scalar(out=wab[:], in0=wab[:], scalar1=IDX_SCALE, scalar2=None, op0=ALU.mult), reads=["wab"], writes=["wab"])
                            op("dve", lambda h: h.tensor_scalar(out=sgn[:], in0=vtm[:, 256:264], scalar1=0.0, scalar2=2.0, op0=ALU.is_ge, op1=ALU.mult), reads=["vtm"], writes=["sgn"])
                            op("dve", lambda h: h.tensor_scalar(out=sgn[:], in0=sgn[:], scalar1=-1.0, scalar2=None, op0=ALU.add), reads=["sgn"], writes=["sgn"])
                            for hh in range(8):
                                op("dve", lambda h, hh=hh: h.tensor_scalar(out=dsg[:, hh, :], in0=ident[:], scalar1=sgn[:, hh:hh + 1], scalar2=None, op0=ALU.mult),
                                   reads=["ident", "sgn"], writes=["dsg%d" % hh])
                            v3 = vg[:].rearrange("p (g c) -> p g c", c=64)
                            op("dve", lambda h: h.tensor_reduce(out=st4[:, 0:4], in_=v3, axis=AX.X, op=ALU.add), reads=["vg"], writes=["st4"])
                            op("dve", lambda h: h.tensor_tensor(out=vsq[:], in0=vg[:], in1=vg[:], op=ALU.mult), reads=["vg"], writes=["vsq"])
                            op("dve", lambda h: h.tensor_reduce(out=st4[:, 4:8], in_=vsq[:].rearrange("p (g c) -> p g c", c=64), axis=AX.X, op=ALU.add), reads=["vsq"], writes=["st4"])
                            op("dve", lambda h: h.tensor_scalar(out=st4[:, 0:4], in0=st4[:, 0:4], scalar1=1.0 / 64, scalar2=None, op0=ALU.mult), reads=["st4"], writes=["st4"])
                            op("dve", lambda h: h.tensor_tensor(out=st4[:, 8:12], in0=st4[:, 0:4], in1=st4[:, 0:4], op=ALU.mult), reads=["st4"], writes=["st4"])
                            op("dve", lambda h: h.scalar_tensor_tensor(out=st4[:, 4:8], in0=st4[:, 4:8], scalar=1.0 / 64, in1=st4[:, 8:12], op0=ALU.mult, op1=ALU.subtract), reads=["st4"], writes=["st4"])
                            op("act", lambda h: h.activation(out=st4[:, 8:12], in_=st4[:, 4:8], func=AF.Ln, bias=EPS), reads=["st4"], writes=["st4"])
                            op("act", lambda h: h.activation(out=st4[:, 12:16], in_=st4[:, 8:12], func=AF.Exp, scale=-0.5), reads=["st4"], writes=["st4"])
                            for gg in range(4):
                                op("dve", lambda h, gg=gg: h.tensor_scalar(out=vsq[:, gg * 64:(gg + 1) * 64], in0=vg[:, gg * 64:(gg + 1) * 64], scalar1=st4[:, gg:gg + 1], scalar2=st4[:, 12 + gg:13 + gg],
                                                                    op0=ALU.subtract, op1=ALU.mult), reads=["vg", "st4"], writes=["vsq"])
                            op("dve", lambda h: h.tensor_tensor(out=vsq[:], in0=vsq[:], in1=lng[:], op=ALU.mult), reads=["vsq", "lng"], writes=["vsq"])
                            op("dve", lambda h: h.tensor_tensor(out=vn[:], in0=vsq[:], in1=lnb[:], op=ALU.add), reads=["vsq", "lnb"], writes=["vn"])
                            for gg in range(4):
                                ck = gg // 2
                                op("pe", lambda h, gg=gg, ck=ck: h.matmul(ps[5][:, (gg % 2) * 128:(gg % 2) * 128 + 128], vn[:, ck * 128:(ck + 1) * 128], wmT[:, gg, :], start=True, stop=True),
                                   reads=["vn", "wmT"], writes=[psk[5]], inc=True)
                                rs = slice((gg % 2) * 64, (gg % 2) * 64 + 64)
                                op("dve", lambda h, gg=gg, ck=ck, rs=rs: h.tensor_tensor(out=tmpa[rs, :], in0=ps[5][rs, (gg % 2) * 128:(gg % 2) * 128 + 128], in1=bsT[rs, ck, :], op=ALU.add),
                                   reads=[psk[5], "bsT"], writes=["tmpa"])
                                op("dve", lambda h, gg=gg, ck=ck, rs=rs, q0=q0: h.tensor_tensor(out=mx[rs, ck, q0:q0 + 128], in0=tmpa[rs, :], in1=uT[rs, ck, q0:q0 + 128], op=ALU.mult),
                                   reads=["tmpa", "uT"], writes=[mxk])

                    def stageA1b(g, r):
                        nonlocal rlc, ptc
                        if True:
                            b = g % 2
                            t0 = g * 512
                            mx = mixT[b]
                            mxk = "mixa%d" % b
                            i = 4 * g + r
                            q0 = r * 128
                            nk = 128 * (i + 1)
                            score = scoreb[i % 2]
                            sck = "score%d" % (i % 2)
                            nk = 128 * (i + 1)
                            if i >= 2:
                                for c0 in range(0, nk, 512):
                                    cw = min(512, nk - c0)
                                    def emit_L(hh, c0=c0, cw=cw, q0=q0):
                                        pk = 4 + hh % 2
                                        op("pe", lambda h: h.matmul(ps[pk][:, 0:cw], qip[:, hh, q0:q0 + 128], ki4[:, c0:c0 + cw], start=True, stop=True),
                                           reads=["qip", "ki4"], writes=[psk[pk]], inc=True)
                                    emit_L(0)
                                    for hh in range(8):
                                        pk = 4 + hh % 2
                                        if hh + 1 < 8:
                                            emit_L(hh + 1)
                                        rb = rlc % 4
                                        rlc += 1
                                        op("act", lambda h, pk=pk, hh=hh, cw=cw, rb=rb: h.activation(out=rl[rb][:, 0:cw], in_=ps[pk][:, 0:cw], func=AF.Relu, scale=wab[:, hh:hh + 1]),
                                           reads=[psk[pk], "wab"], writes=["rl%d" % rb])
                                        op("pe", lambda h, hh=hh, cw=cw, rb=rb: h.matmul(ps[6][:, 0:cw], dsg[:, hh, :], rl[rb][:, 0:cw], start=(hh == 0), stop=(hh == 7)),
                                           reads=["dsg%d" % hh, "rl%d" % rb], writes=[psk[6]], inc=(hh == 7))
                                    if c0 + cw == nk:
                                        op("pe", lambda h, cw=cw: h.matmul(ps[6][:, cw - 128:cw], ident[:], cmb[:], start=False, stop=True, skip_group_check=True),
                                           reads=["ident", "cmb"], writes=[psk[6]], inc=True)
                                    op("act", lambda h, c0=c0, cw=cw: h.copy(out=score[:, c0:c0 + cw], in_=ps[6][:, 0:cw]), reads=[psk[6]], writes=[sck])

                    def stageA2(g, r):
                        nonlocal rlc, ptc
                        if True:
                            b = g % 2
                            t0 = g * 512
                            mx = mixT[b]
                            mxk = "mixa%d" % b
                            i = 4 * g + r
                            q0 = r * 128
                            nk = 128 * (i + 1)
                            score = scoreb[i % 2]
                            sck = "score%d" % (i % 2)
                            if i >= 2:
                                op("dve", lambda h: h.memset(bis[:, 0:1], BIS_LO), writes=["bis"])
                                for it in range(NBIS):
                                    cst = BIS_W / (2.0 ** (it + 1))
                                    op("dve", lambda h, cst=cst: h.tensor_scalar(out=bis[:, 1:2], in0=bis[:, 0:1], scalar1=cst, scalar2=None, op0=ALU.add), reads=["bis"], writes=["bis"])
                                    op("dve", lambda h, nk=nk: h.tensor_scalar(out=mb[:, 0:nk], in0=score[:, 0:nk], scalar1=bis[:, 1:2], scalar2=None, op0=ALU.is_ge, op1=ALU.add, accum_out=bis[:, 2:3]),
                                       reads=[sck, "bis"], writes=["mb", "bis"])
                                    op("dve", lambda h, cst=cst: h.tensor_scalar(out=bis[:, 3:4], in0=bis[:, 2:3], scalar1=float(TOPK), scalar2=cst, op0=ALU.is_ge, op1=ALU.mult), reads=["bis"], writes=["bis"])
                                    op("dve", lambda h: h.tensor_tensor(out=bis[:, 0:1], in0=bis[:, 0:1], in1=bis[:, 3:4], op=ALU.add), reads=["bis"], writes=["bis"])
                                op("dve", lambda h, nk=nk: h.tensor_scalar(out=mb[:, 0:nk], in0=score[:, 0:nk], scalar1=bis[:, 0:1], scalar2=NEG, op0=ALU.is_lt, op1=ALU.mult), reads=[sck, "bis"], writes=["mb"])
                                nb = i + 1
                                op("dve", lambda h, nk=nk, nb=nb: h.tensor_reduce(out=anys[:, 0:nb], in_=mb[:, 0:nk].rearrange("p (j k) -> p j k", k=128), axis=AX.X, op=ALU.max), reads=["mb"], writes=["anys"])
                                op("dve", lambda h, nb=nb: h.tensor_scalar(out=anys[:, 0:nb], in0=anys[:, 0:nb], scalar1=-1.0, scalar2=None, op0=ALU.is_ge), reads=["anys"], writes=["anys"])
                                op("dve", lambda h, nb=nb: h.tensor_tensor(out=anys[:, 0:nb], in0=anys[:, 0:nb], in1=jpos1[:, 0:nb], op=ALU.mult), reads=["anys", "jpos1"], writes=["anys"])
                                op("dve", lambda h, nb=nb: h.tensor_reduce(out=anys[:, 32:33], in_=anys[:, 0:nb], axis=AX.X, op=ALU.max), reads=["anys"], writes=["anys"])
                                op("dve", lambda h, nb=nb: h.tensor_scalar(out=dblk[:, 0:1], in0=anys[:, 32:33], scalar1=-128.0, scalar2=128.0 * nb, op0=ALU.mult, op1=ALU.add), reads=["anys"], writes=["dblk"])

                    def stageB1(g, r):
                        nonlocal rlc, ptc
                        if True:
                            b = g % 2
                            t0 = g * 512
                            mx = mixT[b]
                            mxk = "mixa%d" % b
                            i = 4 * g + r
                            q0 = r * 128
                            nk = 128 * (i + 1)
                            if i >= 2:
                                for j0 in range(0, i + 1, 8):
                                    nj = min(8, i + 1 - j0)
                                    for jj in range(nj):
                                        op("pe", lambda h, j0=j0, jj=jj: h.transpose(pst[:, jj * 128:(jj + 1) * 128], mb[:, (j0 + jj) * 128:(j0 + jj + 1) * 128], ident[:]),
                                           reads=["mb", "ident"], writes=["pst"], inc=(jj == nj - 1))
                                    op("act" if (j0 // 8) % 2 == 0 else "dve",
                                       (lambda h, j0=j0, nj=nj: h.copy(out=mbT[:, j0:j0 + nj, :], in_=pst[:, 0:nj * 128].rearrange("p (j t) -> p j t", t=128))) if (j0 // 8) % 2 == 0 else
                                       (lambda h, j0=j0, nj=nj: h.tensor_copy(out=mbT[:, j0:j0 + nj, :], in_=pst[:, 0:nj * 128].rearrange("p (j t) -> p j t", t=128))),
                                       reads=["pst"], writes=["mbT"])
                                op("pe", lambda h: h.transpose(pst[:, 0:128], dblk[:], ident[:]), reads=["dblk", "ident"], writes=["pst"], inc=True)
                                op("dve", lambda h: h.tensor_copy(out=dT[:], in_=pst[:, 0:128]), reads=["pst"], writes=["dT"])

                    def stageB2(g, r):
                        nonlocal rlc, ptc
                        if True:
                            b = g % 2
                            t0 = g * 512
                            mx = mixT[b]
                            mxk = "mixa%d" % b
                            i = 4 * g + r
                            q0 = r * 128
                            nk = 128 * (i + 1)
                            if debug and l == 0 and s == 0 and i == 2:
                                dump("vn", vn[:], 256, ["vn"])
                                dump("wmT", wmT[:].rearrange("p a b -> p (a b)"), 512, ["wmT"])
                                dump("st4", st4[:], 16, ["st4"])
                                dump("tmpa", tmpa[:], 128, ["tmpa"])
                                dump("vg", vg[:], 256, ["vg"])
                                dump("uT", uT[:].rearrange("p a b -> p (a b)")[:, 0:512], 512, ["uT"])
                                dump("score", scoreb[0][:, 0:384], 384, ["score0"])
                                dump("bis", bis[:], 8, ["bis"])
                                dump("mb", mb[:, 0:384], 384, ["mb"])
                                dump("mbT", mbT[:].rearrange("p a b -> p (a b)")[:, 0:384], 384, ["mbT"])
                                dump("anys", anys[:], 64, ["anys"])
                                dump("dT", dT[:], 128, ["dT"])
                                dump("wab", wab[:], 8, ["wab"])
                                dump("sgn", sgn[:], 8, ["sgn"])
                                dump("bsT", bsT[:].rearrange("p a b -> p (a b)"), 256, ["bsT"])
                            pend = []

                            def emit_pv_c(item, i=i):
                                j, pslot = item
                                first, last = (j == 0), (j == i)
                                pk_ = "pTa%d" % pslot
                                for hh in range(4):
                                    ab = hh // 2
                                    co = (hh % 2) * 256
                                    op("pe", lambda h: h.matmul(ps[ab][:, co:co + 128], vc[:, j, ab * 128:(ab + 1) * 128], pT[pslot][:, hh, :], start=(first and hh % 2 == 0), stop=last, skip_group_check=True),
                                       reads=["vc", pk_], writes=[psk[ab]], inc=False)
                                    op("pe", lambda h: h.matmul(ps[ab][:, co + 128:co + 256], ones[:], pT[pslot][:, hh, :], start=False, stop=last, skip_group_check=True),
                                       reads=["ones", pk_], writes=[psk[ab]], inc=(hh == 3))

                            for j in range(0, i + 1):
                                sbk = 2 + (ptc % 2)
                                pslot = ptc % 3
                                ptc += 1
                                dg = (j == i)
                                for hh in range(4):
                                    ck = hh // 2
                                    cs = hh * 128
                                    op("pe", lambda h, sbk=sbk, cs=cs, hh=hh, j=j, ck=ck: h.matmul(ps[sbk][:, cs:cs + 128], ktc[:, ck, j * 128:(j + 1) * 128], qcpb[g % 2][:, hh, q0:q0 + 128], start=(hh == 0), stop=False, skip_group_check=True),
                                       reads=["ktc", "qcp%d" % (g % 2)], writes=[psk[sbk]], inc=False)
                                    op("pe", lambda h, sbk=sbk, cs=cs, j=j: h.matmul(ps[sbk][:, cs:cs + 128], ident[:], mbT[:, j, :], start=False, stop=False, skip_group_check=True),
                                       reads=["ident", "mbT"], writes=[psk[sbk]], inc=False)
                                    op("pe", lambda h, sbk=sbk, cs=cs, hh=hh, dg=dg: h.matmul(ps[sbk][:, cs:cs + 128], slmat[:, hh, :], dT[:], start=False, stop=(not dg), skip_group_check=True),
                                       reads=["slmat", "dT"], writes=[psk[sbk]], inc=((not dg) and hh == 3))
                                    if dg:
                                        op("pe", lambda h, sbk=sbk, cs=cs, hh=hh: h.matmul(ps[sbk][:, cs:cs + 128], ident[:], diag[:, hh, :], start=False, stop=True, skip_group_check=True),
                                           reads=["ident", "diag"], writes=[psk[sbk]], inc=(hh == 3))
                                if dg:
                                    op("act", lambda h, sbk=sbk, pslot=pslot: h.activation(out=pT[pslot][:].rearrange("p a b -> p (a b)"), in_=ps[sbk][:], func=AF.Exp, scale=0.125),
                                       reads=[psk[sbk]], writes=["pTa%d" % pslot])
                                else:
                                    kk = i - j + 1
                                    for hh in range(4):
                                        op("act", lambda h, sbk=sbk, pslot=pslot, hh=hh, kk=kk: h.activation(out=pT[pslot][:, hh, :], in_=ps[sbk][:, hh * 128:(hh + 1) * 128], func=AF.Exp, scale=0.125,
                                                                                                    bias=alibi[:, hh * 34 + kk:hh * 34 + kk + 1]),
                                           reads=[psk[sbk], "alibi"], writes=["pTa%d" % pslot])
                                pend.append((j, pslot))
                                if len(pend) > 1:
                                    emit_pv_c(pend.pop(0))
                            while pend:
                                emit_pv_c(pend.pop(0))
                            for hh in range(4):
                                ab = hh // 2
                                co = (hh % 2) * 256
                                rs = slice(64 * (hh % 2), 64 * (hh % 2) + 64)
                                op("dve", lambda h, ab=ab, co=co, rs=rs: h.reciprocal(out=rcp[rs, :], in_=ps[ab][rs, co + 128:co + 256]), reads=[psk[ab]], writes=["rcp"])
                                op("dve", lambda h, ab=ab, co=co, rs=rs, q0=q0: h.tensor_tensor(out=mx[rs, 2 + ab, q0:q0 + 128], in0=ps[ab][rs, co:co + 128], in1=rcp[rs, :], op=ALU.mult),
                                   reads=[psk[ab], "rcp"], writes=[mxk])
                            if r == 3:
                                op("sp", lambda h, b=b, t0=t0: h.dma_start(out=mixT_s[s, :, 0:2, t0:t0 + 512], in_=mixT[b][:, 0:2, :]), reads=[mxk], writes=["mixT_s"], dma="st_mx", nowaw=True)
                                op("sp", lambda h, b=b, t0=t0: h.dma_start(out=mixT_s[s, :, 6:8, t0:t0 + 512], in_=mixT[b][:, 2:4, :]), reads=[mxk], writes=["mixT_s"], dma="st_mx", nowaw=True)

                    tiles = [(g_, r_) for g_ in range(NGD) for r_ in range(4)]
                    NTL = len(tiles)
                    prologue(0)
                    stageA1a(*tiles[0])
                    stageA1b(*tiles[0])
                    stageA2(*tiles[0])
                    if NTL > 1:
                        stageA1a(*tiles[1])
                        stageA1b(*tiles[1])
                    for ti in range(NTL):
                        stageB1(*tiles[ti])
                        if ti + 2 < NTL:
                            if tiles[ti + 2][1] == 0:
                                prologue(tiles[ti + 2][0])
                            stageA1a(*tiles[ti + 2])
                        if ti + 1 < NTL:
                            stageA2(*tiles[ti + 1])
                        if ti + 2 < NTL:
                            stageA1b(*tiles[ti + 2])
                        stageB2(*tiles[ti])
                    P.barrier()
                    phase_ctr[0] += 1
                    if stop is not None and phase_ctr[0] >= stop:
                        P.stopped = True

                with ExitStack() as ph:
                    ktb = sb("ktb", [128, 4, S], BF16, ph)
                    vb = sb("vb", [128, NT, 512], BF16, ph)
                    op("sp", lambda h: h.dma_start(out=ktb[:], in_=ktb_s[s]), writes=["ktb"], dma="ldkv")
                    op("sp", lambda h: h.dma_start(out=vb[:], in_=vb_s[s]), writes=["vb"], dma="ldkv")
                    wq = sb("wqb", [128, 8, 512], BF16, ph)
                    for c in range(8):
                        op("pool", lambda h, c=c: h.dma_start(out=wq[:, c, :], in_=w_in[l, c * 128:(c + 1) * 128, OFF_B_Q:OFF_B_Q + 512]), writes=["wqb"], dma="wq", nowaw=True)
                    lv = sb("lv", [128, 4, 64], F32, ph)
                    lsm = sb("lsm", [128, 8], F32, ph)
                    gcol = sb("gcol", [128, 1], F32, ph)
                    op("sp", lambda h: h.dma_start(out=lv[:].rearrange("p a b -> p (a b)"), in_=lamv[l:l + 1].rearrange("o a b -> o (a b)").partition_broadcast(128)), writes=["lv"], dma="gp")
                    op("sp", lambda h: h.dma_start(out=gcol[:], in_=subg[l]), writes=["gcol"], dma="gp")
                    op("dve", lambda h: h.tensor_tensor(out=lv[:, 0, :], in0=lv[:, 0, :], in1=lv[:, 1, :], op=ALU.mult), reads=["lv"], writes=["lv"])
                    op("dve", lambda h: h.tensor_tensor(out=lv[:, 2, :], in0=lv[:, 2, :], in1=lv[:, 3, :], op=ALU.mult), reads=["lv"], writes=["lv"])
                    op("dve", lambda h: h.tensor_reduce(out=lsm[:, 0:1], in_=lv[:, 0, :], axis=AX.X, op=ALU.add), reads=["lv"], writes=["lsm"])
                    op("dve", lambda h: h.tensor_reduce(out=lsm[:, 1:2], in_=lv[:, 2, :], axis=AX.X, op=ALU.add), reads=["lv"], writes=["lsm"])
                    op("act", lambda h: h.activation(out=lsm[:, 2:4], in_=lsm[:, 0:2], func=AF.Exp), reads=["lsm"], writes=["lsm2"])
                    op("dve", lambda h: h.tensor_tensor(out=lsm[:, 4:5], in0=lsm[:, 3:4], in1=lsm[:, 2:3], op=ALU.subtract), reads=["lsm2"], writes=["lsm3"])
                    op("dve", lambda h: h.tensor_scalar(out=lsm[:, 5:6], in0=lsm[:, 4:5], scalar1=-lam_init, scalar2=None, op0=ALU.add), reads=["lsm3"], writes=["neglam"])
                    xT = [sb("xTb%d" % i, [128, 8, 512], BF16, ph) for i in range(2)]
                    qbp = sb("qbp", [128, 8, 512], BF16, ph)
                    pT = [sb("pTb%d" % i, [128, 4, 128], BF16, ph) for i in range(3)]
                    mixT = [sb("mixb%d" % i, [128, 4, 512], BF16, ph) for i in range(2)]
                    r1 = sb("r1", [128, 256], F32, ph)
                    oa = sb("oa", [128, 128], F32, ph)
                    ob = sb("ob", [128, 128], F32, ph)
                    oo = sb("oo", [128, 128], F32, ph)
                    osq = sb("osq", [128, 128], BF16, ph)
                    rsd = sb("rsd", [128, 128], F32, ph)
                    r1g = sb("r1g", [128, 1024], F32, ph)
                    oag = sb("oag", [128, 512], F32, ph)
                    obg = sb("obg", [128, 512], F32, ph)
                    oog = sb("oog", [128, 512], F32, ph)
                    osqg = sb("osqg", [128, 512], BF16, ph)
                    rsdg = sb("rsdg", [128, 512], F32, ph)
                    op("dve", lambda h: h.memset(qbp[:], 0.0), writes=["qbp"])
                    ptc = 0
                    for g in range(NGD):
                        b = g % 2
                        t0 = g * 512
                        mx = mixT[b]
                        mxk = "mixb%d" % b
                        op("sp", lambda h, b=b, t0=t0: h.dma_start(out=xT[b][:], in_=xT_s[s, :, :, t0:t0 + 512]), writes=["xTb%d" % b], dma="ldxT%d" % b)
                        for cc in range(4):
                            pk = cc % 2
                            for c in range(8):
                                op("pe", lambda h, pk=pk, cc=cc, c=c, b=b: h.matmul(ps[pk][:], wq[:, c, cc * 128:(cc + 1) * 128], xT[b][:, c, :], start=(c == 0), stop=(c == 7)),
                                   reads=["wqb", "xTb%d" % b], writes=[psk[pk]], inc=(c == 7))
                            op("dve", lambda h, pk=pk, cc=cc: h.tensor_copy(out=qbp[0:64, 2 * cc, :], in_=ps[pk][0:64, :]), reads=[psk[pk]], writes=["qbp"])
                            op("act", lambda h, pk=pk, cc=cc: h.copy(out=qbp[64:128, 2 * cc + 1, :], in_=ps[pk][64:128, :]), reads=[psk[pk]], writes=["qbp"])
                        for r in range(4):
                            i = 4 * g + r
                            q0 = r * 128
                            for pp in range(1):
                                hA, hB = 2 * pp, 2 * pp + 1
                                jlo = max(0, i - max(WIN[hA], WIN[hB]))
                                pend = []

                                def emit_pv_b(item, i=i):
                                    j, heads, pslot = item
                                    pk_ = "pTb%d" % pslot
                                    last = (j == i)
                                    for hi_, hh in enumerate(heads):
                                        ab = ACCB[hh]
                                        first = (j == max(0, i - WIN[hh]))
                                        for m in range(2):
                                            blk = (hh % 2) * 2 + m
                                            op("pe", lambda h: h.matmul(ps[ab][:, m * 256:m * 256 + 128], vb[:, j, hh * 128:(hh + 1) * 128], pT[pslot][:, blk, :], start=(first and m == 0), stop=last, skip_group_check=True),
                                               reads=["vb", pk_], writes=[psk[ab]], inc=False)
                                            op("pe", lambda h: h.matmul(ps[ab][:, m * 256 + 128:m * 256 + 256], ones[:], pT[pslot][:, blk, :], start=False, stop=last, skip_group_check=True),
                                               reads=["ones", pk_], writes=[psk[ab]], inc=(hi_ == len(heads) - 1 and m == 1))

                                for j in range(jlo, i + 1):
                                    heads = [hh for hh in (hA, hB) if j >= i - WIN[hh]]
                                    sbk = 2 + (ptc % 3)
                                    pslot = ptc % 3
                                    ptc += 1
                                    dg = (j == i)
                                    firstmm = True
                                    nmm = len(heads) * 2
                                    cnt_ = 0
                                    for hh in heads:
                                        for m in range(2):
                                            cs = ((hh % 2) * 2 + m) * 128
                                            cnt_ += 1
                                            lastmm = (cnt_ == nmm)
                                            op("pe", lambda h, sbk=sbk, cs=cs, hh=hh, m=m, j=j, dg=dg, fm=firstmm: h.matmul(ps[sbk][:, cs:cs + 128], ktb[:, hh, j * 128:(j + 1) * 128], qbp[:, 2 * hh + m, q0:q0 + 128], start=fm, stop=(not dg), skip_group_check=True),
                                               reads=["ktb", "qbp"], writes=[psk[sbk]], inc=((not dg) and lastmm))
                                            firstmm = False
                                            if dg:
                                                op("pe", lambda h, sbk=sbk, cs=cs, hh=hh: h.matmul(ps[sbk][:, cs:cs + 128], ident[:], diag[:, hh, :], start=False, stop=True, skip_group_check=True),
                                                   reads=["ident", "diag"], writes=[psk[sbk]], inc=lastmm)
                                    c_lo = (heads[0] % 2) * 256
                                    c_hi = (heads[-1] % 2) * 256 + 256
                                    if dg:
                                        op("act", lambda h, sbk=sbk, pslot=pslot, c_lo=c_lo, c_hi=c_hi: h.activation(out=pT[pslot][:].rearrange("p a b -> p (a b)")[:, c_lo:c_hi], in_=ps[sbk][:, c_lo:c_hi], func=AF.Exp, scale=0.125),
                                           reads=[psk[sbk]], writes=["pTb%d" % pslot])
                                    else:
                                        kk = i - j + 1
                                        for hh in heads:
                                            cl = (hh % 2) * 256
                                            op("act", lambda h, sbk=sbk, pslot=pslot, hh=hh, kk=kk, cl=cl: h.activation(out=pT[pslot][:].rearrange("p a b -> p (a b)")[:, cl:cl + 256], in_=ps[sbk][:, cl:cl + 256], func=AF.Exp, scale=0.125,
                                                                                                               bias=alibi[:, hh * 34 + kk:hh * 34 + kk + 1]),
                                               reads=[psk[sbk], "alibi"], writes=["pTb%d" % pslot])
                                    pend.append((j, heads, pslot))
                                    if len(pend) > 2:
                                        emit_pv_b(pend.pop(0))
                                while pend:
                                    emit_pv_b(pend.pop(0))
                                for hh in (hA, hB):
                                    pa = ACCB[hh]
                                    A = ps[pa]
                                    op("dve", lambda h, A=A: h.reciprocal(out=r1[:, 0:128], in_=A[:, 128:256]), reads=[psk[pa]], writes=["r1"])
                                    op("dve", lambda h, A=A: h.reciprocal(out=r1[:, 128:256], in_=A[:, 384:512]), reads=[psk[pa]], writes=["r1"])
                                    op("dve", lambda h, A=A: h.tensor_tensor(out=oa[:], in0=A[:, 0:128], in1=r1[:, 0:128], op=ALU.mult), reads=[psk[pa], "r1"], writes=["oa"])
                                    op("dve", lambda h, A=A: h.tensor_tensor(out=ob[:], in0=A[:, 256:384], in1=r1[:, 128:256], op=ALU.mult), reads=[psk[pa], "r1"], writes=["ob"])
                                    op("dve", lambda h: h.scalar_tensor_tensor(out=oo[:], in0=ob[:], scalar=lsm[:, 5:6], in1=oa[:], op0=ALU.mult, op1=ALU.add), reads=["oa", "ob", "neglam"], writes=["oo"])
                                    op("pool", lambda h: h.tensor_tensor(out=osq[:], in0=oo[:], in1=oo[:], op=ALU.mult), reads=["oo"], writes=["osq"])
                                    op("pe", lambda h, pa=pa: h.matmul(ps[pa][:, 0:128], ones[:], osq[:], start=True, stop=True), reads=["ones", "osq"], writes=[psk[pa]], inc=True)
                                    op("act", lambda h, pa=pa: h.activation(out=rsd[:], in_=ps[pa][:, 0:128], func=AF.Ln, scale=1.0 / 128, bias=EPS), reads=[psk[pa]], writes=["rsd"])
                                    op("act", lambda h: h.activation(out=rsd[:], in_=rsd[:], func=AF.Exp, scale=-0.5), reads=["rsd"], writes=["rsd"])
                                    op("dve", lambda h: h.tensor_tensor(out=oo[:], in0=oo[:], in1=rsd[:], op=ALU.mult), reads=["oo", "rsd"], writes=["oo"])
                                    op("dve", lambda h, hh=hh, q0=q0: h.tensor_scalar(out=mx[:, hh, q0:q0 + 128], in0=oo[:], scalar1=gcol[:, 0:1], scalar2=(1.0 - lam_init), op0=ALU.mult, op1=ALU.mult),
                                       reads=["oo", "gcol"], writes=[mxk])
                        for hh in (2, 3):
                            aO = [0, 5]
                            aS = [1, 6]
                            jl = 4 * g + 3
                            pendg = []

                            def emit_pv_g(item, hh=hh, jl=jl, aO=aO, aS=aS):
                                j, m, pslot, c0 = item
                                first, last = (j == 0), (j == jl)
                                pk_ = "pTb%d" % pslot
                                pf = pT[pslot][:].rearrange("p a b -> p (a b)")
                                op("pe", lambda h: h.matmul(ps[aO[m]][:, c0:512], vb[:, j, hh * 128:(hh + 1) * 128], pf[:, c0:512], start=first, stop=last),
                                   reads=["vb", pk_], writes=[psk[aO[m]]], inc=False)
                                op("pe", lambda h: h.matmul(ps[aS[m]][:, c0:512], ones[:], pf[:, c0:512], start=first, stop=last),
                                   reads=["ones", pk_], writes=[psk[aS[m]]], inc=True)

                            for j in range(0, jl + 1):
                                rd = j - 4 * g
                                c0 = 128 * max(0, rd)
                                kk = 4 * g + 4 - j
                                for m in range(2):
                                    sbk = 2 + (ptc % 3)
                                    pslot = ptc % 3
                                    ptc += 1
                                    pf = pT[pslot][:].rearrange("p a b -> p (a b)")
                                    pk_ = "pTb%d" % pslot
                                    op("pe", lambda h, sbk=sbk, c0=c0, j=j, m=m, hh=hh, rd=rd: h.matmul(ps[sbk][:, c0:512], ktb[:, hh, j * 128:(j + 1) * 128], qbp[:, 2 * hh + m, c0:512], start=True, stop=(rd < 0), skip_group_check=True),
                                       reads=["ktb", "qbp"], writes=[psk[sbk]], inc=(rd < 0))
                                    if rd >= 0:
                                        op("pe", lambda h, sbk=sbk, c0=c0, hh=hh: h.matmul(ps[sbk][:, c0:c0 + 128], ident[:], diag[:, hh, :], start=False, stop=True, skip_group_check=True),
                                           reads=["ident", "diag"], writes=[psk[sbk]], inc=True)
                                        op("act", lambda h, sbk=sbk, c0=c0, pf=pf, hh=hh, rd=rd: h.activation(out=pf[:, c0:c0 + 128], in_=ps[sbk][:, c0:c0 + 128], func=AF.Exp, scale=0.125, bias=float(-SLOPES[hh] * 128.0 * (3 - rd))),
                                           reads=[psk[sbk]], writes=[pk_])
                                        if c0 + 128 < 512:
                                            op("act", lambda h, sbk=sbk, c0=c0, pf=pf, hh=hh, kk=kk: h.activation(out=pf[:, c0 + 128:512], in_=ps[sbk][:, c0 + 128:512], func=AF.Exp, scale=0.125, bias=alibi[:, hh * 34 + kk:hh * 34 + kk + 1]),
                                               reads=[psk[sbk], "alibi"], writes=[pk_])
                                    else:
                                        op("act", lambda h, sbk=sbk, pf=pf, hh=hh, kk=kk: h.activation(out=pf[:, 0:512], in_=ps[sbk][:, 0:512], func=AF.Exp, scale=0.125, bias=alibi[:, hh * 34 + kk:hh * 34 + kk + 1]),
                                           reads=[psk[sbk], "alibi"], writes=[pk_])
                                    pendg.append((j, m, pslot, c0))
                                    if len(pendg) > 2:
                                        emit_pv_g(pendg.pop(0))
                            while pendg:
                                emit_pv_g(pendg.pop(0))
                            k0, k1, k2, k3 = psk[aO[0]], psk[aS[0]], psk[aO[1]], psk[aS[1]]
                            op("dve", lambda h, aS=aS: h.reciprocal(out=r1g[:, 0:512], in_=ps[aS[0]][:]), reads=[k1], writes=["r1g"])
                            op("dve", lambda h, aS=aS: h.reciprocal(out=r1g[:, 512:1024], in_=ps[aS[1]][:]), reads=[k3], writes=["r1g"])
                            op("dve", lambda h, aO=aO: h.tensor_tensor(out=oag[:], in0=ps[aO[0]][:], in1=r1g[:, 0:512], op=ALU.mult), reads=[k0, "r1g"], writes=["oag"])
                            op("dve", lambda h, aO=aO: h.tensor_tensor(out=obg[:], in0=ps[aO[1]][:], in1=r1g[:, 512:1024], op=ALU.mult), reads=[k2, "r1g"], writes=["obg"])
                            op("dve", lambda h: h.scalar_tensor_tensor(out=oog[:], in0=obg[:], scalar=lsm[:, 5:6], in1=oag[:], op0=ALU.mult, op1=ALU.add), reads=["oag", "obg", "neglam"], writes=["oog"])
                            op("pool", lambda h: h.tensor_tensor(out=osqg[:], in0=oog[:], in1=oog[:], op=ALU.mult), reads=["oog"], writes=["osqg"])
                            op("pe", lambda h, aO=aO: h.matmul(ps[aO[0]][:], ones[:], osqg[:], start=True, stop=True), reads=["ones", "osqg"], writes=[k0], inc=True)
                            op("act", lambda h, aO=aO: h.activation(out=rsdg[:], in_=ps[aO[0]][:], func=AF.Ln, scale=1.0 / 128, bias=EPS), reads=[k0], writes=["rsdg"])
                            op("act", lambda h: h.activation(out=rsdg[:], in_=rsdg[:], func=AF.Exp, scale=-0.5), reads=["rsdg"], writes=["rsdg"])
                            op("dve", lambda h: h.tensor_tensor(out=oog[:], in0=oog[:], in1=rsdg[:], op=ALU.mult), reads=["oog", "rsdg"], writes=["oog"])
                            op("dve", lambda h, hh=hh: h.tensor_scalar(out=mx[:, hh, :], in0=oog[:], scalar1=gcol[:, 0:1], scalar2=(1.0 - lam_init), op0=ALU.mult, op1=ALU.mult),
                               reads=["oog", "gcol"], writes=[mxk])
                        op("sp", lambda h, b=b, t0=t0: h.dma_start(out=mixT_s[s, :, 2:6, t0:t0 + 512], in_=mixT[b][:]), reads=[mxk], writes=["mixT_s"], dma="st_mx", nowaw=True)
                    P.barrier()
                    phase_ctr[0] += 1
                    if stop is not None and phase_ctr[0] >= stop:
                        P.stopped = True

            with ExitStack() as ph:
                wo = sb("wo", [128, 8, D], BF16, ph)
                wd = sb("wd", [128, NF, D], BF16, ph)
                for c in range(8):
                    op("pool", lambda h, c=c: h.dma_start(out=wo[:, c, :], in_=w_out[l, c * 128:(c + 1) * 128, :]), writes=["wo"], dma="wq", nowaw=True)
                for f in range(NF):
                    op("pool", lambda h, f=f: h.dma_start(out=wd[:, f, :], in_=w_dn[l, f * 128:(f + 1) * 128, :]), writes=["wd"], dma="wq", nowaw=True)
                lnp = sb("lnp", [128, 4, D], F32, ph)
                for k, src in enumerate((ln1g, ln1b, ln2g, ln2b)):
                    op("sp", lambda h, k=k, src=src: h.dma_start(out=lnp[:, k, :], in_=src[l].partition_broadcast(128)), writes=["lnp"], dma="gp", nowaw=True)
                wg = [sb("wg%d" % i, [128, 8, 256], BF16, ph) for i in range(3)]
                mxT = [sb("mxT%d" % i, [128, 8, 512], BF16, ph) for i in range(2)]
                xin = [sb("xr%d" % i, [128, D], F32, ph) for i in range(2)]
                x1 = sb("x1", [128, 4, D], F32, ph)
                x1b = sb("x1b", [128, D], BF16, ph)
                x1T = sb("x1T", [128, 8, 512], BF16, ph)
                actT = sb("actT", [128, NF, 512], BF16, ph)
                sg = [sb("sg%d" % i, [128, 512], F32, ph) for i in range(2)]
                zt = sb("zt", [128, D], F32, ph)
                yo = [sb("yo%d" % i, [128, D], F32, ph) for i in range(2)]
                bst = sb("bst", [128, 16], F32, ph)

                def layer_norm(zin, zkey, gk, bk, out_ap, out_key):
                    op("dve", lambda h: h.bn_stats(out=bst[:, 0:6], in_=zin[:, 0:512]), reads=[zkey], writes=["bst"])
                    op("dve", lambda h: h.bn_stats(out=bst[:, 6:12], in_=zin[:, 512:1024]), reads=[zkey], writes=["bst"])
                    op("dve", lambda h: h.bn_aggr(out=bst[:, 12:14], in_=bst[:, 0:12]), reads=["bst"], writes=["bst2"])
                    op("act", lambda h: h.activation(out=bst[:, 14:15], in_=bst[:, 13:14], func=AF.Ln, bias=EPS), reads=["bst2"], writes=["bst3"])
                    op("act", lambda h: h.activation(out=bst[:, 15:16], in_=bst[:, 14:15], func=AF.Exp, scale=-0.5), reads=["bst3"], writes=["bst4"])
                    op("dve", lambda h: h.tensor_scalar(out=zin[:], in0=zin[:], scalar1=bst[:, 12:13], scalar2=bst[:, 15:16], op0=ALU.subtract, op1=ALU.mult), reads=[zkey, "bst2", "bst4"], writes=[zkey])
                    op("pool", lambda h: h.tensor_tensor(out=zin[:], in0=zin[:], in1=lnp[:, gk, :], op=ALU.mult), reads=[zkey, "lnp"], writes=[zkey])
                    op("dve", lambda h: h.tensor_tensor(out=out_ap, in0=zin[:], in1=lnp[:, bk, :], op=ALU.add), reads=[zkey, "lnp"], writes=[out_key])

                gi = 0
                xc = 0
                wc = 0
                yc = 0
                for s in range(NSEQ):
                    for g in range(NGD):
                        b = gi % 2
                        gi += 1
                        t0 = g * 512
                        op("sp", lambda h, b=b, t0=t0, s=s: h.dma_start(out=mxT[b][:], in_=mixT_s[s, :, :, t0:t0 + 512]), writes=["mxT%d" % b], dma="ldmx%d" % b)
                        for r in range(4):
                            xb = xc % 2
                            xc += 1
                            tt = t0 + r * 128
                            op("sp", lambda h, xb=xb, tt=tt, s=s: h.dma_start(out=xin[xb][:], in_=x_src[s, tt:tt + 128, :]), writes=["xr%d" % xb], dma="ldxr%d" % xb)
                            for hf in range(2):
                                for c in range(8):
                                    op("pe", lambda h, hf=hf, c=c, b=b, r=r: h.matmul(ps[hf][:], mxT[b][:, c, r * 128:(r + 1) * 128], wo[:, c, hf * 512:(hf + 1) * 512], start=(c == 0), stop=(c == 7)),
                                       reads=["mxT%d" % b, "wo"], writes=[psk[hf]], inc=(c == 7))
                                op("dve", lambda h, hf=hf, xb=xb: h.scalar_tensor_tensor(out=zt[:, hf * 512:(hf + 1) * 512], in0=xin[xb][:, hf * 512:(hf + 1) * 512], scalar=ALPHA, in1=ps[hf][:], op0=ALU.mult, op1=ALU.add),
                                   reads=["xr%d" % xb, psk[hf]], writes=["zt"])
                            layer_norm(zt, "zt", 0, 1, x1[:, r, :], "x1_%d" % r)
                            op("act", lambda h, r=r: h.copy(out=x1b[:], in_=x1[:, r, :]), reads=["x1_%d" % r], writes=["x1b"])
                            for c in range(8):
                                op("pe", lambda h, c=c: h.transpose(pst[:, c * 128:(c + 1) * 128], x1b[:, c * 128:(c + 1) * 128], ident[:]), reads=["x1b", "ident"], writes=["pst"], inc=(c == 7))
                            op("act", lambda h, r=r: h.copy(out=x1T[:, :, r * 128:(r + 1) * 128], in_=pst[:].rearrange("p (c t) -> p c t", c=8)), reads=["pst"], writes=["x1T"])
                        for f in range(NF):
                            wb_ = wc % 3
                            wc += 1
                            op("sp", lambda h, wb_=wb_, f=f: h.dma_start(out=wg[wb_][:], in_=wgu_s[l, f]), writes=["wg%d" % wb_], dma="ldwg%d" % wb_)
                            pg, pu = 2 + 2 * (f % 2), 3 + 2 * (f % 2)
                            for c in range(8):
                                op("pe", lambda h, pg=pg, c=c, wb_=wb_: h.matmul(ps[pg][:], wg[wb_][:, c, 0:128], x1T[:, c, :], start=(c == 0), stop=(c == 7)),
                                   reads=["wg%d" % wb_, "x1T"], writes=[psk[pg]], inc=(c == 7))
                            for c in range(8):
                                op("pe", lambda h, pu=pu, c=c, wb_=wb_: h.matmul(ps[pu][:], wg[wb_][:, c, 128:256], x1T[:, c, :], start=(c == 0), stop=(c == 7)),
                                   reads=["wg%d" % wb_, "x1T"], writes=[psk[pu]], inc=(c == 7))
                            sb_ = f % 2
                            op("act", lambda h, pg=pg, sb_=sb_: h.activation(out=sg[sb_][:], in_=ps[pg][:], func=AF.Silu), reads=[psk[pg]], writes=["sg%d" % sb_])
                            op("dve", lambda h, pu=pu, sb_=sb_, f=f: h.tensor_tensor(out=actT[:, f, :], in0=sg[sb_][:], in1=ps[pu][:], op=ALU.mult), reads=["sg%d" % sb_, psk[pu]], writes=["actT"])
                        for r in range(4):
                            tt = t0 + r * 128
                            for hf in range(2):
                                for f in range(NF):
                                    op("pe", lambda h, hf=hf, f=f, r=r: h.matmul(ps[hf][:], actT[:, f, r * 128:(r + 1) * 128], wd[:, f, hf * 512:(hf + 1) * 512], start=(f == 0), stop=(f == NF - 1)),
                                       reads=["actT", "wd"], writes=[psk[hf]], inc=(f == NF - 1))
                                op("dve", lambda h, hf=hf, r=r: h.scalar_tensor_tensor(out=zt[:, hf * 512:(hf + 1) * 512], in0=x1[:, r, hf * 512:(hf + 1) * 512], scalar=ALPHA, in1=ps[hf][:], op0=ALU.mult, op1=ALU.add),
                                   reads=["x1_%d" % r, psk[hf]], writes=["zt"])
                            yb = yc % 2
                            yc += 1
                            layer_norm(zt, "zt", 2, 3, yo[yb][:], "yo%d" % yb)
                            op("sp", lambda h, yb=yb, tt=tt, s=s: h.dma_start(out=x_dst[s, tt:tt + 128, :], in_=yo[yb][:]), reads=["yo%d" % yb], writes=["xdst"], dma="st_y", nowaw=True)
                P.barrier()
                phase_ctr[0] += 1
                if stop is not None and phase_ctr[0] >= stop:
                    P.stopped = True
        except _Stop:
            pass
        P.final_wait("sp")
    return nc


def _consts():
    ident = np.eye(128, dtype=np.float32)
    so = np.arange(128)[:, None]
    to = np.arange(128)[None, :]
    diag = np.zeros((128, 4, 128), np.float32)
    for h in range(4):
        v = (-np.abs(to - so) + (to - 128)).astype(np.float32) * SLOPES[h] * 8.0
        v = np.where((so // 64) > (to // 64), NEG * 8.0, v)
        diag[:, h, :] = v
    alibi = np.zeros((128, 4 * 34), np.float32)
    for h in range(4):
        for k in range(34):
            alibi[:, h * 34 + k] = SLOPES[h] * (np.arange(128) - 128.0 * k)
    cmask = np.where((to // 64) > (so // 64), -1e30, 0.0).astype(np.float32)
    gmask = ((so // 64) <= (to // 64)).astype(np.float32)
    jpos = np.tile(np.arange(1, 33, dtype=np.float32)[None, :], (128, 1))
    return ident, diag, alibi, cmask, gmask, jpos


_CACHE = {}


def kernel(**inputs):
    n = 8
    f = lambda a: np.ascontiguousarray(np.asarray(a, dtype=np.float32))
    x = f(inputs["x"])
    ident, diag, alibi, cmask, gmask, jpos = _consts()
    lamv = np.stack([f(inputs["lam_q1"]), f(inputs["lam_k1"]), f(inputs["lam_q2"]), f(inputs["lam_k2"])], axis=1)
    shared = {
        "w_in": f(inputs["w_in"]),
        "gmlp_w_s": f(inputs["gmlp_w_s"]),
        "gmlp_b_s": f(inputs["gmlp_b_s"]),
        "gmlp_ln_g": f(inputs["gmlp_ln_g"]).reshape(DEPTH, 1, 256),
        "gmlp_ln_b": f(inputs["gmlp_ln_b"]).reshape(DEPTH, 1, 256),
        "lamv": np.ascontiguousarray(lamv),
        "diff_subln_g": f(inputs["diff_subln_g"]).reshape(DEPTH, 128, 1),
        "w_out": f(inputs["w_out"]),
        "ln1_g": f(inputs["ln1_g"]).reshape(DEPTH, 1, D),
        "ln1_b": f(inputs["ln1_b"]).reshape(DEPTH, 1, D),
        "w_gu": f(inputs["w_gu"]),
        "w_down": f(inputs["w_down"]),
        "ln2_g": f(inputs["ln2_g"]).reshape(DEPTH, 1, D),
        "ln2_b": f(inputs["ln2_b"]).reshape(DEPTH, 1, D),
        "c_ident": ident, "c_diag": diag, "c_alibi": alibi, "c_cmask": cmask, "c_gmask": gmask, "c_jpos": jpos,
    }
    if "nc" not in _CACHE:
        _CACHE["nc"] = build_program()
    nc = _CACHE["nc"]
    in_maps = []
    for c in range(n):
        m = dict(shared)
        m["x"] = np.ascontiguousarray(x[NSEQ * c:NSEQ * (c + 1)])
        in_maps.append(m)
    res = run_bass_kernel_spmd(nc, in_maps, core_ids=list(range(n)))
    out = np.concatenate([np.asarray(r["y"], dtype=np.float32) for r in res.results], axis=0)
    return out
```

```python
import math
from contextlib import ExitStack
import numpy as np
import concourse.bass as bass
import concourse.mybir as mybir
from concourse.bass_utils import run_bass_kernel_spmd

F32 = mybir.dt.float32
BF16 = mybir.dt.bfloat16
AF = mybir.ActivationFunctionType
ALU = mybir.AluOpType
AX = mybir.AxisListType

D = 1024
S = 4096
DEPTH = 2
NSEQ = 2
NT = S // 128
NG = S // 512
FF = 2816
NF = FF // 128
IN_W = 3112
OFF_A_U, OFF_A_V, OFF_B_Q, OFF_B_K, OFF_B_V = 0, 256, 512, 1024, 1536
OFF_C_Q, OFF_C_K, OFF_C_V, OFF_I_Q, OFF_I_K, OFF_I_W = 2048, 2304, 2560, 2816, 3072, 3104
ALPHA = (2 * DEPTH) ** 0.25
EPS = 1e-5
SLOPES = [2.0 ** (-8.0 * (h + 1) / 4) for h in range(4)]
WIN = [2, 9, 31, 31]
NBIS = 16
BIS_LO, BIS_W = -8.0, 16.0
ACCB = [0, 1, 5, 6]
TOPK = 256
NEG = -30000.0
IDX_SCALE = (8 ** -0.5) * (32 ** -0.5)


class Tok:
    __slots__ = ("sem", "val")

    def __init__(self, sem, val):
        self.sem = sem
        self.val = val


class DSem:
    def __init__(self, h):
        self.h = h
        self.total = 0


class Eng:
    def __init__(self, name, handle, sem):
        self.name = name
        self.h = handle
        self.sem = sem
        self.count = 0
        self.waited = {}


class Prog:
    def __init__(self, nc, es):
        self.nc = nc
        self.es = es
        self.eng = {}
        for name, h in (("pe", nc.tensor), ("act", nc.scalar), ("dve", nc.vector), ("pool", nc.gpsimd), ("sp", nc.sync)):
            self.eng[name] = Eng(name, h, es.enter_context(nc.semaphore("sem_" + name)))
        self.dsems = {}
        self.last_w = {}
        self.readers = {}
        self.stopped = False

    def dsem(self, name):
        if name not in self.dsems:
            self.dsems[name] = DSem(self.es.enter_context(self.nc.semaphore("dma_" + name)))
        return self.dsems[name]

    def _wait(self, e, tok):
        if isinstance(tok.sem, DSem):
            sem, val = tok.sem.h, tok.sem.total
            key = ("d", id(tok.sem))
        else:
            if tok.sem is e and e.name == "pe":
                return
            sem, val = tok.sem.sem, tok.val
            key = ("e", tok.sem.name)
        if e.waited.get(key, 0) >= val:
            return
        e.h.wait_ge(sem, val)
        e.waited[key] = val

    def op(self, eng, fn, reads=(), writes=(), inc=True, dma=None, nowaw=False):
        if self.stopped:
            return None
        e = self.eng[eng]
        deps = []
        for k in reads:
            t = self.last_w.get(k)
            if t is not None:
                deps.append(t)
        for k in writes:
            t = self.last_w.get(k)
            if t is not None and not nowaw:
                deps.append(t)
            deps.extend(self.readers.get(k, ()))
        for t in deps:
            self._wait(e, t)
        ins = fn(e.h)
        if dma is not None:
            ds = self.dsem(dma)
            ds.total += 16
            ins.then_inc(ds.h, 16)
            tok = Tok(ds, None)
        else:
            if inc:
                e.count += 1
                ins.then_inc(e.sem, 1)
                tok = Tok(e, e.count)
            else:
                tok = Tok(e, e.count + 1)
        for k in reads:
            self.readers.setdefault(k, []).append(tok)
            if len(self.readers[k]) > 24:
                self.readers[k] = self.readers[k][-24:] if False else self.readers[k]
        for k in writes:
            self.last_w[k] = tok
            self.readers[k] = []
        return ins

    def barrier(self):
        if self.stopped:
            return
        names = list(self.eng)
        for n in names:
            e = self.eng[n]
            for m in names:
                if m != n and self.eng[m].count > 0:
                    self._wait(e, Tok(self.eng[m], self.eng[m].count))
            for ds in self.dsems.values():
                if ds.total > 0:
                    self._wait(e, Tok(ds, None))
        self.last_w.clear()
        self.readers.clear()

    def final_wait(self, eng):
        e = self.eng[eng]
        for m in self.eng:
            if m != eng and self.eng[m].count > 0:
                self._wait(e, Tok(self.eng[m], self.eng[m].count))
        for ds in self.dsems.values():
            if ds.total > 0:
                self._wait(e, Tok(ds, None))


class _Stop(Exception):
    pass


def build_program(depth=DEPTH, debug=False, stop=None, dbg_groups=None):
    NGD = NG if dbg_groups is None else dbg_groups
    nc = bass.Bass("TRN2", target_bir_lowering=False)
    dt = nc.dram_tensor

    def din(name, shape):
        return dt(name, list(shape), F32, kind="ExternalInput").ap()

    x_in = din("x", [NSEQ, S, D])
    w_in = din("w_in", [DEPTH, D, IN_W])
    w_s = din("gmlp_w_s", [DEPTH, 4, 128, 128])
    b_s = din("gmlp_b_s", [DEPTH, 4, 128])
    a_g = din("gmlp_ln_g", [DEPTH, 1, 256])
    a_b = din("gmlp_ln_b", [DEPTH, 1, 256])
    lamv = din("lamv", [DEPTH, 4, 64])
    subg = din("diff_subln_g", [DEPTH, 128, 1])
    w_out = din("w_out", [DEPTH, D, D])
    ln1g = din("ln1_g", [DEPTH, 1, D])
    ln1b = din("ln1_b", [DEPTH, 1, D])
    w_gu = din("w_gu", [DEPTH, D, 2 * FF])
    w_dn = din("w_down", [DEPTH, FF, D])
    ln2g = din("ln2_g", [DEPTH, 1, D])
    ln2b = din("ln2_b", [DEPTH, 1, D])
    c_ident = din("c_ident", [128, 128])
    c_diag = din("c_diag", [128, 4, 128])
    c_alibi = din("c_alibi", [128, 4 * 34])
    c_cmask = din("c_cmask", [128, 128])
    c_gmask = din("c_gmask", [128, 128])
    c_jpos = din("c_jpos", [128, 32])
    y_out = dt("y", [NSEQ, S, D], F32, kind="ExternalOutput").ap()

    skind = "ExternalOutput" if debug else "Internal"
    xs = dt("xs", [NSEQ, S, D], F32, kind=skind).ap()
    xT_s = dt("xT_s", [NSEQ, 128, 8, S], BF16, kind=skind).ap()
    ktb_s = dt("ktb_s", [NSEQ, 128, 4, S], BF16, kind=skind).ap()
    ktc_s = dt("ktc_s", [NSEQ, 128, 2, S], BF16, kind=skind).ap()
    ki4_s = dt("ki4_s", [NSEQ, 128, S], BF16, kind=skind).ap()
    vb_s = dt("vb_s", [NSEQ, 128, NT, 512], BF16, kind=skind).ap()
    vc_s = dt("vc_s", [NSEQ, 128, NT, 256], BF16, kind=skind).ap()
    mixT_s = dt("mixT_s", [NSEQ, 128, 8, S], BF16, kind=skind).ap()
    wgu_s = dt("wgu_s", [DEPTH, NF, 128, 8, 256], BF16, kind="Internal").ap()

    with ExitStack() as es:
        E = es.enter_context
        P = Prog(nc, es)
        op = P.op
        dbgbuf = dt("dbgbuf", [128, 16384], F32, kind="ExternalOutput").ap() if debug else None
        dbgpos = {}

        def dump(name, ap2d, ncols, reads, cond=True):
            if not debug or not cond or name in dbgpos:
                return
            off = sum(v[1] for v in dbgpos.values())
            dbgpos[name] = (off, ncols)
            op("pool", lambda h: h.dma_start(out=dbgbuf[0:ap2d.shape[0], off:off + ncols], in_=ap2d), reads=reads, writes=["dbgbuf"], dma="dbg", nowaw=True)
        build_program.dbgpos = dbgpos

        uid = [0]

        def sb(name, shape, dtype, stack=es):
            uid[0] += 1
            return stack.enter_context(nc.sbuf_tensor("%s_%d" % (name, uid[0]), list(shape), dtype))

        ident = sb("ident", [128, 128], BF16)
        ones = sb("ones", [128, 128], BF16)
        diag = sb("diag", [128, 4, 128], BF16)
        alibi = sb("alibi", [128, 4 * 34], F32)
        cmask = sb("cmask", [128, 128], F32)
        gmask = sb("gmask", [128, 128], F32)
        op("pool", lambda h: h.dma_start(out=ident[:], in_=c_ident), writes=["ident"], dma="c0")
        op("pool", lambda h: h.dma_start(out=diag[:], in_=c_diag), writes=["diag"], dma="c0")
        op("sp", lambda h: h.dma_start(out=alibi[:], in_=c_alibi), writes=["alibi"], dma="c1")
        op("sp", lambda h: h.dma_start(out=cmask[:], in_=c_cmask), writes=["cmask"], dma="c1")
        op("sp", lambda h: h.dma_start(out=gmask[:], in_=c_gmask), writes=["gmask"], dma="c1")
        op("dve", lambda h: h.memset(ones[:], 1.0), writes=["ones"])
        jpos1 = sb("jpos1", [128, 32], F32)
        slmat = sb("slmat", [128, 4, 128], BF16)
        op("sp", lambda h: h.dma_start(out=jpos1[:], in_=c_jpos), writes=["jpos1"], dma="c1")
        for hh in range(4):
            op("dve", lambda h, hh=hh: h.memset(slmat[:, hh, :], 8.0 * SLOPES[hh]), writes=["slmat"])

        ps = [E(nc.psum_tensor("ps%d" % i, [128, 512], F32)) for i in range(7)]
        pst = E(nc.psum_tensor("pst", [128, 1024], BF16))
        psk = ["ps%d" % i for i in range(7)]

        for l in range(depth):
            for f in range(NF):
                for half in range(2):
                    src = w_gu[l, :, half * FF + f * 128: half * FF + (f + 1) * 128].rearrange("(c p) j -> p c j", p=128)
                    op("pool", lambda h, src=src, f=f, half=half, l=l: h.dma_start(
                        out=wgu_s[l, f, :, :, half * 128:(half + 1) * 128], in_=src),
                        writes=["wgu_s"], dma="wgucvt", nowaw=True)
        P.barrier()

        phase_ctr = [0]
        if stop == 0:
            depth_iter = []
        else:
            depth_iter = range(depth)
        try:
          for l in depth_iter:
            lam_init = 0.8 - 0.6 * math.exp(-0.3 * l)
            x_src = x_in if l == 0 else xs
            x_dst = y_out if l == depth - 1 else xs
            for s in range(NSEQ):
                with ExitStack() as ph:
                    wk = sb("wk", [128, 8, 1664], BF16, ph)
                    for c in range(8):
                        rows = w_in[l, c * 128:(c + 1) * 128, :]
                        op("pool", lambda h, c=c, rows=rows: h.dma_start(out=wk[:, c, 0:512], in_=rows[:, OFF_B_K:OFF_B_K + 512]), writes=["wk"], dma="wk", nowaw=True)
                        op("pool", lambda h, c=c, rows=rows: h.dma_start(out=wk[:, c, 512:768], in_=rows[:, OFF_C_K:OFF_C_K + 256]), writes=["wk"], dma="wk", nowaw=True)
                        for r in range(4):
                            op("pool", lambda h, c=c, rows=rows, r=r: h.dma_start(out=wk[:, c, 768 + 32 * r:800 + 32 * r], in_=rows[:, OFF_I_K:OFF_I_K + 32]), writes=["wk"], dma="wk", nowaw=True)
                        op("pool", lambda h, c=c, rows=rows: h.dma_start(out=wk[:, c, 896:1408], in_=rows[:, OFF_B_V:OFF_B_V + 512]), writes=["wk"], dma="wk", nowaw=True)
                        op("pool", lambda h, c=c, rows=rows: h.dma_start(out=wk[:, c, 1408:1664], in_=rows[:, OFF_C_V:OFF_C_V + 256]), writes=["wk"], dma="wk", nowaw=True)
                    xin = [sb("xin%d" % i, [128, 4, D], F32, ph) for i in range(2)]
                    xbf = [sb("xbf%d" % i, [128, 4, D], BF16, ph) for i in range(2)]
                    xT = [sb("xT%d" % i, [128, 8, 512], BF16, ph) for i in range(2)]
                    kst = [sb("kst%d" % i, [128, 7, 512], BF16, ph) for i in range(2)]
                    vst = [sb("vst%d" % i, [128, 4, 768], BF16, ph) for i in range(2)]
                    cnt = 0
                    for g in range(NGD):
                        b = g % 2
                        t0 = g * 512
                        op("sp", lambda h, b=b, t0=t0: h.dma_start(out=xin[b][:], in_=x_src[s, t0:t0 + 512, :].rearrange("(r p) d -> p r d", p=128)),
                           writes=["xin%d" % b], dma="xin%d" % b)
                        op("dve" if g % 2 == 0 else "pool", lambda h, b=b: h.tensor_copy(out=xbf[b][:], in_=xin[b][:]), reads=["xin%d" % b], writes=["xbf%d" % b])
                        for r in range(4):
                            for c in range(8):
                                op("pe", lambda h, b=b, r=r, c=c: h.transpose(pst[:, c * 128:(c + 1) * 128], xbf[b][:, r, c * 128:(c + 1) * 128], ident[:]),
                                   reads=["xbf%d" % b, "ident"], writes=["pst"], inc=(c == 7))
                            op("act" if r % 2 == 0 else "dve",
                               (lambda h, b=b, r=r: h.copy(out=xT[b][:, :, r * 128:(r + 1) * 128], in_=pst[:].rearrange("p (c t) -> p c t", c=8))) if r % 2 == 0 else
                               (lambda h, b=b, r=r: h.tensor_copy(out=xT[b][:, :, r * 128:(r + 1) * 128], in_=pst[:].rearrange("p (c t) -> p c t", c=8))),
                               reads=["pst"], writes=["xT%d" % b])
                        op("sp", lambda h, b=b, t0=t0: h.dma_start(out=xT_s[s, :, :, t0:t0 + 512], in_=xT[b][:]), reads=["xT%d" % b], writes=["xT_s"], dma="st_xT", nowaw=True)
                        for cc in range(7):
                            pk = cnt % 6
                            cnt += 1
                            for c in range(8):
                                op("pe", lambda h, pk=pk, cc=cc, c=c, b=b: h.matmul(ps[pk][:], wk[:, c, cc * 128:(cc + 1) * 128], xT[b][:, c, :], start=(c == 0), stop=(c == 7)),
                                   reads=["wk", "xT%d" % b], writes=[psk[pk]], inc=(c == 7))
                            if cc % 2 == 0:
                                op("act", lambda h, pk=pk, cc=cc, b=b: h.copy(out=kst[b][:, cc, :], in_=ps[pk][:]), reads=[psk[pk]], writes=["kst%d" % b])
                            else:
                                op("dve", lambda h, pk=pk, cc=cc, b=b: h.tensor_copy(out=kst[b][:, cc, :], in_=ps[pk][:]), reads=[psk[pk]], writes=["kst%d" % b])
                        op("sp", lambda h, b=b, t0=t0: h.dma_start(out=ktb_s[s, :, :, t0:t0 + 512], in_=kst[b][:, 0:4, :]), reads=["kst%d" % b], writes=["ktb_s"], dma="st_k", nowaw=True)
                        op("sp", lambda h, b=b, t0=t0: h.dma_start(out=ktc_s[s, :, :, t0:t0 + 512], in_=kst[b][:, 4:6, :]), reads=["kst%d" % b], writes=["ktc_s"], dma="st_k", nowaw=True)
                        op("sp", lambda h, b=b, t0=t0: h.dma_start(out=ki4_s[s, :, t0:t0 + 512], in_=kst[b][:, 6, :]), reads=["kst%d" % b], writes=["ki4_s"], dma="st_k", nowaw=True)
                        for r in range(4):
                            pk = cnt % 6
                            cnt += 1
                            for c in range(8):
                                op("pe", lambda h, pk=pk, r=r, c=c, b=b: h.matmul(ps[pk][:], xT[b][:, c, r * 128:(r + 1) * 128], wk[:, c, 896:1408], start=(c == 0), stop=(c == 7)),
                                   reads=["wk", "xT%d" % b], writes=[psk[pk]], inc=(c == 7))
                            op("act", lambda h, pk=pk, r=r, b=b: h.copy(out=vst[b][:, r, 0:512], in_=ps[pk][:]), reads=[psk[pk]], writes=["vst%d" % b])
                            pk = cnt % 6
                            cnt += 1
                            for c in range(8):
                                op("pe", lambda h, pk=pk, r=r, c=c, b=b: h.matmul(ps[pk][:, 0:256], xT[b][:, c, r * 128:(r + 1) * 128], wk[:, c, 1408:1664], start=(c == 0), stop=(c == 7)),
                                   reads=["wk", "xT%d" % b], writes=[psk[pk]], inc=(c == 7))
                            op("dve", lambda h, pk=pk, r=r, b=b: h.tensor_copy(out=vst[b][:, r, 512:768], in_=ps[pk][:, 0:256]), reads=[psk[pk]], writes=["vst%d" % b])
                        op("sp", lambda h, b=b, g=g: h.dma_start(out=vb_s[s, :, 4 * g:4 * g + 4, :], in_=vst[b][:, :, 0:512]), reads=["vst%d" % b], writes=["vb_s"], dma="st_v", nowaw=True)
                        op("sp", lambda h, b=b, g=g: h.dma_start(out=vc_s[s, :, 4 * g:4 * g + 4, :], in_=vst[b][:, :, 512:768]), reads=["vst%d" % b], writes=["vc_s"], dma="st_v", nowaw=True)
                    P.barrier()
                    phase_ctr[0] += 1
                    if stop is not None and phase_ctr[0] >= stop:
                        P.stopped = True

                with ExitStack() as ph:
                    ktc = sb("ktc", [128, 2, S], BF16, ph)
                    vc = sb("vc", [128, NT, 256], BF16, ph)
                    ki4 = sb("ki4", [128, S], BF16, ph)
                    op("sp", lambda h: h.dma_start(out=ktc[:], in_=ktc_s[s]), writes=["ktc"], dma="ldkv")
                    op("sp", lambda h: h.dma_start(out=vc[:], in_=vc_s[s]), writes=["vc"], dma="ldkv")
                    op("sp", lambda h: h.dma_start(out=ki4[:], in_=ki4_s[s]), writes=["ki4"], dma="ldkv")
                    wq = sb("wq", [128, 8, 768], BF16, ph)
                    wv = sb("wv", [128, 8, 264], BF16, ph)
                    for c in range(8):
                        rows = w_in[l, c * 128:(c + 1) * 128, :]
                        op("pool", lambda h, c=c, rows=rows: h.dma_start(out=wq[:, c, 0:256], in_=rows[:, OFF_A_U:OFF_A_U + 256]), writes=["wq"], dma="wq", nowaw=True)
                        op("pool", lambda h, c=c, rows=rows: h.dma_start(out=wq[:, c, 256:512], in_=rows[:, OFF_C_Q:OFF_C_Q + 256]), writes=["wq"], dma="wq", nowaw=True)
                        op("pool", lambda h, c=c, rows=rows: h.dma_start(out=wq[:, c, 512:768], in_=rows[:, OFF_I_Q:OFF_I_Q + 256]), writes=["wq"], dma="wq", nowaw=True)
                        op("pool", lambda h, c=c, rows=rows: h.dma_start(out=wv[:, c, 0:256], in_=rows[:, OFF_A_V:OFF_A_V + 256]), writes=["wv"], dma="wq", nowaw=True)
                        op("pool", lambda h, c=c, rows=rows: h.dma_start(out=wv[:, c, 256:264], in_=rows[:, OFF_I_W:OFF_I_W + 8]), writes=["wv"], dma="wq", nowaw=True)
                    wmT = sb("wmT", [128, 4, 128], BF16, ph)
                    wmf = sb("wmf", [128, 4, 128], F32, ph)
                    bsT = sb("bsT", [128, 2, 128], F32, ph)
                    lng = sb("lng", [128, 256], F32, ph)
                    lnb = sb("lnb", [128, 256], F32, ph)
                    wmb = sb("wmb", [128, 4, 128], BF16, ph)
                    op("sp", lambda h: h.dma_start(out=wmf[:], in_=w_s[l].rearrange("g t s -> t g s")), writes=["wmf"], dma="gpw")
                    op("dve", lambda h: h.tensor_copy(out=wmb[:], in_=wmf[:]), reads=["wmf"], writes=["wmb"])
                    for gg in range(4):
                        op("pe", lambda h, gg=gg: h.transpose(pst[:, gg * 128:(gg + 1) * 128], wmb[:, gg, :], ident[:]), reads=["wmb", "ident"], writes=["pst"], inc=(gg == 3))
                    for gg in range(4):
                        op("sp", lambda h, gg=gg: h.dma_start(out=bsT[(gg % 2) * 64:(gg % 2) * 64 + 64, gg // 2, :], in_=b_s[l, gg:gg + 1, :].partition_broadcast(64)),
                           writes=["bsT"], dma="gp", nowaw=True)
                    op("sp", lambda h: h.dma_start(out=lng[:], in_=a_g[l].partition_broadcast(128)), writes=["lng"], dma="gp")
                    op("sp", lambda h: h.dma_start(out=lnb[:], in_=a_b[l].partition_broadcast(128)), writes=["lnb"], dma="gp")
                    for gg in range(4):
                        op("dve", lambda h, gg=gg: h.tensor_tensor(out=wmT[:, gg, :], in0=pst[:, gg * 128:(gg + 1) * 128], in1=gmask[:], op=ALU.mult), reads=["pst", "gmask"], writes=["wmT"])

                    xT = [sb("xTa%d" % i, [128, 8, 512], BF16, ph) for i in range(2)]
                    uT = sb("uT", [128, 2, 512], BF16, ph)
                    qcp = sb("qcp", [128, 4, 512], BF16, ph)
                    qip = sb("qip", [128, 8, 512], BF16, ph)
                    vtm = sb("vtm", [128, 264], F32, ph)
                    vg = sb("vg", [128, 256], F32, ph)
                    vsq = sb("vsq", [128, 256], F32, ph)
                    st4 = sb("st4", [128, 16], F32, ph)
                    vn = sb("vn", [128, 256], BF16, ph)
                    wab = sb("wab", [128, 8], F32, ph)
                    sgn = sb("sgn", [128, 8], F32, ph)
                    dsg = sb("dsg", [128, 8, 128], BF16, ph)
                    scoreb = [sb("score%d" % i_, [128, S], F32, ph) for i_ in range(2)]
                    cmb = sb("cmb", [128, 128], BF16, ph)
                    op("pool", lambda h: h.dma_start(out=cmb[:], in_=c_cmask), writes=["cmb"], dma="gpc")
                    mb = sb("mb", [128, S], BF16, ph)
                    mbT = sb("mbT", [128, NT, 128], BF16, ph)
                    rl = [sb("rl%d" % i, [128, 512], BF16, ph) for i in range(4)]
                    pT = [sb("pTa%d" % i, [128, 4, 128], BF16, ph) for i in range(3)]
                    bis = sb("bis", [128, 8], F32, ph)
                    mixT = [sb("mixa%d" % i, [128, 4, 512], BF16, ph) for i in range(2)]
                    tmpa = sb("tmpa", [128, 128], F32, ph)
                    rcp = sb("rcp", [128, 128], F32, ph)
                    qcp2 = sb("qcp2", [128, 4, 512], BF16, ph)
                    op("dve", lambda h: h.memset(qcp[:], 0.0), writes=["qcp0"])
                    op("dve", lambda h: h.memset(qcp2[:], 0.0), writes=["qcp1"])
                    op("pool", lambda h: h.memset(qip[:], 0.0), writes=["qip"])
                    op("pool", lambda h: h.memset(mbT[:], 0.0), writes=["mbT"])
                    dblk = sb("dblk", [128, 128], BF16, ph)
                    dT = sb("dT", [128, 128], BF16, ph)
                    anys = sb("anys", [128, 64], F32, ph)
                    op("dve", lambda h: h.memset(dblk[:], 0.0), writes=["dblk"])
                    op("dve", lambda h: h.memset(dT[:], 0.0), writes=["dT"])

                    rlc = 0
                    ptc = 0
                    qcpb = [qcp, qcp2]

                    def prologue(g):
                        nonlocal rlc, ptc
                        b = g % 2
                        t0 = g * 512
                        mx = mixT[b]
                        mxk = "mixa%d" % b
                        op("sp", lambda h, b=b, t0=t0: h.dma_start(out=xT[b][:], in_=xT_s[s, :, :, t0:t0 + 512]), writes=["xTa%d" % b], dma="ldxT%d" % b)
                        for cc in range(6):
                            pk = cc % 2
                            for c in range(8):
                                op("pe", lambda h, pk=pk, cc=cc, c=c, b=b: h.matmul(ps[pk][:], wq[:, c, cc * 128:(cc + 1) * 128], xT[b][:, c, :], start=(c == 0), stop=(c == 7)),
                                   reads=["wq", "xTa%d" % b], writes=[psk[pk]], inc=(c == 7))
                            if cc < 2:
                                op("act", lambda h, pk=pk, cc=cc: h.activation(out=uT[:, cc, :], in_=ps[pk][:], func=AF.Gelu_apprx_tanh), reads=[psk[pk]], writes=["uT"])
                            elif cc < 4:
                                for k in range(2):
                                    hh = (cc - 2) * 2 + k
                                    op("dve", lambda h, pk=pk, hh=hh, k=k: h.tensor_copy(out=qcpb[g % 2][64 * k:64 * k + 64, hh, :], in_=ps[pk][64 * k:64 * k + 64, :]), reads=[psk[pk]], writes=["qcp%d" % (g % 2)])
                            else:
                                for k in range(4):
                                    hh = (cc - 4) * 4 + k
                                    op("dve" if k % 2 == 0 else "act",
                                       (lambda h, pk=pk, hh=hh, k=k: h.tensor_copy(out=qip[32 * k:32 * k + 32, hh, :], in_=ps[pk][32 * k:32 * k + 32, :])) if k % 2 == 0 else
                                       (lambda h, pk=pk, hh=hh, k=k: h.copy(out=qip[32 * k:32 * k + 32, hh, :], in_=ps[pk][32 * k:32 * k + 32, :])),
                                       reads=[psk[pk]], writes=["qip"])

                    def stageA1a(g, r):
                        nonlocal rlc, ptc
                        if True:
                            b = g % 2
                            t0 = g * 512
                            mx = mixT[b]
                            mxk = "mixa%d" % b
                            i = 4 * g + r
                            q0 = r * 128
                            nk = 128 * (i + 1)
                            for c in range(8):
                                op("pe", lambda h, c=c, b=b, q0=q0: h.matmul(ps[4][:, 0:264], xT[b][:, c, q0:q0 + 128], wv[:, c, :], start=(c == 0), stop=(c == 7)),
                                   reads=["wv", "xTa%d" % b], writes=[psk[4]], inc=(c == 7))
                            op("act", lambda h: h.activation(out=vg[:], in_=ps[4][:, 0:256], func=AF.Gelu_apprx_tanh), reads=[psk[4]], writes=["vg"])
                            op("dve", lambda h: h.tensor_copy(out=vtm[:, 256:264], in_=ps[4][:, 256:264]), reads=[psk[4]], writes=["vtm"])
                            op("dve", lambda h: h.tensor_scalar(out=wab[:], in0=vtm[:, 256:264], scalar1=-1.0, scalar2=None, op0=ALU.mult), reads=["vtm"], writes=["wab"])
                            op("dve", lambda h: h.tensor_tensor(out=wab[:], in0=wab[:], in1=vtm[:, 256:264], op=ALU.max), reads=["vtm", "wab"], writes=["wab"])
                            op("dve", lambda h: h.tensor_scalar(out=wab[:], in0=wab[:], scalar1=IDX_SCALE, scalar2=None, op0=ALU.mult), reads=["wab"], writes=["wab"])
                            op("dve", lambda h: h.tensor_scalar(out=sgn[:], in0=vtm[:, 256:264], scalar1=0.0, scalar2=2.0, op0=ALU.is_ge, op1=ALU.mult), reads=["vtm"], writes=["sgn"])
                            op("dve", lambda h: h.tensor_scalar(out=sgn[:], in0=sgn[:], scalar1=-1.0, scalar2=None, op0=ALU.add), reads=["sgn"], writes=["sgn"])
                            for hh in range(8):
                                op("dve", lambda h, hh=hh: h.tensor_scalar(out=dsg[:, hh, :], in0=ident[:], scalar1=sgn[:, hh:hh + 1], scalar2=None, op0=ALU.mult),
                                   reads=["ident", "sgn"], writes=["dsg%d" % hh])
                            v3 = vg[:].rearrange("p (g c) -> p g c", c=64)
                            op("dve", lambda h: h.tensor_reduce(out=st4[:, 0:4], in_=v3, axis=AX.X, op=ALU.add), reads=["vg"], writes=["st4"])
                            op("dve", lambda h: h.tensor_tensor(out=vsq[:], in0=vg[:], in1=vg[:], op=ALU.mult), reads=["vg"], writes=["vsq"])
                            op("dve", lambda h: h.tensor_reduce(out=st4[:, 4:8], in_=vsq[:].rearrange("p (g c) -> p g c", c=64), axis=AX.X, op=ALU.add), reads=["vsq"], writes=["st4"])
                            op("dve", lambda h: h.tensor_scalar(out=st4[:, 0:4], in0=st4[:, 0:4], scalar1=1.0 / 64, scalar2=None, op0=ALU.mult), reads=["st4"], writes=["st4"])
                            op("dve", lambda h: h.tensor_tensor(out=st4[:, 8:12], in0=st4[:, 0:4], in1=st4[:, 0:4], op=ALU.mult), reads=["st4"], writes=["st4"])
                            op("dve", lambda h: h.scalar_tensor_tensor(out=st4[:, 4:8], in0=st4[:, 4:8], scalar=1.0 / 64, in1=st4[:, 8:12], op0=ALU.mult, op1=ALU.subtract), reads=["st4"], writes=["st4"])
                            op("act", lambda h: h.activation(out=st4[:, 8:12], in_=st4[:, 4:8], func=AF.Ln, bias=EPS), reads=["st4"], writes=["st4"])
                            op("act", lambda h: h.activation(out=st4[:, 12:16], in_=st4[:, 8:12], func=AF.Exp, scale=-0.5), reads=["st4"], writes=["st4"])
                            for gg in range(4):
                                op("dve", lambda h, gg=gg: h.tensor_scalar(out=vsq[:, gg * 64:(gg + 1) * 64], in0=vg[:, gg * 64:(gg + 1) * 64], scalar1=st4[:, gg:gg + 1], scalar2=st4[:, 12 + gg:13 + gg],
                                                                    op0=ALU.subtract, op1=ALU.mult), reads=["vg", "st4"], writes=["vsq"])
                            op("dve", lambda h: h.tensor_tensor(out=vsq[:], in0=vsq[:], in1=lng[:], op=ALU.mult), reads=["vsq", "lng"], writes=["vsq"])
                            op("dve", lambda h: h.tensor_tensor(out=vn[:], in0=vsq[:], in1=lnb[:], op=ALU.add), reads=["vsq", "lnb"], writes=["vn"])
                            for gg in range(4):
                                ck = gg // 2
                                op("pe", lambda h, gg=gg, ck=ck: h.matmul(ps[5][:, (gg % 2) * 128:(gg % 2) * 128 + 128], vn[:, ck * 128:(ck + 1) * 128], wmT[:, gg, :], start=True, stop=True),
                                   reads=["vn", "wmT"], writes=[psk[5]], inc=True)
                                rs = slice((gg % 2) * 64, (gg % 2) * 64 + 64)
                                op("dve", lambda h, gg=gg, ck=ck, rs=rs: h.tensor_tensor(out=tmpa[rs, :], in0=ps[5][rs, (gg % 2) * 128:(gg % 2) * 128 + 128], in1=bsT[rs, ck, :], op=ALU.add),
                                   reads=[psk[5], "bsT"], writes=["tmpa"])
                                op("dve", lambda h, gg=gg, ck=ck, rs=rs, q0=q0: h.tensor_tensor(out=mx[rs, ck, q0:q0 + 128], in0=tmpa[rs, :], in1=uT[rs, ck, q0:q0 + 128], op=ALU.mult),
                                   reads=["tmpa", "uT"], writes=[mxk])

                    def stageA1b(g, r):
                        nonlocal rlc, ptc
                        if True:
                            b = g % 2
                            t0 = g * 512
                            mx = mixT[b]
                            mxk = "mixa%d" % b
                            i = 4 * g + r
                            q0 = r * 128
                            nk = 128 * (i + 1)
                            score = scoreb[i % 2]
                            sck = "score%d" % (i % 2)
                            nk = 128 * (i + 1)
                            if i >= 2:
                                for c0 in range(0, nk, 512):
                                    cw = min(512, nk - c0)
                                    def emit_L(hh, c0=c0, cw=cw, q0=q0):
                                        pk = 4 + hh % 2
                                        op("pe", lambda h: h.matmul(ps[pk][:, 0:cw], qip[:, hh, q0:q0 + 128], ki4[:, c0:c0 + cw], start=True, stop=True),
                                           reads=["qip", "ki4"], writes=[psk[pk]], inc=True)
                                    emit_L(0)
                                    for hh in range(8):
                                        pk = 4 + hh % 2
                                        if hh + 1 < 8:
                                            emit_L(hh + 1)
                                        rb = rlc % 4
                                        rlc += 1
                                        op("act", lambda h, pk=pk, hh=hh, cw=cw, rb=rb: h.activation(out=rl[rb][:, 0:cw], in_=ps[pk][:, 0:cw], func=AF.Relu, scale=wab[:, hh:hh + 1]),
                                           reads=[psk[pk], "wab"], writes=["rl%d" % rb])
                                        op("pe", lambda h, hh=hh, cw=cw, rb=rb: h.matmul(ps[6][:, 0:cw], dsg[:, hh, :], rl[rb][:, 0:cw], start=(hh == 0), stop=(hh == 7)),
                                           reads=["dsg%d" % hh, "rl%d" % rb], writes=[psk[6]], inc=(hh == 7))
                                    if c0 + cw == nk:
                                        op("pe", lambda h, cw=cw: h.matmul(ps[6][:, cw - 128:cw], ident[:], cmb[:], start=False, stop=True, skip_group_check=True),
                                           reads=["ident", "cmb"], writes=[psk[6]], inc=True)
                                    op("act", lambda h, c0=c0, cw=cw: h.copy(out=score[:, c0:c0 + cw], in_=ps[6][:, 0:cw]), reads=[psk[6]], writes=[sck])

                    def stageA2(g, r):
                        nonlocal rlc, ptc
                        if True:
                            b = g % 2
                            t0 = g * 512
                            mx = mixT[b]
                            mxk = "mixa%d" % b
                            i = 4 * g + r
                            q0 = r * 128
                            nk = 128 * (i + 1)
                            score = scoreb[i % 2]
                            sck = "score%d" % (i % 2)
                            if i >= 2:
                                op("dve", lambda h: h.memset(bis[:, 0:1], BIS_LO), writes=["bis"])
                                for it in range(NBIS):
                                    cst = BIS_W / (2.0 ** (it + 1))
                                    op("dve", lambda h, cst=cst: h.tensor_scalar(out=bis[:, 1:2], in0=bis[:, 0:1], scalar1=cst, scalar2=None, op0=ALU.add), reads=["bis"], writes=["bis"])
                                    op("dve", lambda h, nk=nk: h.tensor_scalar(out=mb[:, 0:nk], in0=score[:, 0:nk], scalar1=bis[:, 1:2], scalar2=None, op0=ALU.is_ge, op1=ALU.add, accum_out=bis[:, 2:3]),
                                       reads=[sck, "bis"], writes=["mb", "bis"])
                                    op("dve", lambda h, cst=cst: h.tensor_scalar(out=bis[:, 3:4], in0=bis[:, 2:3], scalar1=float(TOPK), scalar2=cst, op0=ALU.is_ge, op1=ALU.mult), reads=["bis"], writes=["bis"])
                                    op("dve", lambda h: h.tensor_tensor(out=bis[:, 0:1], in0=bis[:, 0:1], in1=bis[:, 3:4], op=ALU.add), reads=["bis"], writes=["bis"])
                                op("dve", lambda h, nk=nk: h.tensor_scalar(out=mb[:, 0:nk], in0=score[:, 0:nk], scalar1=bis[:, 0:1], scalar2=NEG, op0=ALU.is_lt, op1=ALU.mult), reads=[sck, "bis"], writes=["mb"])
                                nb = i + 1
                                op("dve", lambda h, nk=nk, nb=nb: h.tensor_reduce(out=anys[:, 0:nb], in_=mb[:, 0:nk].rearrange("p (j k) -> p j k", k=128), axis=AX.X, op=ALU.max), reads=["mb"], writes=["anys"])
                                op("dve", lambda h, nb=nb: h.tensor_scalar(out=anys[:, 0:nb], in0=anys[:, 0:nb], scalar1=-1.0, scalar2=None, op0=ALU.is_ge), reads=["anys"], writes=["anys"])
                                op("dve", lambda h, nb=nb: h.tensor_tensor(out=anys[:, 0:nb], in0=anys[:, 0:nb], in1=jpos1[:, 0:nb], op=ALU.mult), reads=["anys", "jpos1"], writes=["anys"])
                                op("dve", lambda h, nb=nb: h.tensor_reduce(out=anys[:, 32:33], in_=anys[:, 0:nb], axis=AX.X, op=ALU.max), reads=["anys"], writes=["anys"])
                                op("dve", lambda h, nb=nb: h.tensor_scalar(out=dblk[:, 0:1], in0=anys[:, 32:33], scalar1=-128.0, scalar2=128.0 * nb, op0=ALU.mult, op1=ALU.add), reads=["anys"], writes=["dblk"])

                    def stageB1(g, r):
                        nonlocal rlc, ptc
                        if True:
                            b = g % 2
                            t0 = g * 512
                            mx = mixT[b]
                            mxk = "mixa%d" % b
                            i = 4 * g + r
                            q0 = r * 128
                            nk = 128 * (i + 1)
                            if i >= 2:
                                for j0 in range(0, i + 1, 8):
                                    nj = min(8, i + 1 - j0)
                                    for jj in range(nj):
                                        op("pe", lambda h, j0=j0, jj=jj: h.transpose(pst[:, jj * 128:(jj + 1) * 128], mb[:, (j0 + jj) * 128:(j0 + jj + 1) * 128], ident[:]),
                                           reads=["mb", "ident"], writes=["pst"], inc=(jj == nj - 1))
                                    op("act" if (j0 // 8) % 2 == 0 else "dve",
                                       (lambda h, j0=j0, nj=nj: h.copy(out=mbT[:, j0:j0 + nj, :], in_=pst[:, 0:nj * 128].rearrange("p (j t) -> p j t", t=128))) if (j0 // 8) % 2 == 0 else
                                       (lambda h, j0=j0, nj=nj: h.tensor_copy(out=mbT[:, j0:j0 + nj, :], in_=pst[:, 0:nj * 128].rearrange("p (j t) -> p j t", t=128))),
                                       reads=["pst"], writes=["mbT"])
                                op("pe", lambda h: h.transpose(pst[:, 0:128], dblk[:], ident[:]), reads=["dblk", "ident"], writes=["pst"], inc=True)
                                op("dve", lambda h: h.tensor_copy(out=dT[:], in_=pst[:, 0:128]), reads=["pst"], writes=["dT"])

                    def stageB2(g, r):
                        nonlocal rlc, ptc
                        if True:
                            b = g % 2
                            t0 = g * 512
                            mx = mixT[b]
                            mxk = "mixa%d" % b
                            i = 4 * g + r
                            q0 = r * 128
                            nk = 128 * (i + 1)
                            if debug and l == 0 and s == 0 and i == 2:
                                dump("vn", vn[:], 256, ["vn"])
                                dump("wmT", wmT[:].rearrange("p a b -> p (a b)"), 512, ["wmT"])
                                dump("st4", st4[:], 16, ["st4"])
                                dump("tmpa", tmpa[:], 128, ["tmpa"])
                                dump("vg", vg[:], 256, ["vg"])
                                dump("uT", uT[:].rearrange("p a b -> p (a b)")[:, 0:512], 512, ["uT"])
                                dump("score", scoreb[0][:, 0:384], 384, ["score0"])
                                dump("bis", bis[:], 8, ["bis"])
                                dump("mb", mb[:, 0:384], 384, ["mb"])
                                dump("mbT", mbT[:].rearrange("p a b -> p (a b)")[:, 0:384], 384, ["mbT"])
                                dump("anys", anys[:], 64, ["anys"])
                                dump("dT", dT[:], 128, ["dT"])
                                dump("wab", wab[:], 8, ["wab"])
                                dump("sgn", sgn[:], 8, ["sgn"])
                                dump("bsT", bsT[:].rearrange("p a b -> p (a b)"), 256, ["bsT"])
                            pend = []

                            def emit_pv_c(item, i=i):
                                j, pslot = item
                                first, last = (j == 0), (j == i)
                                pk_ = "pTa%d" % pslot
                                for hh in range(4):
                                    ab = hh // 2
                                    co = (hh % 2) * 256
                                    op("pe", lambda h: h.matmul(ps[ab][:, co:co + 128], vc[:, j, ab * 128:(ab + 1) * 128], pT[pslot][:, hh, :], start=(first and hh % 2 == 0), stop=last, skip_group_check=True),
                                       reads=["vc", pk_], writes=[psk[ab]], inc=False)
                                    op("pe", lambda h: h.matmul(ps[ab][:, co + 128:co + 256], ones[:], pT[pslot][:, hh, :], start=False, stop=last, skip_group_check=True),
                                       reads=["ones", pk_], writes=[psk[ab]], inc=(hh == 3))

                            for j in range(0, i + 1):
                                sbk = 2 + (ptc % 2)
                                pslot = ptc % 3
                                ptc += 1
                                dg = (j == i)
                                for hh in range(4):
                                    ck = hh // 2
                                    cs = hh * 128
                                    op("pe", lambda h, sbk=sbk, cs=cs, hh=hh, j=j, ck=ck: h.matmul(ps[sbk][:, cs:cs + 128], ktc[:, ck, j * 128:(j + 1) * 128], qcpb[g % 2][:, hh, q0:q0 + 128], start=(hh == 0), stop=False, skip_group_check=True),
                                       reads=["ktc", "qcp%d" % (g % 2)], writes=[psk[sbk]], inc=False)
                                    op("pe", lambda h, sbk=sbk, cs=cs, j=j: h.matmul(ps[sbk][:, cs:cs + 128], ident[:], mbT[:, j, :], start=False, stop=False, skip_group_check=True),
                                       reads=["ident", "mbT"], writes=[psk[sbk]], inc=False)
                                    op("pe", lambda h, sbk=sbk, cs=cs, hh=hh, dg=dg: h.matmul(ps[sbk][:, cs:cs + 128], slmat[:, hh, :], dT[:], start=False, stop=(not dg), skip_group_check=True),
                                       reads=["slmat", "dT"], writes=[psk[sbk]], inc=((not dg) and hh == 3))
                                    if dg:
                                        op("pe", lambda h, sbk=sbk, cs=cs, hh=hh: h.matmul(ps[sbk][:, cs:cs + 128], ident[:], diag[:, hh, :], start=False, stop=True, skip_group_check=True),
                                           reads=["ident", "diag"], writes=[psk[sbk]], inc=(hh == 3))
                                if dg:
                                    op("act", lambda h, sbk=sbk, pslot=pslot: h.activation(out=pT[pslot][:].rearrange("p a b -> p (a b)"), in_=ps[sbk][:], func=AF.Exp, scale=0.125),
                                       reads=[psk[sbk]], writes=["pTa%d" % pslot])
                                else:
                                    kk = i - j + 1
                                    for hh in range(4):
                                        op("act", lambda h, sbk=sbk, pslot=pslot, hh=hh, kk=kk: h.activation(out=pT[pslot][:, hh, :], in_=ps[sbk][:, hh * 128:(hh + 1) * 128], func=AF.Exp, scale=0.125,
                                                                                                    bias=alibi[:, hh * 34 + kk:hh * 34 + kk + 1]),
                                           reads=[psk[sbk], "alibi"], writes=["pTa%d" % pslot])
                                pend.append((j, pslot))
                                if len(pend) > 1:
                                    emit_pv_c(pend.pop(0))
                            while pend:
                                emit_pv_c(pend.pop(0))
                            for hh in range(4):
                                ab = hh // 2
                                co = (hh % 2) * 256
                                rs = slice(64 * (hh % 2), 64 * (hh % 2) + 64)
                                op("dve", lambda h, ab=ab, co=co, rs=rs: h.reciprocal(out=rcp[rs, :], in_=ps[ab][rs, co + 128:co + 256]), reads=[psk[ab]], writes=["rcp"])
                                op("dve", lambda h, ab=ab, co=co, rs=rs, q0=q0: h.tensor_tensor(out=mx[rs, 2 + ab, q0:q0 + 128], in0=ps[ab][rs, co:co + 128], in1=rcp[rs, :], op=ALU.mult),
                                   reads=[psk[ab], "rcp"], writes=[mxk])
                            if r == 3:
                                op("sp", lambda h, b=b, t0=t0: h.dma_start(out=mixT_s[s, :, 0:2, t0:t0 + 512], in_=mixT[b][:, 0:2, :]), reads=[mxk], writes=["mixT_s"], dma="st_mx", nowaw=True)
                                op("sp", lambda h, b=b, t0=t0: h.dma_start(out=mixT_s[s, :, 6:8, t0:t0 + 512], in_=mixT[b][:, 2:4, :]), reads=[mxk], writes=["mixT_s"], dma="st_mx", nowaw=True)

                    tiles = [(g_, r_) for g_ in range(NGD) for r_ in range(4)]
                    NTL = len(tiles)
                    prologue(0)
                    stageA1a(*tiles[0])
                    stageA1b(*tiles[0])
                    stageA2(*tiles[0])
                    if NTL > 1:
                        stageA1a(*tiles[1])
                        stageA1b(*tiles[1])
                    for ti in range(NTL):
                        stageB1(*tiles[ti])
                        if ti + 2 < NTL:
                            if tiles[ti + 2][1] == 0:
                                prologue(tiles[ti + 2][0])
                            stageA1a(*tiles[ti + 2])
                        if ti + 1 < NTL:
                            stageA2(*tiles[ti + 1])
                        if ti + 2 < NTL:
                            stageA1b(*tiles[ti + 2])
                        stageB2(*tiles[ti])
                    P.barrier()
                    phase_ctr[0] += 1
                    if stop is not None and phase_ctr[0] >= stop:
                        P.stopped = True

                with ExitStack() as ph:
                    ktb = sb("ktb", [128, 4, S], BF16, ph)
                    vb = sb("vb", [128, NT, 512], BF16, ph)
                    op("sp", lambda h: h.dma_start(out=ktb[:], in_=ktb_s[s]), writes=["ktb"], dma="ldkv")
                    op("sp", lambda h: h.dma_start(out=vb[:], in_=vb_s[s]), writes=["vb"], dma="ldkv")
                    wq = sb("wqb", [128, 8, 512], BF16, ph)
                    for c in range(8):
                        op("pool", lambda h, c=c: h.dma_start(out=wq[:, c, :], in_=w_in[l, c * 128:(c + 1) * 128, OFF_B_Q:OFF_B_Q + 512]), writes=["wqb"], dma="wq", nowaw=True)
                    lv = sb("lv", [128, 4, 64], F32, ph)
                    lsm = sb("lsm", [128, 8], F32, ph)
                    gcol = sb("gcol", [128, 1], F32, ph)
                    op("sp", lambda h: h.dma_start(out=lv[:].rearrange("p a b -> p (a b)"), in_=lamv[l:l + 1].rearrange("o a b -> o (a b)").partition_broadcast(128)), writes=["lv"], dma="gp")
                    op("sp", lambda h: h.dma_start(out=gcol[:], in_=subg[l]), writes=["gcol"], dma="gp")
                    op("dve", lambda h: h.tensor_tensor(out=lv[:, 0, :], in0=lv[:, 0, :], in1=lv[:, 1, :], op=ALU.mult), reads=["lv"], writes=["lv"])
                    op("dve", lambda h: h.tensor_tensor(out=lv[:, 2, :], in0=lv[:, 2, :], in1=lv[:, 3, :], op=ALU.mult), reads=["lv"], writes=["lv"])
                    op("dve", lambda h: h.tensor_reduce(out=lsm[:, 0:1], in_=lv[:, 0, :], axis=AX.X, op=ALU.add), reads=["lv"], writes=["lsm"])
                    op("dve", lambda h: h.tensor_reduce(out=lsm[:, 1:2], in_=lv[:, 2, :], axis=AX.X, op=ALU.add), reads=["lv"], writes=["lsm"])
                    op("act", lambda h: h.activation(out=lsm[:, 2:4], in_=lsm[:, 0:2], func=AF.Exp), reads=["lsm"], writes=["lsm2"])
                    op("dve", lambda h: h.tensor_tensor(out=lsm[:, 4:5], in0=lsm[:, 3:4], in1=lsm[:, 2:3], op=ALU.subtract), reads=["lsm2"], writes=["lsm3"])
                    op("dve", lambda h: h.tensor_scalar(out=lsm[:, 5:6], in0=lsm[:, 4:5], scalar1=-lam_init, scalar2=None, op0=ALU.add), reads=["lsm3"], writes=["neglam"])
                    xT = [sb("xTb%d" % i, [128, 8, 512], BF16, ph) for i in range(2)]
                    qbp = sb("qbp", [128, 8, 512], BF16, ph)
                    pT = [sb("pTb%d" % i, [128, 4, 128], BF16, ph) for i in range(3)]
                    mixT = [sb("mixb%d" % i, [128, 4, 512], BF16, ph) for i in range(2)]
                    r1 = sb("r1", [128, 256], F32, ph)
                    oa = sb("oa", [128, 128], F32, ph)
                    ob = sb("ob", [128, 128], F32, ph)
                    oo = sb("oo", [128, 128], F32, ph)
                    osq = sb("osq", [128, 128], BF16, ph)
                    rsd = sb("rsd", [128, 128], F32, ph)
                    r1g = sb("r1g", [128, 1024], F32, ph)
                    oag = sb("oag", [128, 512], F32, ph)
                    obg = sb("obg", [128, 512], F32, ph)
                    oog = sb("oog", [128, 512], F32, ph)
                    osqg = sb("osqg", [128, 512], BF16, ph)
                    rsdg = sb("rsdg", [128, 512], F32, ph)
                    op("dve", lambda h: h.memset(qbp[:], 0.0), writes=["qbp"])
                    ptc = 0
                    for g in range(NGD):
                        b = g % 2
                        t0 = g * 512
                        mx = mixT[b]
                        mxk = "mixb%d" % b
                        op("sp", lambda h, b=b, t0=t0: h.dma_start(out=xT[b][:], in_=xT_s[s, :, :, t0:t0 + 512]), writes=["xTb%d" % b], dma="ldxT%d" % b)
                        for cc in range(4):
                            pk = cc % 2
                            for c in range(8):
                                op("pe", lambda h, pk=pk, cc=cc, c=c, b=b: h.matmul(ps[pk][:], wq[:, c, cc * 128:(cc + 1) * 128], xT[b][:, c, :], start=(c == 0), stop=(c == 7)),
                                   reads=["wqb", "xTb%d" % b], writes=[psk[pk]], inc=(c == 7))
                            op("dve", lambda h, pk=pk, cc=cc: h.tensor_copy(out=qbp[0:64, 2 * cc, :], in_=ps[pk][0:64, :]), reads=[psk[pk]], writes=["qbp"])
                            op("act", lambda h, pk=pk, cc=cc: h.copy(out=qbp[64:128, 2 * cc + 1, :], in_=ps[pk][64:128, :]), reads=[psk[pk]], writes=["qbp"])
                        for r in range(4):
                            i = 4 * g + r
                            q0 = r * 128
                            for pp in range(1):
                                hA, hB = 2 * pp, 2 * pp + 1
                                jlo = max(0, i - max(WIN[hA], WIN[hB]))
                                pend = []

                                def emit_pv_b(item, i=i):
                                    j, heads, pslot = item
                                    pk_ = "pTb%d" % pslot
                                    last = (j == i)
                                    for hi_, hh in enumerate(heads):
                                        ab = ACCB[hh]
                                        first = (j == max(0, i - WIN[hh]))
                                        for m in range(2):
                                            blk = (hh % 2) * 2 + m
                                            op("pe", lambda h: h.matmul(ps[ab][:, m * 256:m * 256 + 128], vb[:, j, hh * 128:(hh + 1) * 128], pT[pslot][:, blk, :], start=(first and m == 0), stop=last, skip_group_check=True),
                                               reads=["vb", pk_], writes=[psk[ab]], inc=False)
                                            op("pe", lambda h: h.matmul(ps[ab][:, m * 256 + 128:m * 256 + 256], ones[:], pT[pslot][:, blk, :], start=False, stop=last, skip_group_check=True),
                                               reads=["ones", pk_], writes=[psk[ab]], inc=(hi_ == len(heads) - 1 and m == 1))

                                for j in range(jlo, i + 1):
                                    heads = [hh for hh in (hA, hB) if j >= i - WIN[hh]]
                                    sbk = 2 + (ptc % 3)
                                    pslot = ptc % 3
                                    ptc += 1
                                    dg = (j == i)
                                    firstmm = True
                                    nmm = len(heads) * 2
                                    cnt_ = 0
                                    for hh in heads:
                                        for m in range(2):
                                            cs = ((hh % 2) * 2 + m) * 128
                                            cnt_ += 1
                                            lastmm = (cnt_ == nmm)
                                            op("pe", lambda h, sbk=sbk, cs=cs, hh=hh, m=m, j=j, dg=dg, fm=firstmm: h.matmul(ps[sbk][:, cs:cs + 128], ktb[:, hh, j * 128:(j + 1) * 128], qbp[:, 2 * hh + m, q0:q0 + 128], start=fm, stop=(not dg), skip_group_check=True),
                                               reads=["ktb", "qbp"], writes=[psk[sbk]], inc=((not dg) and lastmm))
                                            firstmm = False
                                            if dg:
                                                op("pe", lambda h, sbk=sbk, cs=cs, hh=hh: h.matmul(ps[sbk][:, cs:cs + 128], ident[:], diag[:, hh, :], start=False, stop=True, skip_group_check=True),
                                                   reads=["ident", "diag"], writes=[psk[sbk]], inc=lastmm)
                                    c_lo = (heads[0] % 2) * 256
                                    c_hi = (heads[-1] % 2) * 256 + 256
                                    if dg:
                                        op("act", lambda h, sbk=sbk, pslot=pslot, c_lo=c_lo, c_hi=c_hi: h.activation(out=pT[pslot][:].rearrange("p a b -> p (a b)")[:, c_lo:c_hi], in_=ps[sbk][:, c_lo:c_hi], func=AF.Exp, scale=0.125),
                                           reads=[psk[sbk]], writes=["pTb%d" % pslot])
                                    else:
                                        kk = i - j + 1
                                        for hh in heads:
                                            cl = (hh % 2) * 256
                                            op("act", lambda h, sbk=sbk, pslot=pslot, hh=hh, kk=kk, cl=cl: h.activation(out=pT[pslot][:].rearrange("p a b -> p (a b)")[:, cl:cl + 256], in_=ps[sbk][:, cl:cl + 256], func=AF.Exp, scale=0.125,
                                                                                                               bias=alibi[:, hh * 34 + kk:hh * 34 + kk + 1]),
                                               reads=[psk[sbk], "alibi"], writes=["pTb%d" % pslot])
                                    pend.append((j, heads, pslot))
                                    if len(pend) > 2:
                                        emit_pv_b(pend.pop(0))
                                while pend:
                                    emit_pv_b(pend.pop(0))
                                for hh in (hA, hB):
                                    pa = ACCB[hh]
                                    A = ps[pa]
                                    op("dve", lambda h, A=A: h.reciprocal(out=r1[:, 0:128], in_=A[:, 128:256]), reads=[psk[pa]], writes=["r1"])
                                    op("dve", lambda h, A=A: h.reciprocal(out=r1[:, 128:256], in_=A[:, 384:512]), reads=[psk[pa]], writes=["r1"])
                                    op("dve", lambda h, A=A: h.tensor_tensor(out=oa[:], in0=A[:, 0:128], in1=r1[:, 0:128], op=ALU.mult), reads=[psk[pa], "r1"], writes=["oa"])
                                    op("dve", lambda h, A=A: h.tensor_tensor(out=ob[:], in0=A[:, 256:384], in1=r1[:, 128:256], op=ALU.mult), reads=[psk[pa], "r1"], writes=["ob"])
                                    op("dve", lambda h: h.scalar_tensor_tensor(out=oo[:], in0=ob[:], scalar=lsm[:, 5:6], in1=oa[:], op0=ALU.mult, op1=ALU.add), reads=["oa", "ob", "neglam"], writes=["oo"])
                                    op("pool", lambda h: h.tensor_tensor(out=osq[:], in0=oo[:], in1=oo[:], op=ALU.mult), reads=["oo"], writes=["osq"])
                                    op("pe", lambda h, pa=pa: h.matmul(ps[pa][:, 0:128], ones[:], osq[:], start=True, stop=True), reads=["ones", "osq"], writes=[psk[pa]], inc=True)
                                    op("act", lambda h, pa=pa: h.activation(out=rsd[:], in_=ps[pa][:, 0:128], func=AF.Ln, scale=1.0 / 128, bias=EPS), reads=[psk[pa]], writes=["rsd"])
                                    op("act", lambda h: h.activation(out=rsd[:], in_=rsd[:], func=AF.Exp, scale=-0.5), reads=["rsd"], writes=["rsd"])
                                    op("dve", lambda h: h.tensor_tensor(out=oo[:], in0=oo[:], in1=rsd[:], op=ALU.mult), reads=["oo", "rsd"], writes=["oo"])
                                    op("dve", lambda h, hh=hh, q0=q0: h.tensor_scalar(out=mx[:, hh, q0:q0 + 128], in0=oo[:], scalar1=gcol[:, 0:1], scalar2=(1.0 - lam_init), op0=ALU.mult, op1=ALU.mult),
                                       reads=["oo", "gcol"], writes=[mxk])
                        for hh in (2, 3):
                            aO = [0, 5]
                            aS = [1, 6]
                            jl = 4 * g + 3
                            pendg = []

                            def emit_pv_g(item, hh=hh, jl=jl, aO=aO, aS=aS):
                                j, m, pslot, c0 = item
                                first, last = (j == 0), (j == jl)
                                pk_ = "pTb%d" % pslot
                                pf = pT[pslot][:].rearrange("p a b -> p (a b)")
                                op("pe", lambda h: h.matmul(ps[aO[m]][:, c0:512], vb[:, j, hh * 128:(hh + 1) * 128], pf[:, c0:512], start=first, stop=last),
                                   reads=["vb", pk_], writes=[psk[aO[m]]], inc=False)
                                op("pe", lambda h: h.matmul(ps[aS[m]][:, c0:512], ones[:], pf[:, c0:512], start=first, stop=last),
                                   reads=["ones", pk_], writes=[psk[aS[m]]], inc=True)

                            for j in range(0, jl + 1):
                                rd = j - 4 * g
                                c0 = 128 * max(0, rd)
                                kk = 4 * g + 4 - j
                                for m in range(2):
                                    sbk = 2 + (ptc % 3)
                                    pslot = ptc % 3
                                    ptc += 1
                                    pf = pT[pslot][:].rearrange("p a b -> p (a b)")
                                    pk_ = "pTb%d" % pslot
                                    op("pe", lambda h, sbk=sbk, c0=c0, j=j, m=m, hh=hh, rd=rd: h.matmul(ps[sbk][:, c0:512], ktb[:, hh, j * 128:(j + 1) * 128], qbp[:, 2 * hh + m, c0:512], start=True, stop=(rd < 0), skip_group_check=True),
                                       reads=["ktb", "qbp"], writes=[psk[sbk]], inc=(rd < 0))
                                    if rd >= 0:
                                        op("pe", lambda h, sbk=sbk, c0=c0, hh=hh: h.matmul(ps[sbk][:, c0:c0 + 128], ident[:], diag[:, hh, :], start=False, stop=True, skip_group_check=True),
                                           reads=["ident", "diag"], writes=[psk[sbk]], inc=True)
                                        op("act", lambda h, sbk=sbk, c0=c0, pf=pf, hh=hh, rd=rd: h.activation(out=pf[:, c0:c0 + 128], in_=ps[sbk][:, c0:c0 + 128], func=AF.Exp, scale=0.125, bias=float(-SLOPES[hh] * 128.0 * (3 - rd))),
                                           reads=[psk[sbk]], writes=[pk_])
                                        if c0 + 128 < 512:
                                            op("act", lambda h, sbk=sbk, c0=c0, pf=pf, hh=hh, kk=kk: h.activation(out=pf[:, c0 + 128:512], in_=ps[sbk][:, c0 + 128:512], func=AF.Exp, scale=0.125, bias=alibi[:, hh * 34 + kk:hh * 34 + kk + 1]),
                                               reads=[psk[sbk], "alibi"], writes=[pk_])
                                    else:
                                        op("act", lambda h, sbk=sbk, pf=pf, hh=hh, kk=kk: h.activation(out=pf[:, 0:512], in_=ps[sbk][:, 0:512], func=AF.Exp, scale=0.125, bias=alibi[:, hh * 34 + kk:hh * 34 + kk + 1]),
                                           reads=[psk[sbk], "alibi"], writes=[pk_])
                                    pendg.append((j, m, pslot, c0))
                                    if len(pendg) > 2:
                                        emit_pv_g(pendg.pop(0))
                            while pendg:
                                emit_pv_g(pendg.pop(0))
                            k0, k1, k2, k3 = psk[aO[0]], psk[aS[0]], psk[aO[1]], psk[aS[1]]
                            op("dve", lambda h, aS=aS: h.reciprocal(out=r1g[:, 0:512], in_=ps[aS[0]][:]), reads=[k1], writes=["r1g"])
                            op("dve", lambda h, aS=aS: h.reciprocal(out=r1g[:, 512:1024], in_=ps[aS[1]][:]), reads=[k3], writes=["r1g"])
                            op("dve", lambda h, aO=aO: h.tensor_tensor(out=oag[:], in0=ps[aO[0]][:], in1=r1g[:, 0:512], op=ALU.mult), reads=[k0, "r1g"], writes=["oag"])
                            op("dve", lambda h, aO=aO: h.tensor_tensor(out=obg[:], in0=ps[aO[1]][:], in1=r1g[:, 512:1024], op=ALU.mult), reads=[k2, "r1g"], writes=["obg"])
                            op("dve", lambda h: h.scalar_tensor_tensor(out=oog[:], in0=obg[:], scalar=lsm[:, 5:6], in1=oag[:], op0=ALU.mult, op1=ALU.add), reads=["oag", "obg", "neglam"], writes=["oog"])
                            op("pool", lambda h: h.tensor_tensor(out=osqg[:], in0=oog[:], in1=oog[:], op=ALU.mult), reads=["oog"], writes=["osqg"])
                            op("pe", lambda h, aO=aO: h.matmul(ps[aO[0]][:], ones[:], osqg[:], start=True, stop=True), reads=["ones", "osqg"], writes=[k0], inc=True)
                            op("act", lambda h, aO=aO: h.activation(out=rsdg[:], in_=ps[aO[0]][:], func=AF.Ln, scale=1.0 / 128, bias=EPS), reads=[k0], writes=["rsdg"])
                            op("act", lambda h: h.activation(out=rsdg[:], in_=rsdg[:], func=AF.Exp, scale=-0.5), reads=["rsdg"], writes=["rsdg"])
                            op("dve", lambda h: h.tensor_tensor(out=oog[:], in0=oog[:], in1=rsdg[:], op=ALU.mult), reads=["oog", "rsdg"], writes=["oog"])
                            op("dve", lambda h, hh=hh: h.tensor_scalar(out=mx[:, hh, :], in0=oog[:], scalar1=gcol[:, 0:1], scalar2=(1.0 - lam_init), op0=ALU.mult, op1=ALU.mult),
                               reads=["oog", "gcol"], writes=[mxk])
                        op("sp", lambda h, b=b, t0=t0: h.dma_start(out=mixT_s[s, :, 2:6, t0:t0 + 512], in_=mixT[b][:]), reads=[mxk], writes=["mixT_s"], dma="st_mx", nowaw=True)
                    P.barrier()
                    phase_ctr[0] += 1
                    if stop is not None and phase_ctr[0] >= stop:
                        P.stopped = True

            with ExitStack() as ph:
                wo = sb("wo", [128, 8, D], BF16, ph)
                wd = sb("wd", [128, NF, D], BF16, ph)
                for c in range(8):
                    op("pool", lambda h, c=c: h.dma_start(out=wo[:, c, :], in_=w_out[l, c * 128:(c + 1) * 128, :]), writes=["wo"], dma="wq", nowaw=True)
                for f in range(NF):
                    op("pool", lambda h, f=f: h.dma_start(out=wd[:, f, :], in_=w_dn[l, f * 128:(f + 1) * 128, :]), writes=["wd"], dma="wq", nowaw=True)
                lnp = sb("lnp", [128, 4, D], F32, ph)
                for k, src in enumerate((ln1g, ln1b, ln2g, ln2b)):
                    op("sp", lambda h, k=k, src=src: h.dma_start(out=lnp[:, k, :], in_=src[l].partition_broadcast(128)), writes=["lnp"], dma="gp", nowaw=True)
                wg = [sb("wg%d" % i, [128, 8, 256], BF16, ph) for i in range(3)]
                mxT = [sb("mxT%d" % i, [128, 8, 512], BF16, ph) for i in range(2)]
                xin = [sb("xr%d" % i, [128, D], F32, ph) for i in range(2)]
                x1 = sb("x1", [128, 4, D], F32, ph)
                x1b = sb("x1b", [128, D], BF16, ph)
                x1T = sb("x1T", [128, 8, 512], BF16, ph)
                actT = sb("actT", [128, NF, 512], BF16, ph)
                sg = [sb("sg%d" % i, [128, 512], F32, ph) for i in range(2)]
                zt = sb("zt", [128, D], F32, ph)
                yo = [sb("yo%d" % i, [128, D], F32, ph) for i in range(2)]
                bst = sb("bst", [128, 16], F32, ph)

                def layer_norm(zin, zkey, gk, bk, out_ap, out_key):
                    op("dve", lambda h: h.bn_stats(out=bst[:, 0:6], in_=zin[:, 0:512]), reads=[zkey], writes=["bst"])
                    op("dve", lambda h: h.bn_stats(out=bst[:, 6:12], in_=zin[:, 512:1024]), reads=[zkey], writes=["bst"])
                    op("dve", lambda h: h.bn_aggr(out=bst[:, 12:14], in_=bst[:, 0:12]), reads=["bst"], writes=["bst2"])
                    op("act", lambda h: h.activation(out=bst[:, 14:15], in_=bst[:, 13:14], func=AF.Ln, bias=EPS), reads=["bst2"], writes=["bst3"])
                    op("act", lambda h: h.activation(out=bst[:, 15:16], in_=bst[:, 14:15], func=AF.Exp, scale=-0.5), reads=["bst3"], writes=["bst4"])
                    op("dve", lambda h: h.tensor_scalar(out=zin[:], in0=zin[:], scalar1=bst[:, 12:13], scalar2=bst[:, 15:16], op0=ALU.subtract, op1=ALU.mult), reads=[zkey, "bst2", "bst4"], writes=[zkey])
                    op("pool", lambda h: h.tensor_tensor(out=zin[:], in0=zin[:], in1=lnp[:, gk, :], op=ALU.mult), reads=[zkey, "lnp"], writes=[zkey])
                    op("dve", lambda h: h.tensor_tensor(out=out_ap, in0=zin[:], in1=lnp[:, bk, :], op=ALU.add), reads=[zkey, "lnp"], writes=[out_key])

                gi = 0
                xc = 0
                wc = 0
                yc = 0
                for s in range(NSEQ):
                    for g in range(NGD):
                        b = gi % 2
                        gi += 1
                        t0 = g * 512
                        op("sp", lambda h, b=b, t0=t0, s=s: h.dma_start(out=mxT[b][:], in_=mixT_s[s, :, :, t0:t0 + 512]), writes=["mxT%d" % b], dma="ldmx%d" % b)
                        for r in range(4):
                            xb = xc % 2
                            xc += 1
                            tt = t0 + r * 128
                            op("sp", lambda h, xb=xb, tt=tt, s=s: h.dma_start(out=xin[xb][:], in_=x_src[s, tt:tt + 128, :]), writes=["xr%d" % xb], dma="ldxr%d" % xb)
                            for hf in range(2):
                                for c in range(8):
                                    op("pe", lambda h, hf=hf, c=c, b=b, r=r: h.matmul(ps[hf][:], mxT[b][:, c, r * 128:(r + 1) * 128], wo[:, c, hf * 512:(hf + 1) * 512], start=(c == 0), stop=(c == 7)),
                                       reads=["mxT%d" % b, "wo"], writes=[psk[hf]], inc=(c == 7))
                                op("dve", lambda h, hf=hf, xb=xb: h.scalar_tensor_tensor(out=zt[:, hf * 512:(hf + 1) * 512], in0=xin[xb][:, hf * 512:(hf + 1) * 512], scalar=ALPHA, in1=ps[hf][:], op0=ALU.mult, op1=ALU.add),
                                   reads=["xr%d" % xb, psk[hf]], writes=["zt"])
                            layer_norm(zt, "zt", 0, 1, x1[:, r, :], "x1_%d" % r)
                            op("act", lambda h, r=r: h.copy(out=x1b[:], in_=x1[:, r, :]), reads=["x1_%d" % r], writes=["x1b"])
                            for c in range(8):
                                op("pe", lambda h, c=c: h.transpose(pst[:, c * 128:(c + 1) * 128], x1b[:, c * 128:(c + 1) * 128], ident[:]), reads=["x1b", "ident"], writes=["pst"], inc=(c == 7))
                            op("act", lambda h, r=r: h.copy(out=x1T[:, :, r * 128:(r + 1) * 128], in_=pst[:].rearrange("p (c t) -> p c t", c=8)), reads=["pst"], writes=["x1T"])
                        for f in range(NF):
                            wb_ = wc % 3
                            wc += 1
                            op("sp", lambda h, wb_=wb_, f=f: h.dma_start(out=wg[wb_][:], in_=wgu_s[l, f]), writes=["wg%d" % wb_], dma="ldwg%d" % wb_)
                            pg, pu = 2 + 2 * (f % 2), 3 + 2 * (f % 2)
                            for c in range(8):
                                op("pe", lambda h, pg=pg, c=c, wb_=wb_: h.matmul(ps[pg][:], wg[wb_][:, c, 0:128], x1T[:, c, :], start=(c == 0), stop=(c == 7)),
                                   reads=["wg%d" % wb_, "x1T"], writes=[psk[pg]], inc=(c == 7))
                            for c in range(8):
                                op("pe", lambda h, pu=pu, c=c, wb_=wb_: h.matmul(ps[pu][:], wg[wb_][:, c, 128:256], x1T[:, c, :], start=(c == 0), stop=(c == 7)),
                                   reads=["wg%d" % wb_, "x1T"], writes=[psk[pu]], inc=(c == 7))
                            sb_ = f % 2
                            op("act", lambda h, pg=pg, sb_=sb_: h.activation(out=sg[sb_][:], in_=ps[pg][:], func=AF.Silu), reads=[psk[pg]], writes=["sg%d" % sb_])
                            op("dve", lambda h, pu=pu, sb_=sb_, f=f: h.tensor_tensor(out=actT[:, f, :], in0=sg[sb_][:], in1=ps[pu][:], op=ALU.mult), reads=["sg%d" % sb_, psk[pu]], writes=["actT"])
                        for r in range(4):
                            tt = t0 + r * 128
                            for hf in range(2):
                                for f in range(NF):
                                    op("pe", lambda h, hf=hf, f=f, r=r: h.matmul(ps[hf][:], actT[:, f, r * 128:(r + 1) * 128], wd[:, f, hf * 512:(hf + 1) * 512], start=(f == 0), stop=(f == NF - 1)),
                                       reads=["actT", "wd"], writes=[psk[hf]], inc=(f == NF - 1))
                                op("dve", lambda h, hf=hf, r=r: h.scalar_tensor_tensor(out=zt[:, hf * 512:(hf + 1) * 512], in0=x1[:, r, hf * 512:(hf + 1) * 512], scalar=ALPHA, in1=ps[hf][:], op0=ALU.mult, op1=ALU.add),
                                   reads=["x1_%d" % r, psk[hf]], writes=["zt"])
                            yb = yc % 2
                            yc += 1
                            layer_norm(zt, "zt", 2, 3, yo[yb][:], "yo%d" % yb)
                            op("sp", lambda h, yb=yb, tt=tt, s=s: h.dma_start(out=x_dst[s, tt:tt + 128, :], in_=yo[yb][:]), reads=["yo%d" % yb], writes=["xdst"], dma="st_y", nowaw=True)
                P.barrier()
                phase_ctr[0] += 1
                if stop is not None and phase_ctr[0] >= stop:
                    P.stopped = True
        except _Stop:
            pass
        P.final_wait("sp")
    return nc


def _consts():
    ident = np.eye(128, dtype=np.float32)
    so = np.arange(128)[:, None]
    to = np.arange(128)[None, :]
    diag = np.zeros((128, 4, 128), np.float32)
    for h in range(4):
        v = (-np.abs(to - so) + (to - 128)).astype(np.float32) * SLOPES[h] * 8.0
        v = np.where((so // 64) > (to // 64), NEG * 8.0, v)
        diag[:, h, :] = v
    alibi = np.zeros((128, 4 * 34), np.float32)
    for h in range(4):
        for k in range(34):
            alibi[:, h * 34 + k] = SLOPES[h] * (np.arange(128) - 128.0 * k)
    cmask = np.where((to // 64) > (so // 64), -1e30, 0.0).astype(np.float32)
    gmask = ((so // 64) <= (to // 64)).astype(np.float32)
    jpos = np.tile(np.arange(1, 33, dtype=np.float32)[None, :], (128, 1))
    return ident, diag, alibi, cmask, gmask, jpos


_CACHE = {}


def kernel(**inputs):
    n = 8
    f = lambda a: np.ascontiguousarray(np.asarray(a, dtype=np.float32))
    x = f(inputs["x"])
    ident, diag, alibi, cmask, gmask, jpos = _consts()
    lamv = np.stack([f(inputs["lam_q1"]), f(inputs["lam_k1"]), f(inputs["lam_q2"]), f(inputs["lam_k2"])], axis=1)
    shared = {
        "w_in": f(inputs["w_in"]),
        "gmlp_w_s": f(inputs["gmlp_w_s"]),
        "gmlp_b_s": f(inputs["gmlp_b_s"]),
        "gmlp_ln_g": f(inputs["gmlp_ln_g"]).reshape(DEPTH, 1, 256),
        "gmlp_ln_b": f(inputs["gmlp_ln_b"]).reshape(DEPTH, 1, 256),
        "lamv": np.ascontiguousarray(lamv),
        "diff_subln_g": f(inputs["diff_subln_g"]).reshape(DEPTH, 128, 1),
        "w_out": f(inputs["w_out"]),
        "ln1_g": f(inputs["ln1_g"]).reshape(DEPTH, 1, D),
        "ln1_b": f(inputs["ln1_b"]).reshape(DEPTH, 1, D),
        "w_gu": f(inputs["w_gu"]),
        "w_down": f(inputs["w_down"]),
        "ln2_g": f(inputs["ln2_g"]).reshape(DEPTH, 1, D),
        "ln2_b": f(inputs["ln2_b"]).reshape(DEPTH, 1, D),
        "c_ident": ident, "c_diag": diag, "c_alibi": alibi, "c_cmask": cmask, "c_gmask": gmask, "c_jpos": jpos,
    }
    if "nc" not in _CACHE:
        _CACHE["nc"] = build_program()
    nc = _CACHE["nc"]
    in_maps = []
    for c in range(n):
        m = dict(shared)
        m["x"] = np.ascontiguousarray(x[NSEQ * c:NSEQ * (c + 1)])
        in_maps.append(m)
    res = run_bass_kernel_spmd(nc, in_maps, core_ids=list(range(n)))
    out = np.concatenate([np.asarray(r["y"], dtype=np.float32) for r in res.results], axis=0)
    return out
```
